# Optimizing a Trainium2 kernel written in Bass

```python
import math
import jax
import jax.numpy as jnp
from jax import lax
import numpy as np

D_MODEL = 2048
BATCH = 16
SEQ = 256
DEPTH = 1
DEC_BATCH = 2
DEC_SEQ = 4096
PAST_LEN = 256

GRID_W = 64
EPS = 1e-6
N_MOD = 6

D_HYENA = 1024
HYENA_ORDER = 2
SHORT_CONV = 3
FILTER_BANDS = 16
FILTER_EMB = 1 + 2 * FILTER_BANDS
FILTER_HIDDEN = 64
FILTER_OUT = HYENA_ORDER * 2 * D_HYENA
DECAY_TARGET = 1e-2
SHORT_DECAY_PCT = 0.3
LONG_DECAY_PCT = 1.5

N_HEADS = 16
QK_NOPE = 128
QK_ROPE = 64
QK_HEAD = QK_NOPE + QK_ROPE
V_HEAD = 128
Q_LORA = 512
KV_LORA = 256
ROPE_THETA = 10000.0
Q_BLOCK = 128

N_EXPERTS = 32
TOP_K = 4
D_EXPERT = 2048
SWIGLU_LIMIT = 7.0
SWIGLU_ALPHA = 1.702
MOE_BLOCK = 128

IN_COLS = 3 * D_HYENA + Q_LORA + KV_LORA + QK_ROPE + 2 * D_MODEL
IN_SPLITS = (3 * D_HYENA,
             3 * D_HYENA + Q_LORA,
             3 * D_HYENA + Q_LORA + KV_LORA,
             3 * D_HYENA + Q_LORA + KV_LORA + QK_ROPE,
             3 * D_HYENA + Q_LORA + KV_LORA + QK_ROPE + D_MODEL)

kernel_name = 'hyena_mla_moe_diffusion_step'


def rms_norm(x, w):
    xf = x.astype(jnp.float32)
    y = xf * lax.rsqrt(jnp.mean(xf * xf, axis=-1, keepdims=True) + EPS)
    return (y * w.astype(jnp.float32)).astype(x.dtype)


def modulation(cvec, w_mod, b_mod):
    mod = jax.nn.silu(cvec) @ w_mod + b_mod
    return jnp.split(mod, N_MOD, axis=-1)


def axial_rope(n_tokens):
    rows = n_tokens // GRID_W
    row = jnp.repeat(jnp.arange(rows, dtype=jnp.float32), GRID_W)
    col = jnp.tile(jnp.arange(GRID_W, dtype=jnp.float32), rows)
    n_freq = QK_ROPE // 4
    inv_freq = jnp.power(ROPE_THETA, -jnp.arange(n_freq, dtype=jnp.float32) / n_freq)
    ang = jnp.concatenate([row[:, None] * inv_freq, col[:, None] * inv_freq], axis=-1)
    ang = jnp.concatenate([ang, ang], axis=-1)
    return jnp.cos(ang), jnp.sin(ang)


def apply_rope(x, cos, sin):
    xf = x.astype(jnp.float32)
    half = QK_ROPE // 2
    rot = jnp.concatenate([-xf[..., half:], xf[..., :half]], axis=-1)
    return (xf * cos + rot * sin).astype(x.dtype)


def centred_short_conv(u, w, b):
    n = u.shape[1]
    pad = SHORT_CONV // 2
    up = jnp.pad(u, ((0, 0), (pad, pad), (0, 0)))
    out = b
    for j in range(SHORT_CONV):
        out = out + up[:, j:j + n] * w[j]
    return out


def implicit_filters(n, w1, b1, w2, b2, w3, b3, freq):
    f32 = jnp.float32
    t = jnp.linspace(0.0, 1.0, n, dtype=f32)[:, None]
    w = (2.0 * math.pi / n) * jnp.arange(n, dtype=f32)[:, None]
    bands = jnp.linspace(1e-4, FILTER_BANDS - 1, FILTER_BANDS, dtype=f32)[None, :]
    feats = jnp.concatenate([t, jnp.cos(bands * w), -jnp.sin(bands * w)], axis=-1)
    freq = freq.astype(f32)
    hdn = jnp.sin(freq[0] * (feats @ w1.astype(f32) + b1.astype(f32)))
    hdn = jnp.sin(freq[1] * (hdn @ w2.astype(f32) + b2.astype(f32)))
    filt = hdn @ w3.astype(f32) + b3.astype(f32)
    max_decay = math.log(DECAY_TARGET) / SHORT_DECAY_PCT
    min_decay = math.log(DECAY_TARGET) / LONG_DECAY_PCT
    deltas = jnp.linspace(min_decay, max_decay, D_HYENA, dtype=f32)
    deltas = jnp.tile(deltas, HYENA_ORDER * 2)
    return filt * jnp.exp(-t * jnp.abs(deltas)[None, :])


def bidirectional_long_conv(z, h_fwd, h_bwd, skip):
    n = z.shape[1]
    k = jnp.concatenate([h_fwd, jnp.zeros_like(h_fwd[:1]), jnp.flip(h_bwd[:n - 1], axis=0)], axis=0)
    zf = jnp.fft.rfft(z.astype(jnp.float32), n=2 * n, axis=1)
    kf = jnp.fft.rfft(k, n=2 * n, axis=0)
    y = jnp.fft.irfft(zf * kf[None], n=2 * n, axis=1)[:, :n]
    return (y + z.astype(jnp.float32) * skip.astype(jnp.float32)).astype(z.dtype)


def hyena_operator(u3, lp):
    n = u3.shape[1]
    u3 = centred_short_conv(u3, lp['hy_conv_w'], lp['hy_conv_b'])
    v, x1, x2 = jnp.split(u3, 3, axis=-1)
    h = implicit_filters(n, lp['filt_w1'], lp['filt_b1'], lp['filt_w2'], lp['filt_b2'],
                         lp['filt_w3'], lp['filt_b3'], lp['filt_freq'])
    h = h.reshape(n, HYENA_ORDER, 2, D_HYENA)
    z = v
    for o, gate in enumerate((x1, x2)):
        z = gate * bidirectional_long_conv(z, h[:, o, 0], h[:, o, 1], lp['hy_skip'][o])
    return z


def mla_attention(q_nope, q_pe, k_nope, k_pe, v):
    b, lq = q_nope.shape[:2]
    nb = lq // Q_BLOCK
    scale = QK_HEAD ** -0.5
    qn = q_nope.reshape(b, nb, Q_BLOCK, N_HEADS, QK_NOPE).transpose(1, 0, 2, 3, 4)
    qp = q_pe.reshape(b, nb, Q_BLOCK, N_HEADS, QK_ROPE).transpose(1, 0, 2, 3, 4)

    def block(args):
        qn_b, qp_b = args
        s = (jnp.einsum('bqhd,bkhd->bhqk', qn_b, k_nope, preferred_element_type=jnp.float32)
             + jnp.einsum('bqhr,bkr->bhqk', qp_b, k_pe, preferred_element_type=jnp.float32))
        p = jax.nn.softmax(s * scale, axis=-1).astype(v.dtype)
        return jnp.einsum('bhqk,bkhd->bqhd', p, v)

    o = lax.map(block, (qn, qp))
    return o.transpose(1, 0, 2, 3, 4).reshape(b, lq, N_HEADS * V_HEAD)


def token_mixer(h, lp, rope, ctx_kv):
    b, n, _ = h.shape
    proj = jnp.einsum('bld,dc->blc', h, lp['w_in'])
    u3, q_c, kv_c, kpe_raw, g_hy, g_mla = jnp.split(proj, IN_SPLITS, axis=-1)
    y_hy = hyena_operator(u3, lp)
    q = (rms_norm(q_c, lp['q_a_norm']) @ lp['w_uq']).reshape(b, n, N_HEADS, QK_HEAD)
    q_nope = rms_norm(q[..., :QK_NOPE], lp['qn_norm'])
    q_pe = rms_norm(q[..., QK_NOPE:], lp['qr_norm'])
    c_kv = rms_norm(kv_c, lp['kv_a_norm'])
    k_pe = rms_norm(kpe_raw, lp['kr_norm'])
    if rope is not None:
        cos, sin = rope
        q_pe = apply_rope(q_pe, cos[:, None, :], sin[:, None, :])
        k_pe = apply_rope(k_pe, cos, sin)
    if ctx_kv is None:
        ckv_all, kpe_all = c_kv, k_pe
    else:
        ckv_all = jnp.concatenate([c_kv, ctx_kv[0]], axis=1)
        kpe_all = jnp.concatenate([k_pe, ctx_kv[1]], axis=1)
    kv = (ckv_all @ lp['w_ukv']).reshape(b, ckv_all.shape[1], N_HEADS, QK_NOPE + V_HEAD)
    k_nope = rms_norm(kv[..., :QK_NOPE], lp['kn_norm'])
    v = kv[..., QK_NOPE:]
    y_mla = mla_attention(q_nope, q_pe, k_nope, kpe_all, v)
    merged = (jax.nn.sigmoid(g_hy) * (y_hy @ lp['w_hy_out'])
              + jax.nn.sigmoid(g_mla) * (y_mla @ lp['w_mla_out']))
    return merged @ lp['w_o'], c_kv, k_pe


def moe_ffn(x, w_router, b_router, w_gate_up, b_gate_up, w_down, b_down):
    b, n, d = x.shape
    t = b * n
    xt = x.reshape(t, d)
    logits = jnp.einsum('td,de->te', xt, w_router, preferred_element_type=jnp.float32) + b_router.astype(jnp.float32)
    top_logits, top_idx = lax.top_k(logits, TOP_K)
    top_w = jax.nn.softmax(top_logits, axis=-1)
    n_assign = t * TOP_K
    flat_e = top_idx.reshape(n_assign)
    flat_tok = jnp.arange(n_assign, dtype=jnp.int32) // TOP_K
    flat_w = top_w.reshape(n_assign)
    order = jnp.argsort(flat_e)
    e_sorted = flat_e[order]
    counts = jnp.zeros((N_EXPERTS,), jnp.int32).at[flat_e].add(1)
    padded = (counts + MOE_BLOCK - 1) // MOE_BLOCK * MOE_BLOCK
    pad_end = jnp.cumsum(padded)
    pad_start = pad_end - padded
    grp_start = jnp.cumsum(counts) - counts
    dest = pad_start[e_sorted] + jnp.arange(n_assign, dtype=jnp.int32) - grp_start[e_sorted]
    n_blocks = -(-n_assign // MOE_BLOCK) + N_EXPERTS
    cap = n_blocks * MOE_BLOCK
    slot_tok = jnp.full((cap,), t, jnp.int32).at[dest].set(flat_tok[order])
    slot_w = jnp.zeros((cap,), jnp.float32).at[dest].set(flat_w[order])
    block_start = jnp.arange(n_blocks, dtype=jnp.int32) * MOE_BLOCK
    block_expert = jnp.minimum(jnp.searchsorted(pad_end, block_start, side='right'), N_EXPERTS - 1)
    x_pad = jnp.concatenate([xt, jnp.zeros((1, d), xt.dtype)], axis=0)

    def expert_block(args):
        tok, e = args
        xb = x_pad[tok]
        gu = xb @ w_gate_up[e] + b_gate_up[e]
        gate = jnp.minimum(gu[:, 0::2], SWIGLU_LIMIT)
        up = jnp.clip(gu[:, 1::2], -SWIGLU_LIMIT, SWIGLU_LIMIT)
        hid = gate * jax.nn.sigmoid(SWIGLU_ALPHA * gate) * (up + 1.0)
        return hid @ w_down[e] + b_down[e]

    out = lax.map(expert_block, (slot_tok.reshape(n_blocks, MOE_BLOCK), block_expert))
    out = out.reshape(cap, d) * slot_w[:, None].astype(out.dtype)
    y = jax.ops.segment_sum(out, slot_tok, num_segments=t + 1)[:t]
    return y.reshape(b, n, d)


def trunk_layer(x, mod, lp, rope, ctx_kv):
    shift1, scale1, gate1, shift2, scale2, gate2 = mod
    h = rms_norm(x, lp['norm1']) * (1.0 + scale1[:, None, :]) + shift1[:, None, :]
    mix, c_kv, k_pe = token_mixer(h, lp, rope, ctx_kv)
    x = x + gate1[:, None, :] * mix
    h = rms_norm(x, lp['norm2']) * (1.0 + scale2[:, None, :]) + shift2[:, None, :]
    x = x + gate2[:, None, :] * moe_ffn(h, lp['w_router'], lp['b_router'], lp['w_gate_up'],
                                        lp['b_gate_up'], lp['w_down'], lp['b_down'])
    return x, c_kv, k_pe


def setup_inputs(seed: int = 0) -> dict:
    key = jax.random.key(seed)
    ks = jax.random.split(key, 38)
    f32 = jnp.float32

    def nrm(k, shape, std):
        return std * jax.random.normal(k, shape, f32)

    def gain(k, shape):
        return 1.0 + 0.05 * jax.random.normal(k, shape, f32)

    return {
        'x_prompt': nrm(ks[0], (BATCH, SEQ, D_MODEL), 1.0),
        'x_sample': nrm(ks[1], (DEC_BATCH, DEC_SEQ, D_MODEL), 1.0),
        'cache_ckv': nrm(ks[2], (DEC_BATCH, DEPTH, PAST_LEN, KV_LORA), 1.0),
        'cache_kpe': nrm(ks[3], (DEC_BATCH, DEPTH, PAST_LEN, QK_ROPE), 1.0),
        'c': nrm(ks[4], (DEC_BATCH, D_MODEL), 1.0),
        'c_ctx': nrm(ks[5], (D_MODEL,), 1.0),
        'w_mod': nrm(ks[6], (DEPTH, D_MODEL, N_MOD * D_MODEL), D_MODEL ** -0.5),
        'b_mod': nrm(ks[7], (DEPTH, N_MOD * D_MODEL), 0.01),
        'norm1_w': gain(ks[8], (DEPTH, D_MODEL)),
        'norm2_w': gain(ks[9], (DEPTH, D_MODEL)),
        'w_in': nrm(ks[10], (DEPTH, D_MODEL, IN_COLS), D_MODEL ** -0.5),
        'hy_conv_w': nrm(ks[11], (DEPTH, SHORT_CONV, 3 * D_HYENA), SHORT_CONV ** -0.5),
        'hy_conv_b': nrm(ks[12], (DEPTH, 3 * D_HYENA), 0.01),
        'filt_w1': nrm(ks[13], (DEPTH, FILTER_EMB, FILTER_HIDDEN), FILTER_EMB ** -0.5),
        'filt_b1': nrm(ks[14], (DEPTH, FILTER_HIDDEN), 0.1),
        'filt_w2': nrm(ks[15], (DEPTH, FILTER_HIDDEN, FILTER_HIDDEN), FILTER_HIDDEN ** -0.5),
        'filt_b2': nrm(ks[16], (DEPTH, FILTER_HIDDEN), 0.1),
        'filt_w3': nrm(ks[17], (DEPTH, FILTER_HIDDEN, FILTER_OUT), 0.005),
        'filt_b3': nrm(ks[18], (DEPTH, FILTER_OUT), 0.002),
        'filt_freq': 1.0 + 0.1 * jax.random.normal(ks[19], (DEPTH, 2, FILTER_HIDDEN), f32),
        'hy_skip': 1.0 + 0.1 * jax.random.normal(ks[20], (DEPTH, HYENA_ORDER, D_HYENA), f32),
        'q_a_norm_w': gain(ks[21], (DEPTH, Q_LORA)),
        'w_uq': nrm(ks[22], (DEPTH, Q_LORA, N_HEADS * QK_HEAD), Q_LORA ** -0.5),
        'kv_a_norm_w': gain(ks[23], (DEPTH, KV_LORA)),
        'w_ukv': nrm(ks[24], (DEPTH, KV_LORA, N_HEADS * (QK_NOPE + V_HEAD)), KV_LORA ** -0.5),
        'qn_norm_w': gain(ks[25], (DEPTH, QK_NOPE)),
        'kn_norm_w': gain(ks[26], (DEPTH, QK_NOPE)),
        'qr_norm_w': gain(ks[27], (DEPTH, QK_ROPE)),
        'kr_norm_w': gain(ks[28], (DEPTH, QK_ROPE)),
        'w_hy_out': nrm(ks[29], (DEPTH, D_HYENA, D_MODEL), D_HYENA ** -0.5),
        'w_mla_out': nrm(ks[30], (DEPTH, N_HEADS * V_HEAD, D_MODEL), (N_HEADS * V_HEAD) ** -0.5),
        'w_o': nrm(ks[31], (DEPTH, D_MODEL, D_MODEL), D_MODEL ** -0.5),
        'w_router': nrm(ks[32], (DEPTH, D_MODEL, N_EXPERTS), D_MODEL ** -0.5),
        'b_router': nrm(ks[33], (DEPTH, N_EXPERTS), 0.01),
        'w_gate_up': nrm(ks[34], (DEPTH, N_EXPERTS, D_MODEL, 2 * D_EXPERT), D_MODEL ** -0.5),
        'b_gate_up': nrm(ks[35], (DEPTH, N_EXPERTS, 2 * D_EXPERT), 0.01),
        'w_down': nrm(ks[36], (DEPTH, N_EXPERTS, D_EXPERT, D_MODEL), D_EXPERT ** -0.5),
        'b_down': nrm(ks[37], (DEPTH, N_EXPERTS, D_MODEL), 0.01),
    }


def reference(x_prompt, x_sample, cache_ckv, cache_kpe, c, c_ctx, w_mod, b_mod, norm1_w, norm2_w,
              w_in, hy_conv_w, hy_conv_b, filt_w1, filt_b1, filt_w2, filt_b2, filt_w3, filt_b3,
              filt_freq, hy_skip, q_a_norm_w, w_uq, kv_a_norm_w, w_ukv, qn_norm_w, kn_norm_w,
              qr_norm_w, kr_norm_w, w_hy_out, w_mla_out, w_o, w_router, b_router, w_gate_up,
              b_gate_up, w_down, b_down):
    rope_lat = axial_rope(x_sample.shape[1])
    y_p = x_prompt
    y_s = x_sample
    ckv_layers = []
    kpe_layers = []
    for l in range(DEPTH):
        lp = {
            'norm1': norm1_w[l], 'norm2': norm2_w[l], 'w_in': w_in[l],
            'hy_conv_w': hy_conv_w[l], 'hy_conv_b': hy_conv_b[l],
            'filt_w1': filt_w1[l], 'filt_b1': filt_b1[l], 'filt_w2': filt_w2[l], 'filt_b2': filt_b2[l],
            'filt_w3': filt_w3[l], 'filt_b3': filt_b3[l], 'filt_freq': filt_freq[l], 'hy_skip': hy_skip[l],
            'q_a_norm': q_a_norm_w[l], 'w_uq': w_uq[l], 'kv_a_norm': kv_a_norm_w[l], 'w_ukv': w_ukv[l],
            'qn_norm': qn_norm_w[l], 'kn_norm': kn_norm_w[l], 'qr_norm': qr_norm_w[l], 'kr_norm': kr_norm_w[l],
            'w_hy_out': w_hy_out[l], 'w_mla_out': w_mla_out[l], 'w_o': w_o[l],
            'w_router': w_router[l], 'b_router': b_router[l], 'w_gate_up': w_gate_up[l],
            'b_gate_up': b_gate_up[l], 'w_down': w_down[l], 'b_down': b_down[l],
        }
        mod_ctx = modulation(c_ctx[None, :], w_mod[l], b_mod[l])
        y_p, ckv_l, kpe_l = trunk_layer(y_p, mod_ctx, lp, None, None)
        ckv_layers.append(ckv_l)
        kpe_layers.append(kpe_l)
        mod_lat = modulation(c, w_mod[l], b_mod[l])
        y_s, _, _ = trunk_layer(y_s, mod_lat, lp, rope_lat, (cache_ckv[:, l], cache_kpe[:, l]))
    new_ckv = jnp.stack(ckv_layers, axis=1)
    new_kpe = jnp.stack(kpe_layers, axis=1)
    return (y_p, y_s, new_ckv, new_kpe)
```

```python
import contextlib
import math
import numpy as np
import concourse.bass as bass
import concourse.mybir as mybir
from concourse.bass_utils import run_bass_kernel_spmd

F32 = mybir.dt.float32
BF16 = mybir.dt.bfloat16
ALU = mybir.AluOpType
AF = mybir.ActivationFunctionType
AX = mybir.AxisListType
ENGS = ("pe", "act", "dve", "pool", "sp")

D = 2048
NE = 32
EPS = 1e-6
_NC_CACHE = {}


class Buf:
    __slots__ = ("name", "t", "w", "r", "dsem", "dcnt")

    def __init__(self, name, t=None):
        self.name = name
        self.t = t
        self.w = None
        self.r = []
        self.dsem = None
        self.dcnt = 0

    def __getitem__(self, idx):
        return self.t[idx]


class Op:
    __slots__ = ("eng", "fn", "reads", "writes", "dma", "idx", "waits", "mark", "dtok", "key")

    def __init__(self, eng, fn, reads, writes, dma=False, key=None):
        self.eng, self.fn, self.reads, self.writes, self.dma, self.key = eng, fn, reads, writes, dma, key
        self.waits = []
        self.mark = False
        self.dtok = None


class Sched:
    def __init__(self, nc):
        self.nc = nc
        self.ops = []
        self.bufs = []

    def buf(self, name, t=None):
        b = Buf(name, t)
        self.bufs.append(b)
        return b

    def op(self, eng, fn, reads=(), writes=()):
        self.ops.append(Op(eng, fn, tuple(reads), tuple(writes)))

    def dma(self, eng, fn, reads=(), writes=(), key=None):
        self.ops.append(Op(eng, fn, tuple(reads), tuple(writes), dma=True, key=key))

    def barrier(self):
        bb = Buf("bar%d" % len(self.ops))
        allb = list(self.bufs)
        self.ops.append(Op("sp", lambda e: e.nop(), (), tuple(allb) + (bb,)))
        for e in ("pe", "act", "dve", "pool"):
            self.ops.append(Op(e, lambda en: en.nop(), (bb,), ()))

    def finish(self, stack):
        nc = self.nc
        per_eng = {e: [] for e in ENGS}
        for o in self.ops:
            o.idx = len(per_eng[o.eng])
            per_eng[o.eng].append(o)
        esem = {e: stack.enter_context(nc.semaphore("s_" + e)) for e in ENGS}
        waited_e = {e: {f: -1 for f in ENGS} for e in ENGS}
        waited_d = {e: {} for e in ENGS}
        sem_pool = []
        for o in self.ops:
            deps = []
            for b in o.reads:
                if b.w is not None:
                    deps.append(b.w)
            for b in o.writes:
                if b.w is not None:
                    deps.append(b.w)
                deps.extend(b.r)
            if o.dma:
                k = o.key if o.key is not None else (o.writes[0] if o.writes else o.reads[0])
                if k.dsem is None:
                    k.dsem = stack.enter_context(nc.semaphore("d%d" % len(sem_pool)))
                    sem_pool.append(k.dsem)
                k.dcnt += 16
                tok = ("d", k, k.dcnt)
                o.dtok = tok
            else:
                tok = ("e", o.eng, o.idx)
            for t in deps:
                if t[0] == "e":
                    _, pe_, pidx = t
                    if pe_ == o.eng and pe_ == "pe" and not o.dma:
                        continue
                    if waited_e[o.eng][pe_] >= pidx:
                        continue
                    waited_e[o.eng][pe_] = pidx
                    o.waits.append(t)
                    per_eng[pe_][pidx].mark = True
                else:
                    _, k, val = t
                    if waited_d[o.eng].get(id(k), 0) >= val:
                        continue
                    waited_d[o.eng][id(k)] = val
                    o.waits.append(t)
            for b in o.reads:
                b.r.append(tok)
            for b in o.writes:
                b.w = tok
                b.r = []
        sig = {e: [] for e in ENGS}
        for e in ENGS:
            c = 0
            for o in per_eng[e]:
                if o.mark and not o.dma:
                    c += 1
                sig[e].append(c)
        self.nsem = len(sem_pool)
        self.nops = {e: len(per_eng[e]) for e in ENGS}
        with nc.Block() as block:
            def make(e):
                def body(engine):
                    for o in per_eng[e]:
                        for t in o.waits:
                            if t[0] == "e":
                                engine.wait_ge(esem[t[1]], sig[t[1]][t[2]])
                            else:
                                engine.wait_ge(t[1].dsem, t[2])
                        ins = o.fn(engine)
                        if o.dma:
                            ins.then_inc(o.dtok[1].dsem, 16)
                        elif o.mark:
                            ins.then_inc(esem[e], 1)
                return body
            block.tensor(make("pe"))
            block.scalar(make("act"))
            block.vector(make("dve"))
            block.gpsimd(make("pool"))
            block.sync(make("sp"))


NTOK = 1536
NSEQ = 4096
NKEY = 4352
L1S, L2S, LP = 8192, 5120, 512
HY = 1024


def build(dbg=False, skip_moe=False, ncc=8, hy_only=False):
    nc = bass.Bass("TRN2", target_bir_lowering=False)
    st = contextlib.ExitStack()
    S = Sched(nc)

    in_names = []
    _NC_CACHE["in_names"] = in_names

    def din(name, shape, dt=F32):
        in_names.append(name)
        return nc.dram_tensor(name, list(shape), dt, kind="ExternalInput").ap()

    def dout(name, shape, dt=F32):
        return nc.dram_tensor(name, list(shape), dt, kind="ExternalOutput").ap()

    def dscr(name, shape, dt):
        return nc.dram_tensor(name, list(shape), dt, kind=("ExternalOutput" if dbg else "Internal")).ap()

    x_own = din("x_own", [NTOK, D]); x_seq = din("x_seq", [NSEQ, D])
    cvec = din("cvec", [4, D]); w_mod = din("w_mod", [D, 6 * D]); b_mod = din("b_mod", [6 * D])
    norm1_w = din("norm1_w", [D]); norm2_w = din("norm2_w", [D])
    w_in = din("w_in", [D, 8000])
    convw = din("convw", [3, 3072]); convb = din("convb", [3072])
    fw1 = din("fw1", [33, 64]); fb1 = din("fb1", [64]); fw2 = din("fw2", [64, 64]); fb2 = din("fb2", [64])
    fw3 = din("fw3", [64, 4096]); fb3 = din("fb3", [4096]); ffreq = din("ffreq", [2, 64]); skipw = din("skipw", [2, HY])
    q_a_w = din("q_a_w", [512]); w_uq = din("w_uq", [512, 3072]); kv_a_w = din("kv_a_w", [256]); w_ukv = din("w_ukv", [256, 4096])
    qn_w = din("qn_w", [128]); kn_w = din("kn_w", [128]); qr_w = din("qr_w", [64]); kr_w = din("kr_w", [64])
    w_hy_out = din("w_hy_out", [HY, D]); w_mla_out = din("w_mla_out", [D, D]); w_o = din("w_o", [D, D])
    w_router = din("w_router", [D, NE]); b_router = din("b_router", [NE])
    b_gu = din("b_gu", [NE, 2 * D]); b_dn = din("b_dn", [NE, D])
    if not skip_moe:
        w_gu = din("w_gu", [NE, D, 2 * D]); w_dn = din("w_dn", [NE, D, D])
    c_ckv = din("c_ckv", [256, 256]); c_kpe = din("c_kpe", [256, 64])
    ident = din("ident", [128, 128]); jdent = din("jdent", [128, 128]); prot = din("prot", [64, 64])
    ftab = {}
    for nm, L in (("s1", L1S), ("s2", L2S), ("p1", LP), ("p2", LP)):
        ftab[nm] = (din("feat_" + nm, [33, L]), din("aux_" + nm, [4, L]), L)
    absdel = din("absdel", [HY])
    ropek = din("ropek", [2, 64, NSEQ])
    ropeq = din("ropeq", [2, 64, 1024])
    y_out = dout("y_out", [NTOK, D]); ckv_out = dout("ckv_out", [512, 256]); kpe_out = dout("kpe_out", [512, 64])
    modD = dscr("modD", [4, 6 * D], F32)
    hTs = dscr("hTs", [D, NSEQ], BF16); hTo = dscr("hTo", [D, NTOK], BF16)
    Rt = {nm: dscr("R_" + nm, [HY, ftab[nm][2]], BF16) for nm in ftab}
    yacc = dscr("yacc", [NTOK, D], F32)
    x1d = dscr("x1d", [NTOK, D], F32)
    if dbg:
        yhT_d = dout("yhT_d", [128, 8, NTOK], BF16); ymT_d = dout("ymT_d", [128, 16, NTOK], BF16)
        wts_d = dout("wts_d", [128, 12, NE]); h2T_d = dout("h2T_d", [128, 16, NTOK], BF16)

    ARW = 49000
    arena = st.enter_context(nc.sbuf_tensor("arena", [128, ARW], F32))
    ar_pos = [0]

    def alloc(name, shape, dt=F32):
        free = int(np.prod(shape[1:]))
        words = free if dt == F32 else (free + 1) // 2
        words = (words + 7) // 8 * 8
        o = ar_pos[0]
        ar_pos[0] += words
        assert ar_pos[0] <= ARW, (name, ar_pos[0])
        ap = arena[0:shape[0], o:o + words]
        if dt != F32:
            ap = ap.bitcast(dt)[:, 0:free]
        else:
            ap = ap[:, 0:free]
        if len(shape) > 2:
            names = " ".join("d%d" % i for i in range(len(shape) - 1))
            kw = {"d%d" % i: shape[i + 1] for i in range(len(shape) - 1)}
            ap = ap.rearrange("p (%s) -> p %s" % (names, names), **kw)
        return S.buf(name, ap)

    def phase_reset(mark):
        S.barrier()
        ar_pos[0] = mark

    psum = [S.buf("ps%d" % i, st.enter_context(nc.psum_tensor("ps%d" % i, [128, 512], F32))) for i in range(8)]

    def psb(i, dt=F32):
        return psum[i].t if dt == F32 else psum[i].t.bitcast(dt)

    dB = {n: S.buf("D_" + n) for n in ("modD", "hTs", "hTo", "R", "yacc", "x1d", "out", "ckv", "kpe", "dbg")}

    def mm(out, lhsT, rhs, start, stop, reads, writes):
        S.op("pe", lambda e: e.matmul(out, lhsT=lhsT, rhs=rhs, start=start, stop=stop), reads, writes)

    def tr(out, in_, idn, reads, writes):
        S.op("pe", lambda e: e.transpose(out, in_, idn), reads, writes)

    def act(out, in_, func, reads, writes, bias=None, scale=None, accum=None, eng="act"):
        kw = {}
        if bias is not None:
            kw["bias"] = bias
        if scale is not None:
            kw["scale"] = scale
        if accum is not None:
            kw["accum_out"] = accum
        S.op(eng, lambda e: e.activation(out=out, in_=in_, func=func, **kw), reads, writes)

    def ts(out, in0, s1, s2, op0, op1, reads, writes, eng="dve"):
        if op1 is None:
            S.op(eng, lambda e: e.tensor_scalar(out=out, in0=in0, scalar1=s1, scalar2=None, op0=op0), reads, writes)
        else:
            S.op(eng, lambda e: e.tensor_scalar(out=out, in0=in0, scalar1=s1, scalar2=s2, op0=op0, op1=op1), reads, writes)

    def tt(out, in0, in1, op, reads, writes, eng="dve"):
        S.op(eng, lambda e: e.tensor_tensor(out=out, in0=in0, in1=in1, op=op), reads, writes)

    def stt(out, in0, sc, in1, op0, op1, reads, writes, eng="dve"):
        S.op(eng, lambda e: e.scalar_tensor_tensor(out=out, in0=in0, scalar=sc, in1=in1, op0=op0, op1=op1), reads, writes)

    def cp(out, in_, reads, writes, eng="dve"):
        if eng == "act":
            S.op(eng, lambda e: e.activation(out=out, in_=in_, func=AF.Copy), reads, writes)
        else:
            S.op(eng, lambda e: e.tensor_copy(out=out, in_=in_), reads, writes)

    def ld(out, in_, reads, writes, eng="sp", slow=False, key=None):
        if slow:
            S.dma(eng, lambda e: e.dma_start(out=out, in_=in_, allow_slow_non_contiguous=True), reads, writes, key=key)
        else:
            S.dma(eng, lambda e: e.dma_start(out=out, in_=in_), reads, writes, key=key)

    def rsqrt_(out, in_, mult, buf):
        ts(out, in_, mult, EPS, ALU.mult, ALU.add, [buf], [buf])
        act(out, out, AF.Sqrt, [buf], [buf])
        S.op("dve", lambda e: e.reciprocal(out=out, in_=out), [buf], [buf])

    idf = alloc("idf", [128, 128]); idb = alloc("idb", [128, 128], BF16); jdb = alloc("jdb", [128, 128], BF16)
    onesb = alloc("onesb", [128, 128], BF16); protb = alloc("protb", [64, 64], BF16)
    tmpc = alloc("tmpc", [128, 128])
    ld(idf[:], ident, [], [idf])
    cp(idb[:], idf[:], [idf], [idb])
    ld(tmpc[:], jdent, [], [tmpc]); cp(jdb[:], tmpc[:], [tmpc], [jdb])
    ld(tmpc[0:64, 0:64], prot, [jdb], [tmpc]); cp(protb[:], tmpc[0:64, 0:64], [tmpc], [protb])
    S.op("dve", lambda e: e.memset(onesb[:], 1.0), [], [onesb])
    modT = alloc("modT", [128, 96, 4]); n1T = alloc("n1T", [128, 16]); n2T = alloc("n2T", [128, 16])
    gw1 = alloc("gw1", [128, 16, 4]); gw2 = alloc("gw2", [128, 16, 4])
    ld(n1T[:], norm1_w.rearrange("(c p) -> p c", p=128), [], [n1T], slow=True)
    ld(n2T[:], norm2_w.rearrange("(c p) -> p c", p=128), [], [n2T], slow=True)
    mark0 = ar_pos[0]
    cT = alloc("cT", [128, 16, 4]); bmT = alloc("bmT", [128, 96])
    for r in range(4):
        ld(cT[:, :, r], cvec[r].rearrange("(c p) -> p c", p=128), [], [cT], slow=True)
    ld(bmT[:], b_mod.rearrange("(q p) -> p q", p=128), [], [bmT], slow=True)
    act(cT[:], cT[:], AF.Silu, [cT], [cT])
    wm = alloc("wm", [128, 16, 512])
    for g in range(24):
        ld(wm[:], w_mod[:, 512 * g:512 * g + 512].rearrange("(c p) n -> p c n", p=128), [], [wm])
        for m in range(4):
            q = 4 * g + m
            pb = psum[q % 2]
            for kc in range(16):
                mm(pb.t[:, 0:4], wm[:, kc, 128 * m:128 * m + 128], cT[:, kc, :], kc == 0, kc == 15, [wm, cT], [pb])
            ts(modT[:, q, :], pb.t[:, 0:4], bmT[:, q:q + 1], None, ALU.add, None, [pb, bmT], [modT])
    for r in range(4):
        stt(gw1[:, :, r], modT[:, 16:32, r], 1.0, n1T[:], ALU.add, ALU.mult, [modT, n1T], [gw1])
        stt(gw2[:, :, r], modT[:, 64:80, r], 1.0, n2T[:], ALU.add, ALU.mult, [modT, n2T], [gw2])
    for r in range(4):
        ld(modD[r].rearrange("(q p) -> p q", p=128), modT[:, :, r], [modT], [dB["modD"]], slow=True)
    phase_reset(mark0)

    fw1s = alloc("fw1s", [33, 64]); fw2s = alloc("fw2s", [64, 64]); fw3s = alloc("fw3s", [64, 4096])
    fsc = alloc("fsc", [64, 6])
    ld(fw1s[:], fw1, [], [fw1s]); ld(fw2s[:], fw2, [], [fw2s]); ld(fw3s[:], fw3, [], [fw3s])
    ld(fsc[:, 0:1], fb1.rearrange("(p o) -> p o", o=1), [], [fsc], slow=True)
    ld(fsc[:, 1:2], fb2.rearrange("(p o) -> p o", o=1), [], [fsc], slow=True)
    for r in range(2):
        ld(fsc[:, 2 + r:3 + r], ffreq[r].rearrange("(p o) -> p o", o=1), [], [fsc], slow=True)
    inv2pi = 1.0 / (2.0 * math.pi)
    ts(fsc[:, 2:4], fsc[:, 2:4], inv2pi, None, ALU.mult, None, [fsc], [fsc])
    tt(fsc[:, 4:6], fsc[:, 0:2], fsc[:, 2:4], ALU.mult, [fsc], [fsc])
    fb3T = alloc("fb3T", [128, 32]); skT = alloc("skT", [128, 16]); adT = alloc("adT", [128, 8])
    ld(fb3T[:], fb3.rearrange("(q p) -> p q", p=128), [], [fb3T], slow=True)
    for r in range(2):
        ld(skT[:, 8 * r:8 * r + 8], skipw[r].rearrange("(q p) -> p q", p=128), [], [skT], slow=True)
    ld(adT[:], absdel.rearrange("(q p) -> p q", p=128), [], [adT], slow=True)
    ts(adT[:], adT[:], -1.0, None, ALU.mult, None, [adT], [adT])
    CH = 512
    featc = alloc("featc", [33, CH]); auxb = alloc("auxb", [128, 4, CH])
    h1 = alloc("h1", [64, CH]); h2 = alloc("h2", [64, CH]); tq = alloc("tq", [64, CH]); tk = alloc("tk", [64, CH])
    dec = alloc("dec", [128, CH]); ff = alloc("ff", [128, CH]); fbk = alloc("fbk", [128, CH]); rrow = alloc("rrow", [128, CH], BF16)
    MAGIC = 12582912.0
    SC2PI = 2.0 * math.pi * (1.0 - 2e-6)

    def sin_layer(outb, pb, col):
        ts(tq[:], pb.t[0:64, 0:CH], fsc[:, 2 + col:3 + col], fsc[:, 4 + col:5 + col], ALU.mult, ALU.add, [pb, fsc], [tq])
        ts(tk[:], tq[:], MAGIC, MAGIC, ALU.add, ALU.subtract, [tq], [tk])
        tt(tq[:], tq[:], tk[:], ALU.subtract, [tq, tk], [tq])
        act(outb[:], tq[:], AF.Sin, [tq], [outb], scale=SC2PI)

    for nm, o in (("s1", 0), ("s2", 1), ("p1", 0), ("p2", 1)):
        fdr, axr, L = ftab[nm]
        for c0 in range(0, L, CH):
            ld(featc[:], fdr[:, c0:c0 + CH], [], [featc])
            ld(auxb[:], axr[:, c0:c0 + CH].partition_broadcast(128), [], [auxb])
            mm(psum[0].t[0:64, 0:CH], fw1s[:, :], featc[:, :], True, True, [fw1s, featc], [psum[0]])
            sin_layer(h1, psum[0], 0)
            mm(psum[1].t[0:64, 0:CH], fw2s[:, :], h1[:, :], True, True, [fw2s, h1], [psum[1]])
            sin_layer(h2, psum[1], 1)
            for cc in range(8):
                qf = (o * 2 + 0) * 8 + cc
                qb = (o * 2 + 1) * 8 + cc
                mm(psum[2].t[:, 0:CH], fw3s[:, qf * 128:qf * 128 + 128], h2[:, :], True, True, [fw3s, h2], [psum[2]])
                mm(psum[3].t[:, 0:CH], fw3s[:, qb * 128:qb * 128 + 128], h2[:, :], True, True, [fw3s, h2], [psum[3]])
                ts(ff[:], psum[2].t[:, 0:CH], fb3T[:, qf:qf + 1], None, ALU.add, None, [psum[2], fb3T], [ff])
                ts(fbk[:], psum[3].t[:, 0:CH], fb3T[:, qb:qb + 1], None, ALU.add, None, [psum[3], fb3T], [fbk])
                tt(ff[:], ff[:], fbk[:], ALU.subtract, [ff, fbk], [ff])
                tt(ff[:], ff[:], auxb[:, 1, :], ALU.mult, [ff, auxb], [ff])
                tt(ff[:], ff[:], fbk[:], ALU.add, [ff, fbk], [ff])
                act(dec[:], auxb[:, 0, :], AF.Exp, [auxb, adT], [dec], scale=adT[:, cc:cc + 1])
                tt(ff[:], ff[:], dec[:], ALU.mult, [ff, dec], [ff])
                tt(ff[:], ff[:], auxb[:, 2, :], ALU.mult, [ff, auxb], [ff])
                stt(rrow[:], auxb[:, 3, :], skT[:, o * 8 + cc:o * 8 + cc + 1], ff[:], ALU.mult, ALU.add, [auxb, skT, ff], [rrow])
                ld(Rt[nm][cc * 128:cc * 128 + 128, c0:c0 + CH], rrow[:], [rrow], [dB["R"]])
    phase_reset(mark0)

    yhT = alloc("yhT", [128, 8, NTOK], BF16)
    markA = ar_pos[0]
    xt = alloc("xt", [128, D]); xs = alloc("xs", [128, D], BF16); junk = alloc("junk", [128, D], BF16)
    ssq = alloc("ssq", [128, 2]); hTt = alloc("hTt", [128, 16, 128], BF16)

    def norm_T(src_ap, src_reads, r, gw, shbase, emit_dst):
        ld(xt[:], src_ap, src_reads, [xt])
        act(junk[:], xt[:], AF.Square, [xt], [junk, ssq], accum=ssq[:, 0:1])
        rsqrt_(ssq[:, 0:1], ssq[:, 0:1], 1.0 / D, ssq)
        act(xs[:], xt[:], AF.Copy, [xt, ssq], [xs], scale=ssq[:, 0:1])
        for c in range(16):
            pb = psum[4 + c // 8]
            tr(psb(4 + c // 8, BF16)[:, (c % 8) * 128:(c % 8) * 128 + 128], xs[:, c * 128:c * 128 + 128], idb[:], [xs, idb], [pb])
        for c in range(16):
            pb = psum[4 + c // 8]
            ts(hTt[:, c, :], psb(4 + c // 8, BF16)[:, (c % 8) * 128:(c % 8) * 128 + 128], gw[:, c, r:r + 1],
               modT[:, shbase + c, r:r + 1], ALU.mult, ALU.add, [pb, gw, modT], [hTt])
        emit_dst()

    hTo_v = hTo.rearrange("(c p) n -> p c n", p=128)
    hTs_v = hTs.rearrange("(c p) n -> p c n", p=128)
    for t in range(12):
        norm_T(x_own[t * 128:t * 128 + 128, :], [], 0 if t < 4 else 3, gw1, 0,
               lambda t=t: ld(hTo_v[:, :, t * 128:t * 128 + 128], hTt[:], [hTt], [dB["hTo"]]))
    for t in range(32):
        norm_T(x_seq[t * 128:t * 128 + 128, :], [], 3, gw1, 0,
               lambda t=t: ld(hTs_v[:, :, t * 128:t * 128 + 128], hTt[:], [hTt], [dB["hTs"]]))
    phase_reset(markA)

    own1h = din("own1h", [4])
    mskj = alloc("mskj", [128, 4])
    ld(mskj[:], own1h.partition_broadcast(128), [], [mskj])
    wst = alloc("wst", [128, 16, 128]); whb = alloc("whb", [128, 16, 3, 128], BF16)
    cw = alloc("cw", [128, 3, 4])
    hTc = alloc("hTc", [128, 16, 512], BF16)
    Pb = alloc("Pb", [128, 3, NSEQ], BF16)
    ut = alloc("ut", [128, 3, 128], BF16); utf = alloc("utf", [128, 3, 128])
    Z = alloc("Z", [128, 32, 3, 128], BF16)
    z1 = alloc("z1", [128, 32, 128], BF16); z2 = alloc("z2", [128, 8, 128], BF16); x2o = alloc("x2o", [128, 8, 128])
    Gb = [alloc("G%d" % i, [128, L1S - 128], BF16) for i in range(2)]
    gctr = [0]

    def hy_segment(cc, hT_v, hT_key, ntok, nseq, nblk, nm1, nm2, nout, out_col0):
        ntile = ntok // 128
        slen = nblk * 128
        for t0 in range(0, ntok, 512):
            ld(hTc[:], hT_v[:, :, t0:t0 + 512], [dB[hT_key]], [hTc])
            for o in range(3):
                pb = psum[o % 2]
                for c in range(16):
                    mm(pb.t[:, :], whb[:, c, o, :], hTc[:, c, :], c == 0, c == 15, [whb, hTc], [pb])
                cp(Pb[:, o, t0:t0 + 512], pb.t[:, :], [pb], [Pb], eng="act")
        for t in range(ntile):
            a0 = t * 128
            sfirst = (a0 % slen) == 0
            slast = ((a0 + 128) % slen) == 0
            for o in range(3):
                ts(utf[:, o, :], Pb[:, o, a0:a0 + 128], cw[:, o, 1:2], cw[:, o, 3:4], ALU.mult, ALU.add, [Pb, cw], [utf])
                lo = 1 if sfirst else 0
                stt(utf[:, o, lo:128], Pb[:, o, a0 + lo - 1:a0 + 127], cw[:, o, 0:1], utf[:, o, lo:128], ALU.mult, ALU.add, [Pb, cw, utf], [utf])
                hi = 127 if slast else 128
                stt(utf[:, o, 0:hi], Pb[:, o, a0 + 1:a0 + hi + 1], cw[:, o, 2:3], utf[:, o, 0:hi], ALU.mult, ALU.add, [Pb, cw, utf], [utf])
            cp(ut[:], utf[:], [utf], [ut], eng="act")
            pbk = psum[2 + t % 2]
            for o in range(3):
                tr(psb(2 + t % 2, BF16)[:, o * 128:o * 128 + 128], ut[:, o, :], idb[:], [ut, idb], [pbk])
            cp(Z[:, t, :, :], psb(2 + t % 2, BF16)[:, 0:384].rearrange("p (o c) -> p o c", o=3), [pbk], [Z])
            mm(pbk.t[:, 0:128], jdb[:, :], Z[:, t, 1, :], True, True, [jdb, Z], [pbk])
            cp(Z[:, t, 1, :], pbk.t[:, 0:128], [pbk], [Z], eng="act")
        L1 = ftab[nm1][2]
        ncol1 = ntile
        cpb1 = 512 // ncol1
        for c in range(128):
            g = Gb[gctr[0] % 2]; gctr[0] += 1
            src = bass.AP(tensor=Rt[nm1].tensor, offset=(cc * 128 + c) * L1, ap=[[1, 128], [1, L1 - 128]])
            ld(g[:, 0:L1 - 128], src, [dB["R"]], [g])
            pb = psum[4 + (c // cpb1) % 2]
            cb = (c % cpb1) * ncol1
            das = [0] + [d for d in range(-(nblk - 1), nblk) if d != 0]
            for k, da in enumerate(das):
                f0 = 128 * (nblk - 1 - da)
                lo, hi = max(0, -da), min(nblk, nblk - da)
                if nseq == 1:
                    rhs = Z[:, lo:hi, 0, c]; out = pb.t[:, cb + lo + da:cb + hi + da]
                elif da == 0:
                    rhs = Z[:, 0:ntile, 0, c]; out = pb.t[:, cb:cb + ntile]
                else:
                    rhs = Z[:, lo:ntile:nblk, 0, c]; out = pb.t[:, cb + lo + da:cb + ntile:nblk]
                mm(out, g[:, f0:f0 + 128], rhs, k == 0, k == len(das) - 1, [g, Z], [pb])
            if (c + 1) % cpb1 == 0:
                c0 = c + 1 - cpb1
                tt(z1[:, 0:ntile, c0:c0 + cpb1], pb.t[:, 0:cpb1 * ncol1].rearrange("p (c t) -> p t c", t=ncol1),
                   Z[:, 0:ntile, 1, c0:c0 + cpb1], ALU.mult, [pb, Z], [z1])
        L2 = ftab[nm2][2]
        cpb2 = 512 // nout
        if nseq == 1:
            for i in range(8):
                ts(x2o[:, i, :], Z[:, i, 2, :], mskj[:, 0:1], None, ALU.mult, None, [Z, mskj], [x2o])
                for jj in range(1, 4):
                    stt(x2o[:, i, :], Z[:, 8 * jj + i, 2, :], mskj[:, jj:jj + 1], x2o[:, i, :], ALU.mult, ALU.add, [Z, mskj, x2o], [x2o])
        for c in range(128):
            g = Gb[gctr[0] % 2]; gctr[0] += 1
            src = bass.AP(tensor=Rt[nm2].tensor, offset=(cc * 128 + c) * L2, ap=[[1, 128], [1, L2 - 128]])
            ld(g[:, 0:L2 - 128], src, [dB["R"]], [g])
            pb = psum[6 + (c // cpb2) % 2]
            cb = (c % cpb2) * nout
            if nseq == 1:
                das = [0] + [d for d in range(-(nblk - 1), nout) if d != 0]
            else:
                das = [0] + [d for d in range(-(nblk - 1), nblk) if d != 0]
            for k, da in enumerate(das):
                f0 = 128 * (da + nblk - 1)
                if nseq == 1:
                    ilo, ihi = max(0, da), min(nout, nblk + da)
                    rhs = z1[:, ilo - da:ihi - da, c]; out = pb.t[:, cb + ilo:cb + ihi]
                elif da == 0:
                    rhs = z1[:, 0:ntile, c]; out = pb.t[:, cb:cb + ntile]
                else:
                    lo = max(0, -da)
                    rhs = z1[:, lo:ntile:nblk, c]; out = pb.t[:, cb + lo + da:cb + ntile:nblk]
                mm(out, g[:, f0:f0 + 128], rhs, k == 0, k == len(das) - 1, [g, z1], [pb])
            if (c + 1) % cpb2 == 0:
                c0 = c + 1 - cpb2
                x2src = x2o[:, 0:nout, c0:c0 + cpb2] if nseq == 1 else Z[:, 0:nout, 2, c0:c0 + cpb2]
                tt(z2[:, 0:nout, c0:c0 + cpb2], pb.t[:, 0:cpb2 * nout].rearrange("p (c t) -> p t c", t=nout),
                   x2src, ALU.mult, [pb, Z, x2o], [z2])
        for i in range(nout):
            pbk = psum[2 + i % 2]
            tr(psb(2 + i % 2, BF16)[:, 0:128], z2[:, i, :], idb[:], [z2, idb], [pbk])
            cp(yhT[:, cc, out_col0 + i * 128:out_col0 + i * 128 + 128], psb(2 + i % 2, BF16)[:, 0:128], [pbk], [yhT], eng="act")

    for cc in range(ncc):
        for o in range(3):
            col = o * 1024 + cc * 128
            ld(wst[:], w_in[:, col:col + 128].rearrange("(c p) n -> p c n", p=128), [], [wst])
            cp(whb[:, :, o, :], wst[:], [wst], [whb])
            for tap in range(3):
                ld(cw[:, o, tap:tap + 1], convw[tap, col:col + 128].rearrange("(p o) -> p o", o=1), [], [cw], slow=True)
            ld(cw[:, o, 3:4], convb[col:col + 128].rearrange("(p o) -> p o", o=1), [], [cw], slow=True)
        hy_segment(cc, hTo_v, "hTo", 512, 2, 2, "p1", "p2", 4, 0)
        hy_segment(cc, hTs_v, "hTs", NSEQ, 1, 32, "s1", "s2", 8, 512)
    phase_reset(markA)

    if hy_only:
        ld(yhT_d, yhT[:], [yhT], [dB["dbg"]])
        S.op("sp", lambda e: e.nop(), [dB["dbg"]], [])
        S.finish(st)
        return nc, S, st
    ymT = alloc("ymT", [128, 16, NTOK], BF16)
    markC = ar_pos[0]
    wstgf = alloc("wstgf", [128, 2048])
    wstg = S.bufs[-1]
    wst16 = wstgf.t.rearrange("p (c n) -> p c n", c=16)
    gq = alloc("gq", [128, 8])
    gh = alloc("gh", [128, 4])
    ld(gq[:, 0:4], q_a_w.rearrange("(c p) -> p c", p=128), [], [gq], slow=True)
    ld(gq[:, 4:6], kv_a_w.rearrange("(c p) -> p c", p=128), [], [gq], slow=True)
    ld(gh[:, 0:1], qn_w.rearrange("(p o) -> p o", o=1), [], [gh], slow=True)
    ld(gh[:, 1:2], kn_w.rearrange("(p o) -> p o", o=1), [], [gh], slow=True)
    ld(gh[0:64, 2:3], qr_w.rearrange("(p o) -> p o", o=1), [], [gh], slow=True)
    ld(gh[0:64, 3:4], kr_w.rearrange("(p o) -> p o", o=1), [], [gh], slow=True)
    ckvT = alloc("ckvT", [128, 2, NKEY + 512], BF16)
    kpeT = alloc("kpeT", [64, NKEY + 512], BF16)
    qcT = alloc("qcT", [128, 4, NTOK], BF16)
    sq = alloc("sq", [128, 4, 512], BF16); rstd = alloc("rstd", [128, 512]); tmpf = alloc("tmpf", [128, 2, 512])
    rope_t = alloc("rope_t", [64, 2, 512]); ybf = alloc("ybf", [64, 512], BF16)
    markC2 = ar_pos[0]
    wq = alloc("wq", [128, 16, 512], BF16); wkv = alloc("wkv", [128, 16, 320], BF16)
    for hh in range(4):
        ld(wst16, w_in[:, 3072 + 128 * hh:3200 + 128 * hh].rearrange("(c p) n -> p c n", p=128), [], [wstg])
        cp(wq[:, :, 128 * hh:128 * hh + 128], wst16, [wstg], [wq])
    for hh in range(2):
        ld(wst16, w_in[:, 3584 + 128 * hh:3712 + 128 * hh].rearrange("(c p) n -> p c n", p=128), [], [wstg])
        cp(wkv[:, :, 128 * hh:128 * hh + 128], wst16, [wstg], [wkv])
    ld(wst16[:, :, 0:64], w_in[:, 3840:3904].rearrange("(c p) n -> p c n", p=128), [], [wstg]); cp(wkv[:, :, 256:320], wst16[:, :, 0:64], [wstg], [wkv])

    def fm_norm(pbs, rows, dim, gains, outs, out_bufs, extra_reads=()):
        n = len(pbs)
        for i, pb in enumerate(pbs):
            act(sq[0:rows, i, :], pb.t[0:rows, :], AF.Square, [pb], [sq])
        for i in range(n):
            mm(psum[7].t[0:rows, :], onesb[0:rows, 0:rows], sq[0:rows, i, :], i == 0, i == n - 1, [onesb, sq], [psum[7]])
        ts(rstd[0:rows, :], psum[7].t[0:rows, :], 1.0 / dim, EPS, ALU.mult, ALU.add, [psum[7]], [rstd])
        act(rstd[0:rows, :], rstd[0:rows, :], AF.Sqrt, [rstd], [rstd])
        S.op("dve", lambda e: e.reciprocal(out=rstd[0:rows, :], in_=rstd[0:rows, :]), [rstd], [rstd])
        for i, pb in enumerate(pbs):
            stt(outs[i], pb.t[0:rows, :], gains[i], rstd[0:rows, :], ALU.mult, ALU.mult, [pb, rstd] + list(extra_reads), out_bufs)

    def rope_fm(src_f32, dst_bf, tab_ap, reads, dst_buf):
        ld(rope_t[:], tab_ap.rearrange("a p n -> p a n"), [], [rope_t])
        cp(ybf[:], src_f32, reads, [ybf])
        mm(psum[7].t[0:64, :], protb[:, :], ybf[:, :], True, True, [protb, ybf], [psum[7]])
        tt(src_f32, src_f32, rope_t[:, 0, :], ALU.mult, reads + [rope_t], reads)
        tt(rstd[0:64, :], psum[7].t[0:64, :], rope_t[:, 1, :], ALU.mult, [psum[7], rope_t], [rstd])
        tt(dst_bf, src_f32, rstd[0:64, :], ALU.add, reads + [rstd], [dst_buf])

    def kv_chunk(hT_v, key, t0, kcol0, is_prompt):
        ld(hTc[:], hT_v[:, :, t0:t0 + 512], [dB[key]], [hTc])
        for m in range(2):
            for c in range(16):
                mm(psum[m].t[:, :], wkv[:, c, 128 * m:128 * m + 128], hTc[:, c, :], c == 0, c == 15, [wkv, hTc], [psum[m]])
        for c in range(16):
            mm(psum[2].t[0:64, :], wkv[:, c, 256:320], hTc[:, c, :], c == 0, c == 15, [wkv, hTc], [psum[2]])
        fm_norm([psum[0], psum[1]], 128, 256.0, [gq[:, 4:5], gq[:, 5:6]], [tmpf[:, 0, :], tmpf[:, 1, :]], [tmpf], [gq])
        cp(ckvT[:, :, kcol0:kcol0 + 512], tmpf[:], [tmpf], [ckvT])
        if is_prompt:
            for m in range(2):
                ld(ckv_out[t0:t0 + 512, 128 * m:128 * m + 128].rearrange("t p -> p t"), tmpf[:, m, :], [tmpf], [dB["ckv"]], slow=True)
        fm_norm([psum[2]], 64, 64.0, [gh[0:64, 3:4]], [tmpf[0:64, 0, :]], [tmpf], [gh])
        if is_prompt:
            ld(kpe_out[t0:t0 + 512, :].rearrange("t p -> p t"), tmpf[0:64, 0, :], [tmpf], [dB["kpe"]], slow=True)
            cp(kpeT[:, kcol0:kcol0 + 512], tmpf[0:64, 0, :], [tmpf], [kpeT])
        else:
            rope_fm(tmpf[0:64, 0, :], kpeT[:, kcol0:kcol0 + 512], ropek[:, :, t0:t0 + 512], [tmpf], kpeT)

    hTc2 = hTc
    hTc = alloc("hTcC", [128, 16, 512], BF16)
    for t0 in range(0, NSEQ, 512):
        kv_chunk(hTs_v, "hTs", t0, t0, False)
    kv_chunk(hTo_v, "hTo", 0, NKEY, True)
    cst = alloc("cst", [128, 2, 320]); cstb = alloc("cstb", [128, 2, 320], BF16)
    ld(cst[:, :, 0:256], c_ckv.rearrange("(a p) n -> p a n", p=128), [], [cst])
    ld(cst[:, :, 256:320], c_kpe.rearrange("(a p) n -> p a n", p=128), [], [cst])
    cp(cstb[:], cst[:], [cst], [cstb])
    for a in range(2):
        for m in range(2):
            tr(psb(3, BF16)[:, 0:128], cstb[:, a, 128 * m:128 * m + 128], idb[:], [cstb, idb], [psum[3]])
            cp(ckvT[:, m, NSEQ + a * 128:NSEQ + a * 128 + 128], psb(3, BF16)[:, 0:128], [psum[3]], [ckvT])
        tr(psb(3, BF16)[0:64, 0:128], cstb[:, a, 256:320], idb[:], [cstb, idb], [psum[3]])
        cp(kpeT[:, NSEQ + a * 128:NSEQ + a * 128 + 128], psb(3, BF16)[0:64, 0:128], [psum[3]], [kpeT])
    for tcn in range(3):
        ld(hTc[:], hTo_v[:, :, tcn * 512:tcn * 512 + 512], [dB["hTo"]], [hTc])
        for m in range(4):
            for c in range(16):
                mm(psum[m].t[:, :], wq[:, c, 128 * m:128 * m + 128], hTc[:, c, :], c == 0, c == 15, [wq, hTc], [psum[m]])
        fm_norm([psum[m] for m in range(4)], 128, 512.0, [gq[:, m:m + 1] for m in range(4)],
                [qcT[:, m, tcn * 512:tcn * 512 + 512] for m in range(4)], [qcT], [gq])
    phase_reset(markC2)
    wuqh = alloc("wuqh", [128, 4, 192], BF16); wukvh = alloc("wukvh", [128, 2, 256], BF16)
    knT = alloc("knT", [128, NKEY + 512], BF16); Vt = alloc("Vt", [128, 38, 128], BF16)
    qnT = alloc("qnT", [128, 512], BF16); qpT = alloc("qpT", [64, 512], BF16); qpf = alloc("qpf", [64, 512])
    PT = [alloc("PT%d" % i, [128, 512], BF16) for i in range(2)]
    SCALE = 192.0 ** -0.5
    for h in range(16):
        vq = wstgf.t[:, 0:768].rearrange("p (c n) -> p c n", c=4)
        vk = wstgf.t[:, 1024:1536].rearrange("p (c n) -> p c n", c=2)
        ld(vq, w_uq[:, 192 * h:192 * h + 192].rearrange("(c p) n -> p c n", p=128), [], [wstg])
        cp(wuqh[:], vq, [wstg], [wuqh])
        ld(vk, w_ukv[:, 256 * h:256 * h + 256].rearrange("(c p) n -> p c n", p=128), [], [wstg])
        cp(wukvh[:], vk, [wstg], [wukvh])
        nkt = (NKEY + 512) // 128
        for k0 in range(0, NKEY + 512, 512):
            w = min(512, NKEY + 512 - k0)
            for kc in range(2):
                mm(psum[0].t[:, 0:w], wukvh[:, kc, 0:128], ckvT[:, kc, k0:k0 + w], kc == 0, kc == 1, [wukvh, ckvT], [psum[0]])
            act(sq[:, 0, 0:w], psum[0].t[:, 0:w], AF.Square, [psum[0]], [sq])
            mm(psum[7].t[:, 0:w], onesb[:, :], sq[:, 0, 0:w], True, True, [onesb, sq], [psum[7]])
            ts(rstd[:, 0:w], psum[7].t[:, 0:w], 1.0 / 128, EPS, ALU.mult, ALU.add, [psum[7]], [rstd])
            act(rstd[:, 0:w], rstd[:, 0:w], AF.Sqrt, [rstd], [rstd])
            S.op("dve", lambda e, w=w: e.reciprocal(out=rstd[:, 0:w], in_=rstd[:, 0:w]), [rstd], [rstd])
            stt(knT[:, k0:k0 + w], psum[0].t[:, 0:w], gh[:, 1:2], rstd[:, 0:w], ALU.mult, ALU.mult, [psum[0], rstd, gh], [knT])
        for kt in range(nkt):
            pb = psum[1 + kt % 2]
            for kc in range(2):
                mm(pb.t[:, 0:128], ckvT[:, kc, kt * 128:kt * 128 + 128], wukvh[:, kc, 128:256], kc == 0, kc == 1, [ckvT, wukvh], [pb])
            cp(Vt[:, kt, :], pb.t[:, 0:128], [pb], [Vt], eng="act")
        groups = [(0, 256, [34, 35], None), (256, 256, [36, 37], None),
                  (512, 512, list(range(34)), 0), (1024, 512, list(range(34)), 512)]
        for (q0, nq, kts, rp) in groups:
            for kc in range(4):
                mm(psum[0].t[:, 0:nq], wuqh[:, kc, 0:128], qcT[:, kc, q0:q0 + nq], kc == 0, kc == 3, [wuqh, qcT], [psum[0]])
            for kc in range(4):
                mm(psum[3].t[0:64, 0:nq], wuqh[:, kc, 128:192], qcT[:, kc, q0:q0 + nq], kc == 0, kc == 3, [wuqh, qcT], [psum[3]])
            act(sq[:, 0, 0:nq], psum[0].t[:, 0:nq], AF.Square, [psum[0]], [sq])
            mm(psum[7].t[:, 0:nq], onesb[:, :], sq[:, 0, 0:nq], True, True, [onesb, sq], [psum[7]])
            ts(rstd[:, 0:nq], psum[7].t[:, 0:nq], 1.0 / 128, EPS, ALU.mult, ALU.add, [psum[7]], [rstd])
            act(rstd[:, 0:nq], rstd[:, 0:nq], AF.Sqrt, [rstd], [rstd])
            S.op("dve", lambda e, nq=nq: e.reciprocal(out=rstd[:, 0:nq], in_=rstd[:, 0:nq]), [rstd], [rstd])
            stt(qnT[:, 0:nq], psum[0].t[:, 0:nq], gh[:, 0:1], rstd[:, 0:nq], ALU.mult, ALU.mult, [psum[0], rstd, gh], [qnT])
            act(sq[0:64, 1, 0:nq], psum[3].t[0:64, 0:nq], AF.Square, [psum[3]], [sq])
            mm(psum[7].t[0:64, 0:nq], onesb[0:64, 0:64], sq[0:64, 1, 0:nq], True, True, [onesb, sq], [psum[7]])
            ts(rstd[0:64, 0:nq], psum[7].t[0:64, 0:nq], 1.0 / 64, EPS, ALU.mult, ALU.add, [psum[7]], [rstd])
            act(rstd[0:64, 0:nq], rstd[0:64, 0:nq], AF.Sqrt, [rstd], [rstd])
            S.op("dve", lambda e, nq=nq: e.reciprocal(out=rstd[0:64, 0:nq], in_=rstd[0:64, 0:nq]), [rstd], [rstd])
            stt(qpf[:, 0:nq], psum[3].t[0:64, 0:nq], gh[0:64, 2:3], rstd[0:64, 0:nq], ALU.mult, ALU.mult, [psum[3], rstd, gh], [qpf])
            if rp is None:
                cp(qpT[:, 0:nq], qpf[:, 0:nq], [qpf], [qpT])
            else:
                rope_fm(qpf[:, :], qpT[:, :], ropeq[:, :, rp:rp + 512], [qpf], qpT)
            for i, kt in enumerate(kts):
                pbs_ = psum[1 + i % 2]
                mm(pbs_.t[:, 0:nq], knT[:, kt * 128:kt * 128 + 128], qnT[:, 0:nq], True, False, [knT, qnT], [pbs_])
                mm(pbs_.t[:, 0:nq], kpeT[:, kt * 128:kt * 128 + 128], qpT[:, 0:nq], False, True, [kpeT, qpT], [pbs_])
                pt = PT[i % 2]
                act(pt[:, 0:nq], pbs_.t[:, 0:nq], AF.Exp, [pbs_], [pt], scale=SCALE, bias=-8.0)
                mm(psum[4].t[:, 0:nq], Vt[:, kt, :], pt[:, 0:nq], i == 0, i == len(kts) - 1, [Vt, pt], [psum[4]])
                mm(psum[5].t[:, 0:nq], onesb[:, :], pt[:, 0:nq], i == 0, i == len(kts) - 1, [onesb, pt], [psum[5]])
            S.op("dve", lambda e, nq=nq: e.reciprocal(out=rstd[:, 0:nq], in_=psum[5].t[:, 0:nq]), [psum[5]], [rstd])
            tt(ymT[:, h, q0:q0 + nq], psum[4].t[:, 0:nq], rstd[:, 0:nq], ALU.mult, [psum[4], rstd], [ymT])
    if dbg:
        ld(yhT_d, yhT[:], [yhT], [dB["dbg"]]); ld(ymT_d, ymT[:], [ymT], [dB["dbg"]])
    phase_reset(markC)

    markD = ar_pos[0]
    wsD = alloc("wsD", [128, 2048]); wsDb = S.bufs[-1]
    wsD16 = wsD.t.rearrange("p (c n) -> p c n", c=16)
    wgh = alloc("wgh", [128, 16, 128], BF16); wgm = alloc("wgm", [128, 16, 128], BF16)
    why = alloc("why", [128, 8, 128], BF16); wml = alloc("wml", [128, 16, 128], BF16)
    hTd = alloc("hTd", [128, 16, 512], BF16); mgT = alloc("mgT", [128, 16, 512], BF16)
    sg1 = alloc("sg1", [128, 512]); sg2 = alloc("sg2", [128, 512])
    wob = alloc("wob", [128, 16, 256], BF16)
    g1b = alloc("g1b", [128, 2, D])
    xres = alloc("xres", [128, 256]); x1t = alloc("x1t", [128, 256])
    for i, r in enumerate((0, 3)):
        ld(g1b[:, i, :], modD[r, 2 * D:3 * D].partition_broadcast(128), [dB["modD"]], [g1b])
    for tcn in range(3):
        gi = 0 if tcn == 0 else 1
        ld(hTd[:], hTo_v[:, :, tcn * 512:tcn * 512 + 512], [dB["hTo"]], [hTd])
        for m in range(16):
            for (dst, col0, nk, src) in ((wgh, 3904 + 128 * m, 16, w_in), (wgm, 5952 + 128 * m, 16, w_in),
                                         (why, 128 * m, 8, w_hy_out), (wml, 128 * m, 16, w_mla_out)):
                ld(wsD16[:, 0:nk, :], src[:, col0:col0 + 128].rearrange("(c p) n -> p c n", p=128), [], [wsDb])
                cp(dst[:], wsD16[:, 0:nk, :], [wsDb], [dst], eng="pool")
            for c in range(16):
                mm(psum[0].t[:, :], wgh[:, c, :], hTd[:, c, :], c == 0, c == 15, [wgh, hTd], [psum[0]])
            for c in range(16):
                mm(psum[1].t[:, :], wgm[:, c, :], hTd[:, c, :], c == 0, c == 15, [wgm, hTd], [psum[1]])
            for c in range(8):
                mm(psum[2].t[:, :], why[:, c, :], yhT[:, c, tcn * 512:tcn * 512 + 512], c == 0, c == 7, [why, yhT], [psum[2]])
            for c in range(16):
                mm(psum[3].t[:, :], wml[:, c, :], ymT[:, c, tcn * 512:tcn * 512 + 512], c == 0, c == 15, [wml, ymT], [psum[3]])
            act(sg1[:], psum[0].t[:, :], AF.Sigmoid, [psum[0]], [sg1])
            act(sg2[:], psum[1].t[:, :], AF.Sigmoid, [psum[1]], [sg2])
            tt(sg1[:], sg1[:], psum[2].t[:, :], ALU.mult, [sg1, psum[2]], [sg1])
            tt(sg2[:], sg2[:], psum[3].t[:, :], ALU.mult, [sg2, psum[3]], [sg2])
            tt(mgT[:, m, :], sg1[:], sg2[:], ALU.add, [sg1, sg2], [mgT])
        for c8 in range(8):
            ld(wsD16[:, :, :], w_o[:, 256 * c8:256 * c8 + 128].rearrange("(c p) n -> p c n", p=128), [], [wsDb])
            cp(wob[:, :, 0:128], wsD16[:, :, :], [wsDb], [wob], eng="pool")
            ld(wsD16[:, :, :], w_o[:, 256 * c8 + 128:256 * c8 + 256].rearrange("(c p) n -> p c n", p=128), [], [wsDb])
            cp(wob[:, :, 128:256], wsD16[:, :, :], [wsDb], [wob], eng="pool")
            for t4 in range(4):
                row0 = tcn * 512 + t4 * 128
                pb = psum[4 + t4 % 2]
                for c in range(16):
                    mm(pb.t[:, 0:256], mgT[:, c, t4 * 128:t4 * 128 + 128], wob[:, c, :], c == 0, c == 15, [mgT, wob], [pb])
                ld(xres[:], x_own[row0:row0 + 128, 256 * c8:256 * c8 + 256], [], [xres])
                tt(x1t[:], pb.t[:, 0:256], g1b[:, gi, 256 * c8:256 * c8 + 256], ALU.mult, [pb, g1b], [x1t])
                tt(x1t[:], x1t[:], xres[:], ALU.add, [x1t, xres], [x1t])
                ld(x1d[row0:row0 + 128, 256 * c8:256 * c8 + 256], x1t[:], [x1t], [dB["x1d"]])
    phase_reset(markA)

    h2T = alloc("h2T", [128, 16, NTOK], BF16)
    wts = alloc("wts", [128, 12, NE])
    markE = ar_pos[0]
    xt = alloc("xt2", [128, D]); xs = alloc("xs2", [128, D], BF16); junk = alloc("junk2", [128, D], BF16)
    ssq = alloc("ssq2", [128, 2]); hTt = alloc("hTt2", [128, 16, 128], BF16)
    wrs = alloc("wrs", [128, 16, NE]); wrb = alloc("wrb", [128, 16, NE], BF16); brb = alloc("brb", [128, NE])
    lg = alloc("lg", [128, NE]); mx8 = alloc("mx8", [128, 8]); msk = alloc("msk", [128, NE]); sm = alloc("sm", [128, 2])
    bdn = alloc("bdn", [NE, D]); wtT = alloc("wtT", [NE, 128]); ybi = alloc("ybi", [128, D])
    ld(wrs[:], w_router.rearrange("(c p) n -> p c n", p=128), [], [wrs]); cp(wrb[:], wrs[:], [wrs], [wrb])
    ld(brb[:], b_router.partition_broadcast(128), [], [brb])
    ld(bdn[:], b_dn, [], [bdn])
    for t in range(12):
        r = 0 if t < 4 else 3
        norm_T(x1d[t * 128:t * 128 + 128, :], [dB["x1d"]], r, gw2, 48,
               lambda t=t: cp(h2T[:, :, t * 128:t * 128 + 128], hTt[:], [hTt], [h2T], eng="pool"))
        for c in range(16):
            mm(psum[0].t[:, 0:NE], hTt[:, c, :], wrb[:, c, :], c == 0, c == 15, [hTt, wrb], [psum[0]])
        tt(lg[:], psum[0].t[:, 0:NE], brb[:], ALU.add, [psum[0], brb], [lg])
        S.op("dve", lambda e: e.max(out=mx8[:], in_=lg[:]), [lg], [mx8])
        ts(msk[:], lg[:], mx8[:, 3:4], None, ALU.is_ge, None, [lg, mx8], [msk])
        ts(sm[:, 1:2], mx8[:, 0:1], -1.0, None, ALU.mult, None, [mx8], [sm])
        act(lg[:], lg[:], AF.Exp, [lg, sm], [lg], bias=sm[:, 1:2])
        tt(lg[:], lg[:], msk[:], ALU.mult, [lg, msk], [lg])
        S.op("dve", lambda e: e.reduce_sum(out=sm[:, 0:1], in_=lg[:], axis=AX.X), [lg], [sm])
        S.op("dve", lambda e: e.reciprocal(out=sm[:, 0:1], in_=sm[:, 0:1]), [sm], [sm])
        ts(wts[:, t, :], lg[:], sm[:, 0:1], None, ALU.mult, None, [lg, sm], [wts])
        tr(psum[1].t[0:NE, 0:128], wts[:, t, :], idf[:], [wts, idf], [psum[1]])
        cp(wtT[:], psum[1].t[0:NE, 0:128], [psum[1]], [wtT])
        for c4 in range(4):
            mm(psum[2 + c4 % 2].t[:, :], wtT[:, :], bdn[:, 512 * c4:512 * c4 + 512], True, True, [wtT, bdn], [psum[2 + c4 % 2]])
            cp(ybi[:, 512 * c4:512 * c4 + 512], psum[2 + c4 % 2].t[:, :], [psum[2 + c4 % 2]], [ybi], eng="act")
        ld(yacc[t * 128:t * 128 + 128, :], ybi[:], [ybi], [dB["yacc"]])
    if dbg:
        ld(wts_d, wts[:], [wts], [dB["dbg"]]); ld(h2T_d, h2T[:], [h2T], [dB["dbg"]])
    phase_reset(markE)

    hid = alloc("hid", [128, 16, NTOK], BF16)
    wsE = alloc("wsE", [128, 16, 256]); wgb = alloc("wgb", [128, 16, 2, 128], BF16); wdb = alloc("wdb", [128, 16, 256], BF16)
    bgT = alloc("bgT", [128, 2])
    gc = alloc("gc", [128, 512]); sgm = alloc("sgm", [128, 512]); uc = alloc("uc", [128, 512])
    osb = [alloc("osb%d" % i, [128, 256]) for i in range(2)]
    octr = 0
    for e_ in range(0 if skip_moe else NE):
        for j in range(16):
            ld(wsE[:], w_gu[e_, :, 256 * j:256 * j + 256].rearrange("(c p) n -> p c n", p=128), [], [wsE])
            cp(wgb[:], wsE[:].rearrange("p c (f two) -> p c two f", two=2), [wsE], [wgb], eng="pool")
            ld(bgT[:], b_gu[e_, 256 * j:256 * j + 256].rearrange("(f two) -> f two", two=2), [], [bgT], slow=True)
            for tcn in range(3):
                cols = slice(tcn * 512, tcn * 512 + 512)
                for c in range(16):
                    mm(psum[0].t[:, :], wgb[:, c, 0, :], h2T[:, c, cols], c == 0, c == 15, [wgb, h2T], [psum[0]])
                for c in range(16):
                    mm(psum[1].t[:, :], wgb[:, c, 1, :], h2T[:, c, cols], c == 0, c == 15, [wgb, h2T], [psum[1]])
                ts(gc[:], psum[0].t[:, :], bgT[:, 0:1], 7.0, ALU.add, ALU.min, [psum[0], bgT], [gc])
                act(sgm[:], gc[:], AF.Sigmoid, [gc], [sgm], scale=1.702)
                ts(uc[:], psum[1].t[:, :], bgT[:, 1:2], 7.0, ALU.add, ALU.min, [psum[1], bgT], [uc])
                ts(uc[:], uc[:], -7.0, 1.0, ALU.max, ALU.add, [uc], [uc], eng="pool")
                tt(gc[:], gc[:], sgm[:], ALU.mult, [gc, sgm], [gc])
                tt(hid[:, j, cols], gc[:], uc[:], ALU.mult, [gc, uc], [hid], eng="pool")
        for c8 in range(8):
            ld(wsE[:], w_dn[e_, :, 256 * c8:256 * c8 + 256].rearrange("(j p) n -> p j n", p=128), [], [wsE])
            cp(wdb[:], wsE[:], [wsE], [wdb], eng="pool")
            for t in range(12):
                pb = psum[2 + t % 4]
                for j in range(16):
                    mm(pb.t[:, 0:256], hid[:, j, t * 128:t * 128 + 128], wdb[:, j, :], j == 0, j == 15, [hid, wdb], [pb])
                ob = osb[octr % 2]; octr += 1
                ts(ob[:], pb.t[:, 0:256], wts[:, t, e_:e_ + 1], None, ALU.mult, None, [pb, wts], [ob])
                S.dma("pool", lambda e, ob=ob, t=t, c8=c8: e.dma_start(out=yacc[t * 128:t * 128 + 128, 256 * c8:256 * c8 + 256], in_=ob[:], accum_op=ALU.add),
                      [ob], [dB["yacc"]])
    phase_reset(markE)
    g2b = alloc("g2b", [128, 2, D]); ya = alloc("ya", [128, D]); xf = alloc("xf", [128, D])
    for i, r in enumerate((0, 3)):
        ld(g2b[:, i, :], modD[r, 5 * D:6 * D].partition_broadcast(128), [dB["modD"]], [g2b])
    for t in range(12):
        gi = 0 if t < 4 else 1
        ld(ya[:], yacc[t * 128:t * 128 + 128, :], [dB["yacc"]], [ya])
        ld(xf[:], x1d[t * 128:t * 128 + 128, :], [dB["x1d"]], [xf])
        tt(ya[:], ya[:], g2b[:, gi, :], ALU.mult, [ya, g2b], [ya])
        tt(ya[:], ya[:], xf[:], ALU.add, [ya, xf], [ya])
        ld(y_out[t * 128:t * 128 + 128, :], ya[:], [ya], [dB["out"]])
    S.op("sp", lambda e: e.nop(), [dB["out"], dB["ckv"], dB["kpe"], dB["dbg"]], [])
    S.finish(st)
    return nc, S, st


def _feat_table(lags, n):
    lags = np.asarray(lags, np.int64)
    valid = (np.abs(lags) <= n - 1)
    pos = np.where(lags >= 0, lags, -lags - 1)
    pos = np.clip(pos, 0, n - 1)
    t = np.linspace(0.0, 1.0, n, dtype=np.float32)
    w = (np.float32(2.0 * math.pi / n) * np.arange(n, dtype=np.float32)).astype(np.float32)
    bands = np.linspace(1e-4, 15.0, 16, dtype=np.float32)
    arg = (bands[None, :] * w[:, None]).astype(np.float32)
    feats = np.concatenate([t[:, None], np.cos(arg), -np.sin(arg)], axis=-1).astype(np.float32)
    f = np.ascontiguousarray(feats[pos].T)
    aux = np.stack([t[pos], (lags >= 0).astype(np.float32), valid.astype(np.float32),
                    (lags == 0).astype(np.float32)]).astype(np.float32)
    return f, np.ascontiguousarray(aux)


def _rope_tables(n_tokens):
    rows = n_tokens // 64
    row = np.repeat(np.arange(rows, dtype=np.float32), 64)
    col = np.tile(np.arange(64, dtype=np.float32), rows)
    n_freq = 16
    inv_freq = np.power(np.float32(10000.0), -np.arange(n_freq, dtype=np.float32) / n_freq).astype(np.float32)
    ang = np.concatenate([row[:, None] * inv_freq, col[:, None] * inv_freq], axis=-1)
    ang = np.concatenate([ang, ang], axis=-1).astype(np.float32)
    return np.cos(ang).astype(np.float32), np.sin(ang).astype(np.float32)


def kernel(x_prompt, x_sample, cache_ckv, cache_kpe, c, c_ctx, w_mod, b_mod, norm1_w, norm2_w,
           w_in, hy_conv_w, hy_conv_b, filt_w1, filt_b1, filt_w2, filt_b2, filt_w3, filt_b3,
           filt_freq, hy_skip, q_a_norm_w, w_uq, kv_a_norm_w, w_ukv, qn_norm_w, kn_norm_w,
           qr_norm_w, kr_norm_w, w_hy_out, w_mla_out, w_o, w_router, b_router, w_gate_up,
           b_gate_up, w_down, b_down):
    A = lambda a: np.ascontiguousarray(np.asarray(a, dtype=np.float32))
    x_prompt = A(x_prompt); x_sample = A(x_sample); cache_ckv = A(cache_ckv); cache_kpe = A(cache_kpe)
    c = A(c); c_ctx = A(c_ctx)
    dbg = bool(_NC_CACHE.get("dbg", False))
    if "nc" not in _NC_CACHE:
        _NC_CACHE["nc"] = build(dbg=dbg, skip_moe=dbg, **_NC_CACHE.get("bkw", {}))[0]
    nc = _NC_CACHE["nc"]
    ident = np.eye(128, dtype=np.float32)
    jdent = np.ascontiguousarray(ident[::-1])
    prot = np.zeros((64, 64), np.float32)
    for m in range(32):
        prot[m + 32, m] = -1.0
    for m in range(32, 64):
        prot[m - 32, m] = 1.0
    max_decay = math.log(1e-2) / 0.3
    min_decay = math.log(1e-2) / 1.5
    absdel = np.abs(np.linspace(min_decay, max_decay, 1024, dtype=np.float32)).astype(np.float32)
    cosr, sinr = _rope_tables(4096)
    ropek = np.ascontiguousarray(np.stack([cosr.T, sinr.T]))
    f_s1, a_s1 = _feat_table(4095 - np.arange(L1S), 4096)
    f_p1, a_p1 = _feat_table(255 - np.arange(LP), 256)
    f_p2, a_p2 = _feat_table(np.arange(LP) - 255, 256)
    shared = {
        "w_mod": A(w_mod[0]), "b_mod": A(b_mod[0]), "norm1_w": A(norm1_w[0]), "norm2_w": A(norm2_w[0]),
        "w_in": A(w_in[0]), "convw": A(hy_conv_w[0]), "convb": A(hy_conv_b[0]),
        "fw1": A(filt_w1[0]), "fb1": A(filt_b1[0]), "fw2": A(filt_w2[0]), "fb2": A(filt_b2[0]),
        "fw3": A(filt_w3[0]), "fb3": A(filt_b3[0]), "ffreq": A(filt_freq[0]), "skipw": A(hy_skip[0]),
        "q_a_w": A(q_a_norm_w[0]), "w_uq": A(w_uq[0]), "kv_a_w": A(kv_a_norm_w[0]), "w_ukv": A(w_ukv[0]),
        "qn_w": A(qn_norm_w[0]), "kn_w": A(kn_norm_w[0]), "qr_w": A(qr_norm_w[0]), "kr_w": A(kr_norm_w[0]),
        "w_hy_out": A(w_hy_out[0]), "w_mla_out": A(w_mla_out[0]), "w_o": A(w_o[0]),
        "w_router": A(w_router[0]), "b_router": A(b_router[0]),
        "w_gu": A(w_gate_up[0]), "b_gu": A(b_gate_up[0]), "w_dn": A(w_down[0]), "b_dn": A(b_down[0]),
        "ident": ident, "jdent": jdent, "prot": prot, "absdel": absdel, "ropek": ropek,
        "feat_s1": f_s1, "aux_s1": a_s1, "feat_p1": f_p1, "aux_p1": a_p1, "feat_p2": f_p2, "aux_p2": a_p2,
    }
    in_maps = []
    for k in range(8):
        b, j = k // 4, k % 4
        f_s2, a_s2 = _feat_table(np.arange(L2S) + 1024 * j - 4095, 4096)
        own1h = np.zeros(4, np.float32); own1h[j] = 1.0
        m = dict(shared)
        m.update({
            "x_own": np.ascontiguousarray(np.concatenate([x_prompt[2 * k], x_prompt[2 * k + 1],
                                                          x_sample[b, 1024 * j:1024 * j + 1024]], axis=0)),
            "x_seq": np.ascontiguousarray(x_sample[b]),
            "cvec": np.ascontiguousarray(np.stack([c_ctx, c[0], c[1], c[b]])),
            "c_ckv": np.ascontiguousarray(cache_ckv[b, 0]), "c_kpe": np.ascontiguousarray(cache_kpe[b, 0]),
            "feat_s2": f_s2, "aux_s2": a_s2, "own1h": own1h,
            "ropeq": np.ascontiguousarray(ropek[:, :, 1024 * j:1024 * j + 1024]),
        })
        in_maps.append(m)
    if dbg:
        in_maps = [{k2: v for k2, v in m.items() if k2 in _NC_CACHE["in_names"]} for m in in_maps]
    res = run_bass_kernel_spmd(nc, in_maps, core_ids=list(range(8)))
    if dbg:
        _NC_CACHE["res"] = res
        return None
    y_p = np.zeros((16, 256, D), np.float32); y_s = np.zeros((2, 4096, D), np.float32)
    n_ckv = np.zeros((16, 1, 256, 256), np.float32); n_kpe = np.zeros((16, 1, 256, 64), np.float32)
    for k in range(8):
        b, j = k // 4, k % 4
        r = res.results[k]
        yo = np.asarray(r["y_out"], np.float32)
        y_p[2 * k] = yo[0:256]; y_p[2 * k + 1] = yo[256:512]
        y_s[b, 1024 * j:1024 * j + 1024] = yo[512:1536]
        ck = np.asarray(r["ckv_out"], np.float32); kp = np.asarray(r["kpe_out"], np.float32)
        n_ckv[2 * k, 0] = ck[0:256]; n_ckv[2 * k + 1, 0] = ck[256:512]
        n_kpe[2 * k, 0] = kp[0:256]; n_kpe[2 * k + 1, 0] = kp[256:512]
    return (y_p, y_s, n_ckv, n_kpe)
```

```python
import contextlib
import math
import numpy as np
import concourse.bass as bass
import concourse.mybir as mybir
from concourse.bass_utils import run_bass_kernel_spmd

F32 = mybir.dt.float32
BF16 = mybir.dt.bfloat16
ALU = mybir.AluOpType
AF = mybir.ActivationFunctionType
AX = mybir.AxisListType
ENGS = ("pe", "act", "dve", "pool", "sp")

D = 2048
NE = 32
EPS = 1e-6
_NC_CACHE = {}


class Buf:
    __slots__ = ("name", "t", "w", "r", "dsem", "dcnt")

    def __init__(self, name, t=None):
        self.name = name
        self.t = t
        self.w = None
        self.r = []
        self.dsem = None
        self.dcnt = 0

    def __getitem__(self, idx):
        return self.t[idx]


class Op:
    __slots__ = ("eng", "fn", "reads", "writes", "dma", "idx", "waits", "mark", "dtok", "key")

    def __init__(self, eng, fn, reads, writes, dma=False, key=None):
        self.eng, self.fn, self.reads, self.writes, self.dma, self.key = eng, fn, reads, writes, dma, key
        self.waits = []
        self.mark = False
        self.dtok = None


class Sched:
    def __init__(self, nc):
        self.nc = nc
        self.ops = []
        self.bufs = []

    def buf(self, name, t=None):
        b = Buf(name, t)
        self.bufs.append(b)
        return b

    def op(self, eng, fn, reads=(), writes=()):
        self.ops.append(Op(eng, fn, tuple(reads), tuple(writes)))

    def dma(self, eng, fn, reads=(), writes=(), key=None):
        self.ops.append(Op(eng, fn, tuple(reads), tuple(writes), dma=True, key=key))

    def barrier(self):
        bb = Buf("bar%d" % len(self.ops))
        allb = list(self.bufs)
        self.ops.append(Op("sp", lambda e: e.nop(), (), tuple(allb) + (bb,)))
        for e in ("pe", "act", "dve", "pool"):
            self.ops.append(Op(e, lambda en: en.nop(), (bb,), ()))

    def finish(self, stack):
        nc = self.nc
        per_eng = {e: [] for e in ENGS}
        for o in self.ops:
            o.idx = len(per_eng[o.eng])
            per_eng[o.eng].append(o)
        esem = {e: stack.enter_context(nc.semaphore("s_" + e)) for e in ENGS}
        waited_e = {e: {f: -1 for f in ENGS} for e in ENGS}
        waited_d = {e: {} for e in ENGS}
        sem_pool = []
        for o in self.ops:
            deps = []
            for b in o.reads:
                if b.w is not None:
                    deps.append(b.w)
            for b in o.writes:
                if b.w is not None:
                    deps.append(b.w)
                deps.extend(b.r)
            if o.dma:
                k = o.key if o.key is not None else (o.writes[0] if o.writes else o.reads[0])
                if k.dsem is None:
                    k.dsem = stack.enter_context(nc.semaphore("d%d" % len(sem_pool)))
                    sem_pool.append(k.dsem)
                k.dcnt += 16
                tok = ("d", k, k.dcnt)
                o.dtok = tok
            else:
                tok = ("e", o.eng, o.idx)
            for t in deps:
                if t[0] == "e":
                    _, pe_, pidx = t
                    if pe_ == o.eng and pe_ == "pe" and not o.dma:
                        continue
                    if waited_e[o.eng][pe_] >= pidx:
                        continue
                    waited_e[o.eng][pe_] = pidx
                    o.waits.append(t)
                    per_eng[pe_][pidx].mark = True
                else:
                    _, k, val = t
                    if waited_d[o.eng].get(id(k), 0) >= val:
                        continue
                    waited_d[o.eng][id(k)] = val
                    o.waits.append(t)
            for b in o.reads:
                b.r.append(tok)
            for b in o.writes:
                b.w = tok
                b.r = []
        sig = {e: [] for e in ENGS}
        for e in ENGS:
            c = 0
            for o in per_eng[e]:
                if o.mark and not o.dma:
                    c += 1
                sig[e].append(c)
        self.nsem = len(sem_pool)
        self.nops = {e: len(per_eng[e]) for e in ENGS}
        with nc.Block() as block:
            def make(e):
                def body(engine):
                    for o in per_eng[e]:
                        for t in o.waits:
                            if t[0] == "e":
                                engine.wait_ge(esem[t[1]], sig[t[1]][t[2]])
                            else:
                                engine.wait_ge(t[1].dsem, t[2])
                        ins = o.fn(engine)
                        if o.dma:
                            ins.then_inc(o.dtok[1].dsem, 16)
                        elif o.mark:
                            ins.then_inc(esem[e], 1)
                return body
            block.tensor(make("pe"))
            block.scalar(make("act"))
            block.vector(make("dve"))
            block.gpsimd(make("pool"))
            block.sync(make("sp"))


NTOK = 1536
NSEQ = 4096
NKEY = 4352
L1S, L2S, LP = 8192, 5120, 512
HY = 1024


def build(dbg=False, skip_moe=False, ncc=8, hy_only=False):
    nc = bass.Bass("TRN2", target_bir_lowering=False)
    st = contextlib.ExitStack()
    S = Sched(nc)

    in_names = []
    _NC_CACHE["in_names"] = in_names

    def din(name, shape, dt=F32):
        in_names.append(name)
        return nc.dram_tensor(name, list(shape), dt, kind="ExternalInput").ap()

    def dout(name, shape, dt=F32):
        return nc.dram_tensor(name, list(shape), dt, kind="ExternalOutput").ap()

    def dscr(name, shape, dt):
        return nc.dram_tensor(name, list(shape), dt, kind=("ExternalOutput" if dbg else "Internal")).ap()

    x_own = din("x_own", [NTOK, D]); x_seq = din("x_seq", [NSEQ, D])
    cvec = din("cvec", [4, D]); w_mod = din("w_mod", [D, 6 * D]); b_mod = din("b_mod", [6 * D])
    norm1_w = din("norm1_w", [D]); norm2_w = din("norm2_w", [D])
    w_in = din("w_in", [D, 8000])
    convw = din("convw", [3, 3072]); convb = din("convb", [3072])
    fw1 = din("fw1", [33, 64]); fb1 = din("fb1", [64]); fw2 = din("fw2", [64, 64]); fb2 = din("fb2", [64])
    fw3 = din("fw3", [64, 4096]); fb3 = din("fb3", [4096]); ffreq = din("ffreq", [2, 64]); skipw = din("skipw", [2, HY])
    q_a_w = din("q_a_w", [512]); w_uq = din("w_uq", [512, 3072]); kv_a_w = din("kv_a_w", [256]); w_ukv = din("w_ukv", [256, 4096])
    qn_w = din("qn_w", [128]); kn_w = din("kn_w", [128]); qr_w = din("qr_w", [64]); kr_w = din("kr_w", [64])
    w_hy_out = din("w_hy_out", [HY, D]); w_mla_out = din("w_mla_out", [D, D]); w_o = din("w_o", [D, D])
    w_router = din("w_router", [D, NE]); b_router = din("b_router", [NE])
    b_gu = din("b_gu", [NE, 2 * D]); b_dn = din("b_dn", [NE, D])
    if not skip_moe:
        w_gu = din("w_gu", [NE, D, 2 * D]); w_dn = din("w_dn", [NE, D, D])
    c_ckv = din("c_ckv", [256, 256]); c_kpe = din("c_kpe", [256, 64])
    ident = din("ident", [128, 128]); jdent = din("jdent", [128, 128]); prot = din("prot", [64, 64])
    ftab = {}
    for nm, L in (("s1", L1S), ("s2", L2S), ("p1", LP), ("p2", LP)):
        ftab[nm] = (din("feat_" + nm, [33, L]), din("aux_" + nm, [4, L]), L)
    absdel = din("absdel", [HY])
    ropek = din("ropek", [2, 64, NSEQ])
    ropeq = din("ropeq", [2, 64, 1024])
    y_out = dout("y_out", [NTOK, D]); ckv_out = dout("ckv_out", [512, 256]); kpe_out = dout("kpe_out", [512, 64])
    modD = dscr("modD", [4, 6 * D], F32)
    hTs = dscr("hTs", [D, NSEQ], BF16); hTo = dscr("hTo", [D, NTOK], BF16)
    Rt = {nm: dscr("R_" + nm, [HY, ftab[nm][2]], BF16) for nm in ftab}
    yacc = dscr("yacc", [NTOK, D], F32)
    x1d = dscr("x1d", [NTOK, D], F32)
    if dbg:
        yhT_d = dout("yhT_d", [128, 8, NTOK], BF16); ymT_d = dout("ymT_d", [128, 16, NTOK], BF16)
        wts_d = dout("wts_d", [128, 12, NE]); h2T_d = dout("h2T_d", [128, 16, NTOK], BF16)

    ARW = 49000
    arena = st.enter_context(nc.sbuf_tensor("arena", [128, ARW], F32))
    ar_pos = [0]

    def alloc(name, shape, dt=F32):
        free = int(np.prod(shape[1:]))
        words = free if dt == F32 else (free + 1) // 2
        words = (words + 7) // 8 * 8
        o = ar_pos[0]
        ar_pos[0] += words
        assert ar_pos[0] <= ARW, (name, ar_pos[0])
        ap = arena[0:shape[0], o:o + words]
        if dt != F32:
            ap = ap.bitcast(dt)[:, 0:free]
        else:
            ap = ap[:, 0:free]
        if len(shape) > 2:
            names = " ".join("d%d" % i for i in range(len(shape) - 1))
            kw = {"d%d" % i: shape[i + 1] for i in range(len(shape) - 1)}
            ap = ap.rearrange("p (%s) -> p %s" % (names, names), **kw)
        return S.buf(name, ap)

    def phase_reset(mark):
        S.barrier()
        ar_pos[0] = mark

    psum = [S.buf("ps%d" % i, st.enter_context(nc.psum_tensor("ps%d" % i, [128, 512], F32))) for i in range(8)]

    def psb(i, dt=F32):
        return psum[i].t if dt == F32 else psum[i].t.bitcast(dt)

    dB = {n: S.buf("D_" + n) for n in ("modD", "hTs", "hTo", "R", "yacc", "x1d", "out", "ckv", "kpe", "dbg")}

    def mm(out, lhsT, rhs, start, stop, reads, writes):
        S.op("pe", lambda e: e.matmul(out, lhsT=lhsT, rhs=rhs, start=start, stop=stop), reads, writes)

    def tr(out, in_, idn, reads, writes):
        S.op("pe", lambda e: e.transpose(out, in_, idn), reads, writes)

    def act(out, in_, func, reads, writes, bias=None, scale=None, accum=None, eng="act"):
        kw = {}
        if bias is not None:
            kw["bias"] = bias
        if scale is not None:
            kw["scale"] = scale
        if accum is not None:
            kw["accum_out"] = accum
        S.op(eng, lambda e: e.activation(out=out, in_=in_, func=func, **kw), reads, writes)

    def ts(out, in0, s1, s2, op0, op1, reads, writes, eng="dve"):
        if op1 is None:
            S.op(eng, lambda e: e.tensor_scalar(out=out, in0=in0, scalar1=s1, scalar2=None, op0=op0), reads, writes)
        else:
            S.op(eng, lambda e: e.tensor_scalar(out=out, in0=in0, scalar1=s1, scalar2=s2, op0=op0, op1=op1), reads, writes)

    def tt(out, in0, in1, op, reads, writes, eng="dve"):
        S.op(eng, lambda e: e.tensor_tensor(out=out, in0=in0, in1=in1, op=op), reads, writes)

    def stt(out, in0, sc, in1, op0, op1, reads, writes, eng="dve"):
        S.op(eng, lambda e: e.scalar_tensor_tensor(out=out, in0=in0, scalar=sc, in1=in1, op0=op0, op1=op1), reads, writes)

    def cp(out, in_, reads, writes, eng="dve"):
        if eng == "act":
            S.op(eng, lambda e: e.activation(out=out, in_=in_, func=AF.Copy), reads, writes)
        else:
            S.op(eng, lambda e: e.tensor_copy(out=out, in_=in_), reads, writes)

    def ld(out, in_, reads, writes, eng="sp", slow=False, key=None):
        if slow:
            S.dma(eng, lambda e: e.dma_start(out=out, in_=in_, allow_slow_non_contiguous=True), reads, writes, key=key)
        else:
            S.dma(eng, lambda e: e.dma_start(out=out, in_=in_), reads, writes, key=key)

    def rsqrt_(out, in_, mult, buf):
        ts(out, in_, mult, EPS, ALU.mult, ALU.add, [buf], [buf])
        act(out, out, AF.Sqrt, [buf], [buf])
        S.op("dve", lambda e: e.reciprocal(out=out, in_=out), [buf], [buf])

    idf = alloc("idf", [128, 128]); idb = alloc("idb", [128, 128], BF16); jdb = alloc("jdb", [128, 128], BF16)
    onesb = alloc("onesb", [128, 128], BF16); protb = alloc("protb", [64, 64], BF16)
    tmpc = alloc("tmpc", [128, 128])
    ld(idf[:], ident, [], [idf])
    cp(idb[:], idf[:], [idf], [idb])
    ld(tmpc[:], jdent, [], [tmpc]); cp(jdb[:], tmpc[:], [tmpc], [jdb])
    ld(tmpc[0:64, 0:64], prot, [jdb], [tmpc]); cp(protb[:], tmpc[0:64, 0:64], [tmpc], [protb])
    S.op("dve", lambda e: e.memset(onesb[:], 1.0), [], [onesb])
    modT = alloc("modT", [128, 96, 4]); n1T = alloc("n1T", [128, 16]); n2T = alloc("n2T", [128, 16])
    gw1 = alloc("gw1", [128, 16, 4]); gw2 = alloc("gw2", [128, 16, 4])
    ld(n1T[:], norm1_w.rearrange("(c p) -> p c", p=128), [], [n1T], slow=True)
    ld(n2T[:], norm2_w.rearrange("(c p) -> p c", p=128), [], [n2T], slow=True)
    mark0 = ar_pos[0]
    cT = alloc("cT", [128, 16, 4]); bmT = alloc("bmT", [128, 96])
    for r in range(4):
        ld(cT[:, :, r], cvec[r].rearrange("(c p) -> p c", p=128), [], [cT], slow=True)
    ld(bmT[:], b_mod.rearrange("(q p) -> p q", p=128), [], [bmT], slow=True)
    act(cT[:], cT[:], AF.Silu, [cT], [cT])
    wm = alloc("wm", [128, 16, 512])
    for g in range(24):
        ld(wm[:], w_mod[:, 512 * g:512 * g + 512].rearrange("(c p) n -> p c n", p=128), [], [wm])
        for m in range(4):
            q = 4 * g + m
            pb = psum[q % 2]
            for kc in range(16):
                mm(pb.t[:, 0:4], wm[:, kc, 128 * m:128 * m + 128], cT[:, kc, :], kc == 0, kc == 15, [wm, cT], [pb])
            ts(modT[:, q, :], pb.t[:, 0:4], bmT[:, q:q + 1], None, ALU.add, None, [pb, bmT], [modT])
    for r in range(4):
        stt(gw1[:, :, r], modT[:, 16:32, r], 1.0, n1T[:], ALU.add, ALU.mult, [modT, n1T], [gw1])
        stt(gw2[:, :, r], modT[:, 64:80, r], 1.0, n2T[:], ALU.add, ALU.mult, [modT, n2T], [gw2])
    for r in range(4):
        ld(modD[r].rearrange("(q p) -> p q", p=128), modT[:, :, r], [modT], [dB["modD"]], slow=True)
    phase_reset(mark0)

    fw1s = alloc("fw1s", [33, 64]); fw2s = alloc("fw2s", [64, 64]); fw3s = alloc("fw3s", [64, 4096])
    fsc = alloc("fsc", [64, 6])
    ld(fw1s[:], fw1, [], [fw1s]); ld(fw2s[:], fw2, [], [fw2s]); ld(fw3s[:], fw3, [], [fw3s])
    ld(fsc[:, 0:1], fb1.rearrange("(p o) -> p o", o=1), [], [fsc], slow=True)
    ld(fsc[:, 1:2], fb2.rearrange("(p o) -> p o", o=1), [], [fsc], slow=True)
    for r in range(2):
        ld(fsc[:, 2 + r:3 + r], ffreq[r].rearrange("(p o) -> p o", o=1), [], [fsc], slow=True)
    inv2pi = 1.0 / (2.0 * math.pi)
    ts(fsc[:, 2:4], fsc[:, 2:4], inv2pi, None, ALU.mult, None, [fsc], [fsc])
    tt(fsc[:, 4:6], fsc[:, 0:2], fsc[:, 2:4], ALU.mult, [fsc], [fsc])
    fb3T = alloc("fb3T", [128, 32]); skT = alloc("skT", [128, 16]); adT = alloc("adT", [128, 8])
    ld(fb3T[:], fb3.rearrange("(q p) -> p q", p=128), [], [fb3T], slow=True)
    for r in range(2):
        ld(skT[:, 8 * r:8 * r + 8], skipw[r].rearrange("(q p) -> p q", p=128), [], [skT], slow=True)
    ld(adT[:], absdel.rearrange("(q p) -> p q", p=128), [], [adT], slow=True)
    ts(adT[:], adT[:], -1.0, None, ALU.mult, None, [adT], [adT])
    CH = 512
    featc = alloc("featc", [33, CH]); auxb = alloc("auxb", [128, 4, CH])
    h1 = alloc("h1", [64, CH]); h2 = alloc("h2", [64, CH]); tq = alloc("tq", [64, CH]); tk = alloc("tk", [64, CH])
    dec = alloc("dec", [128, CH]); ff = alloc("ff", [128, CH]); fbk = alloc("fbk", [128, CH]); rrow = alloc("rrow", [128, CH], BF16)
    MAGIC = 12582912.0
    SC2PI = 2.0 * math.pi * (1.0 - 2e-6)

    def sin_layer(outb, pb, col):
        ts(tq[:], pb.t[0:64, 0:CH], fsc[:, 2 + col:3 + col], fsc[:, 4 + col:5 + col], ALU.mult, ALU.add, [pb, fsc], [tq])
        ts(tk[:], tq[:], MAGIC, MAGIC, ALU.add, ALU.subtract, [tq], [tk])
        tt(tq[:], tq[:], tk[:], ALU.subtract, [tq, tk], [tq])
        act(outb[:], tq[:], AF.Sin, [tq], [outb], scale=SC2PI)

    for nm, o in (("s1", 0), ("s2", 1), ("p1", 0), ("p2", 1)):
        fdr, axr, L = ftab[nm]
        for c0 in range(0, L, CH):
            ld(featc[:], fdr[:, c0:c0 + CH], [], [featc])
            ld(auxb[:], axr[:, c0:c0 + CH].partition_broadcast(128), [], [auxb])
            mm(psum[0].t[0:64, 0:CH], fw1s[:, :], featc[:, :], True, True, [fw1s, featc], [psum[0]])
            sin_layer(h1, psum[0], 0)
            mm(psum[1].t[0:64, 0:CH], fw2s[:, :], h1[:, :], True, True, [fw2s, h1], [psum[1]])
            sin_layer(h2, psum[1], 1)
            for cc in range(8):
                qf = (o * 2 + 0) * 8 + cc
                qb = (o * 2 + 1) * 8 + cc
                mm(psum[2].t[:, 0:CH], fw3s[:, qf * 128:qf * 128 + 128], h2[:, :], True, True, [fw3s, h2], [psum[2]])
                mm(psum[3].t[:, 0:CH], fw3s[:, qb * 128:qb * 128 + 128], h2[:, :], True, True, [fw3s, h2], [psum[3]])
                ts(ff[:], psum[2].t[:, 0:CH], fb3T[:, qf:qf + 1], None, ALU.add, None, [psum[2], fb3T], [ff])
                ts(fbk[:], psum[3].t[:, 0:CH], fb3T[:, qb:qb + 1], None, ALU.add, None, [psum[3], fb3T], [fbk])
                tt(ff[:], ff[:], fbk[:], ALU.subtract, [ff, fbk], [ff])
                tt(ff[:], ff[:], auxb[:, 1, :], ALU.mult, [ff, auxb], [ff])
                tt(ff[:], ff[:], fbk[:], ALU.add, [ff, fbk], [ff])
                act(dec[:], auxb[:, 0, :], AF.Exp, [auxb, adT], [dec], scale=adT[:, cc:cc + 1])
                tt(ff[:], ff[:], dec[:], ALU.mult, [ff, dec], [ff])
                tt(ff[:], ff[:], auxb[:, 2, :], ALU.mult, [ff, auxb], [ff])
                stt(rrow[:], auxb[:, 3, :], skT[:, o * 8 + cc:o * 8 + cc + 1], ff[:], ALU.mult, ALU.add, [auxb, skT, ff], [rrow])
                ld(Rt[nm][cc * 128:cc * 128 + 128, c0:c0 + CH], rrow[:], [rrow], [dB["R"]])
    phase_reset(mark0)

    markY = ar_pos[0]
    yhT = alloc("yhT", [128, 8, NTOK], BF16)
    markA = ar_pos[0]
    xt = alloc("xt", [128, D]); xs = alloc("xs", [128, D], BF16); junk = alloc("junk", [128, D], BF16)
    ssq = alloc("ssq", [128, 2]); hTt = alloc("hTt", [128, 16, 128], BF16)

    def norm_T(src_ap, src_reads, r, gw, shbase, emit_dst):
        ld(xt[:], src_ap, src_reads, [xt])
        act(junk[:], xt[:], AF.Square, [xt], [junk, ssq], accum=ssq[:, 0:1])
        rsqrt_(ssq[:, 0:1], ssq[:, 0:1], 1.0 / D, ssq)
        act(xs[:], xt[:], AF.Copy, [xt, ssq], [xs], scale=ssq[:, 0:1])
        for c in range(16):
            pb = psum[4 + c // 8]
            tr(psb(4 + c // 8, BF16)[:, (c % 8) * 128:(c % 8) * 128 + 128], xs[:, c * 128:c * 128 + 128], idb[:], [xs, idb], [pb])
        for c in range(16):
            pb = psum[4 + c // 8]
            ts(hTt[:, c, :], psb(4 + c // 8, BF16)[:, (c % 8) * 128:(c % 8) * 128 + 128], gw[:, c, r:r + 1],
               modT[:, shbase + c, r:r + 1], ALU.mult, ALU.add, [pb, gw, modT], [hTt])
        emit_dst()

    hTo_v = hTo.rearrange("(c p) n -> p c n", p=128)
    hTs_v = hTs.rearrange("(c p) n -> p c n", p=128)
    for t in range(12):
        norm_T(x_own[t * 128:t * 128 + 128, :], [], 0 if t < 4 else 3, gw1, 0,
               lambda t=t: ld(hTo_v[:, :, t * 128:t * 128 + 128], hTt[:], [hTt], [dB["hTo"]]))
    for t in range(32):
        norm_T(x_seq[t * 128:t * 128 + 128, :], [], 3, gw1, 0,
               lambda t=t: ld(hTs_v[:, :, t * 128:t * 128 + 128], hTt[:], [hTt], [dB["hTs"]]))
    phase_reset(markA)

    own1h = din("own1h", [4])
    mskj = alloc("mskj", [128, 4])
    ld(mskj[:], own1h.partition_broadcast(128), [], [mskj])
    wst = alloc("wst", [128, 16, 128]); whb = alloc("whb", [128, 16, 3, 128], BF16)
    cw = alloc("cw", [128, 3, 4])
    hTc = alloc("hTc", [128, 16, 256], BF16)
    Pb = alloc("Pb", [128, 3, NSEQ], BF16)
    ut = alloc("ut", [128, 3, 128], BF16); utf = alloc("utf", [128, 3, 128])
    Z = alloc("Z", [128, 32, 3, 128], BF16)
    z1 = alloc("z1", [128, 32, 128], BF16); z2 = alloc("z2", [128, 8, 128], BF16); x2o = alloc("x2o", [128, 8, 128])
    NG = 4
    Gb = [alloc("G%d" % i, [128, L1S - 128], BF16) for i in range(NG)]
    gctr = [0]

    def hy_segment(cc, hT_v, hT_key, ntok, nseq, nblk, nm1, nm2, nout, out_col0):
        ntile = ntok // 128
        slen = nblk * 128
        for t0 in range(0, ntok, 256):
            ld(hTc[:], hT_v[:, :, t0:t0 + 256], [dB[hT_key]], [hTc])
            for o in range(3):
                pb = psum[o % 2]
                for c in range(16):
                    mm(pb.t[:, 0:256], whb[:, c, o, :], hTc[:, c, :], c == 0, c == 15, [whb, hTc], [pb])
                cp(Pb[:, o, t0:t0 + 256], pb.t[:, 0:256], [pb], [Pb], eng="act")
        for t in range(ntile):
            a0 = t * 128
            sfirst = (a0 % slen) == 0
            slast = ((a0 + 128) % slen) == 0
            for o in range(3):
                ts(utf[:, o, :], Pb[:, o, a0:a0 + 128], cw[:, o, 1:2], cw[:, o, 3:4], ALU.mult, ALU.add, [Pb, cw], [utf])
                lo = 1 if sfirst else 0
                stt(utf[:, o, lo:128], Pb[:, o, a0 + lo - 1:a0 + 127], cw[:, o, 0:1], utf[:, o, lo:128], ALU.mult, ALU.add, [Pb, cw, utf], [utf])
                hi = 127 if slast else 128
                stt(utf[:, o, 0:hi], Pb[:, o, a0 + 1:a0 + hi + 1], cw[:, o, 2:3], utf[:, o, 0:hi], ALU.mult, ALU.add, [Pb, cw, utf], [utf])
            cp(ut[:], utf[:], [utf], [ut], eng="act")
            pbk = psum[2 + t % 2]
            for o in range(3):
                tr(psb(2 + t % 2, BF16)[:, o * 128:o * 128 + 128], ut[:, o, :], idb[:], [ut, idb], [pbk])
            cp(Z[:, t, :, :], psb(2 + t % 2, BF16)[:, 0:384].rearrange("p (o c) -> p o c", o=3), [pbk], [Z])
            mm(pbk.t[:, 0:128], jdb[:, :], Z[:, t, 1, :], True, True, [jdb, Z], [pbk])
            cp(Z[:, t, 1, :], pbk.t[:, 0:128], [pbk], [Z], eng="act")
        L1 = ftab[nm1][2]
        ncol1 = ntile
        cpb1 = 512 // ncol1
        for c in range(128):
            if nseq == 1:
                g = Gb[gctr[0] % NG]; gctr[0] += 1
                src = bass.AP(tensor=Rt[nm1].tensor, offset=(cc * 128 + c) * L1, ap=[[1, 128], [1, L1 - 128]])
                ld(g[:, 0:L1 - 128], src, [dB["R"]], [g])
                gv = g.t
            else:
                if c % 8 == 0:
                    g = Gb[gctr[0] % NG]; gctr[0] += 1
                    src = bass.AP(tensor=Rt[nm1].tensor, offset=(cc * 128 + c) * L1, ap=[[1, 128], [L1, 8], [1, L1 - 128]])
                    ld(g[:, 0:8 * (L1 - 128)].rearrange("p (a n) -> p a n", a=8), src, [dB["R"]], [g])
                gv = g.t[:, (c % 8) * (L1 - 128):(c % 8 + 1) * (L1 - 128)]
            pb = psum[4 + (c // cpb1) % 2]
            cb = (c % cpb1) * ncol1
            das = [0] + [d for d in range(-(nblk - 1), nblk) if d != 0]
            for k, da in enumerate(das):
                f0 = 128 * (nblk - 1 - da)
                lo, hi = max(0, -da), min(nblk, nblk - da)
                if nseq == 1:
                    rhs = Z[:, lo:hi, 0, c]; out = pb.t[:, cb + lo + da:cb + hi + da]
                elif da == 0:
                    rhs = Z[:, 0:ntile, 0, c]; out = pb.t[:, cb:cb + ntile]
                else:
                    rhs = Z[:, lo:ntile:nblk, 0, c]; out = pb.t[:, cb + lo + da:cb + ntile:nblk]
                mm(out, gv[:, f0:f0 + 128], rhs, k == 0, k == len(das) - 1, [g, Z], [pb])
            if (c + 1) % cpb1 == 0:
                c0 = c + 1 - cpb1
                tt(z1[:, 0:ntile, c0:c0 + cpb1], pb.t[:, 0:cpb1 * ncol1].rearrange("p (c t) -> p t c", t=ncol1),
                   Z[:, 0:ntile, 1, c0:c0 + cpb1], ALU.mult, [pb, Z], [z1])
        L2 = ftab[nm2][2]
        cpb2 = 512 // nout
        if nseq == 1:
            for i in range(8):
                ts(x2o[:, i, :], Z[:, i, 2, :], mskj[:, 0:1], None, ALU.mult, None, [Z, mskj], [x2o])
                for jj in range(1, 4):
                    stt(x2o[:, i, :], Z[:, 8 * jj + i, 2, :], mskj[:, jj:jj + 1], x2o[:, i, :], ALU.mult, ALU.add, [Z, mskj, x2o], [x2o])
        for c in range(128):
            if nseq == 1:
                g = Gb[gctr[0] % NG]; gctr[0] += 1
                src = bass.AP(tensor=Rt[nm2].tensor, offset=(cc * 128 + c) * L2, ap=[[1, 128], [1, L2 - 128]])
                ld(g[:, 0:L2 - 128], src, [dB["R"]], [g])
                gv = g.t
            else:
                if c % 8 == 0:
                    g = Gb[gctr[0] % NG]; gctr[0] += 1
                    src = bass.AP(tensor=Rt[nm2].tensor, offset=(cc * 128 + c) * L2, ap=[[1, 128], [L2, 8], [1, L2 - 128]])
                    ld(g[:, 0:8 * (L2 - 128)].rearrange("p (a n) -> p a n", a=8), src, [dB["R"]], [g])
                gv = g.t[:, (c % 8) * (L2 - 128):(c % 8 + 1) * (L2 - 128)]
            pb = psum[6 + (c // cpb2) % 2]
            cb = (c % cpb2) * nout
            if nseq == 1:
                das = [0] + [d for d in range(-(nblk - 1), nout) if d != 0]
            else:
                das = [0] + [d for d in range(-(nblk - 1), nblk) if d != 0]
            for k, da in enumerate(das):
                f0 = 128 * (da + nblk - 1)
                if nseq == 1:
                    ilo, ihi = max(0, da), min(nout, nblk + da)
                    rhs = z1[:, ilo - da:ihi - da, c]; out = pb.t[:, cb + ilo:cb + ihi]
                elif da == 0:
                    rhs = z1[:, 0:ntile, c]; out = pb.t[:, cb:cb + ntile]
                else:
                    lo = max(0, -da)
                    rhs = z1[:, lo:ntile:nblk, c]; out = pb.t[:, cb + lo + da:cb + ntile:nblk]
                mm(out, gv[:, f0:f0 + 128], rhs, k == 0, k == len(das) - 1, [g, z1], [pb])
            if (c + 1) % cpb2 == 0:
                c0 = c + 1 - cpb2
                x2src = x2o[:, 0:nout, c0:c0 + cpb2] if nseq == 1 else Z[:, 0:nout, 2, c0:c0 + cpb2]
                tt(z2[:, 0:nout, c0:c0 + cpb2], pb.t[:, 0:cpb2 * nout].rearrange("p (c t) -> p t c", t=nout),
                   x2src, ALU.mult, [pb, Z, x2o], [z2])
        for i in range(nout):
            pbk = psum[2 + i % 2]
            tr(psb(2 + i % 2, BF16)[:, 0:128], z2[:, i, :], idb[:], [z2, idb], [pbk])
            cp(yhT[:, cc, out_col0 + i * 128:out_col0 + i * 128 + 128], psb(2 + i % 2, BF16)[:, 0:128], [pbk], [yhT], eng="act")

    for cc in range(ncc):
        for o in range(3):
            col = o * 1024 + cc * 128
            ld(wst[:], w_in[:, col:col + 128].rearrange("(c p) n -> p c n", p=128), [], [wst])
            cp(whb[:, :, o, :], wst[:], [wst], [whb])
            for tap in range(3):
                ld(cw[:, o, tap:tap + 1], convw[tap, col:col + 128].rearrange("(p o) -> p o", o=1), [], [cw], slow=True)
            ld(cw[:, o, 3:4], convb[col:col + 128].rearrange("(p o) -> p o", o=1), [], [cw], slow=True)
        hy_segment(cc, hTo_v, "hTo", 512, 2, 2, "p1", "p2", 4, 0)
        hy_segment(cc, hTs_v, "hTs", NSEQ, 1, 32, "s1", "s2", 8, 512)
    phase_reset(markA)

    if hy_only:
        ld(yhT_d, yhT[:], [yhT], [dB["dbg"]])
        S.op("sp", lambda e: e.nop(), [dB["dbg"]], [])
        S.finish(st)
        return nc, S, st
    ymT = alloc("ymT", [128, 16, NTOK], BF16)
    markC = ar_pos[0]
    wstgf = alloc("wstgf", [128, 2048])
    wstg = S.bufs[-1]
    wst16 = wstgf.t.rearrange("p (c n) -> p c n", c=16)
    gq = alloc("gq", [128, 8])
    gh = alloc("gh", [128, 4])
    ld(gq[:, 0:4], q_a_w.rearrange("(c p) -> p c", p=128), [], [gq], slow=True)
    ld(gq[:, 4:6], kv_a_w.rearrange("(c p) -> p c", p=128), [], [gq], slow=True)
    ld(gh[:, 0:1], qn_w.rearrange("(p o) -> p o", o=1), [], [gh], slow=True)
    ld(gh[:, 1:2], kn_w.rearrange("(p o) -> p o", o=1), [], [gh], slow=True)
    ld(gh[0:64, 2:3], qr_w.rearrange("(p o) -> p o", o=1), [], [gh], slow=True)
    ld(gh[0:64, 3:4], kr_w.rearrange("(p o) -> p o", o=1), [], [gh], slow=True)
    ckvT = alloc("ckvT", [128, 2, NKEY + 512], BF16)
    kpeT = alloc("kpeT", [64, NKEY + 512], BF16)
    qcT = alloc("qcT", [128, 4, NTOK], BF16)
    sq = alloc("sq", [128, 4, 512], BF16); rstd = alloc("rstd", [128, 512]); tmpf = alloc("tmpf", [128, 2, 512])
    rope_t = alloc("rope_t", [64, 2, 512]); ybf = alloc("ybf", [64, 512], BF16)
    markC2 = ar_pos[0]
    wq = alloc("wq", [128, 16, 512], BF16); wkv = alloc("wkv", [128, 16, 320], BF16)
    for hh in range(4):
        ld(wst16, w_in[:, 3072 + 128 * hh:3200 + 128 * hh].rearrange("(c p) n -> p c n", p=128), [], [wstg])
        cp(wq[:, :, 128 * hh:128 * hh + 128], wst16, [wstg], [wq])
    for hh in range(2):
        ld(wst16, w_in[:, 3584 + 128 * hh:3712 + 128 * hh].rearrange("(c p) n -> p c n", p=128), [], [wstg])
        cp(wkv[:, :, 128 * hh:128 * hh + 128], wst16, [wstg], [wkv])
    ld(wst16[:, :, 0:64], w_in[:, 3840:3904].rearrange("(c p) n -> p c n", p=128), [], [wstg]); cp(wkv[:, :, 256:320], wst16[:, :, 0:64], [wstg], [wkv])

    def fm_norm(pbs, rows, dim, gains, outs, out_bufs, extra_reads=()):
        n = len(pbs)
        for i, pb in enumerate(pbs):
            act(sq[0:rows, i, :], pb.t[0:rows, :], AF.Square, [pb], [sq])
        for i in range(n):
            mm(psum[7].t[0:rows, :], onesb[0:rows, 0:rows], sq[0:rows, i, :], i == 0, i == n - 1, [onesb, sq], [psum[7]])
        ts(rstd[0:rows, :], psum[7].t[0:rows, :], 1.0 / dim, EPS, ALU.mult, ALU.add, [psum[7]], [rstd])
        act(rstd[0:rows, :], rstd[0:rows, :], AF.Sqrt, [rstd], [rstd])
        S.op("dve", lambda e: e.reciprocal(out=rstd[0:rows, :], in_=rstd[0:rows, :]), [rstd], [rstd])
        for i, pb in enumerate(pbs):
            stt(outs[i], pb.t[0:rows, :], gains[i], rstd[0:rows, :], ALU.mult, ALU.mult, [pb, rstd] + list(extra_reads), out_bufs)

    def rope_fm(src_f32, dst_bf, tab_ap, reads, dst_buf):
        ld(rope_t[:], tab_ap.rearrange("a p n -> p a n"), [], [rope_t])
        cp(ybf[:], src_f32, reads, [ybf])
        mm(psum[7].t[0:64, :], protb[:, :], ybf[:, :], True, True, [protb, ybf], [psum[7]])
        tt(src_f32, src_f32, rope_t[:, 0, :], ALU.mult, reads + [rope_t], reads)
        tt(rstd[0:64, :], psum[7].t[0:64, :], rope_t[:, 1, :], ALU.mult, [psum[7], rope_t], [rstd])
        tt(dst_bf, src_f32, rstd[0:64, :], ALU.add, reads + [rstd], [dst_buf])

    def kv_chunk(hT_v, key, t0, kcol0, is_prompt):
        ld(hTc[:], hT_v[:, :, t0:t0 + 512], [dB[key]], [hTc])
        for m in range(2):
            for c in range(16):
                mm(psum[m].t[:, :], wkv[:, c, 128 * m:128 * m + 128], hTc[:, c, :], c == 0, c == 15, [wkv, hTc], [psum[m]])
        for c in range(16):
            mm(psum[2].t[0:64, :], wkv[:, c, 256:320], hTc[:, c, :], c == 0, c == 15, [wkv, hTc], [psum[2]])
        fm_norm([psum[0], psum[1]], 128, 256.0, [gq[:, 4:5], gq[:, 5:6]], [tmpf[:, 0, :], tmpf[:, 1, :]], [tmpf], [gq])
        cp(ckvT[:, :, kcol0:kcol0 + 512], tmpf[:], [tmpf], [ckvT])
        if is_prompt:
            for m in range(2):
                ld(ckv_out[t0:t0 + 512, 128 * m:128 * m + 128].rearrange("t p -> p t"), tmpf[:, m, :], [tmpf], [dB["ckv"]], slow=True)
        fm_norm([psum[2]], 64, 64.0, [gh[0:64, 3:4]], [tmpf[0:64, 0, :]], [tmpf], [gh])
        if is_prompt:
            ld(kpe_out[t0:t0 + 512, :].rearrange("t p -> p t"), tmpf[0:64, 0, :], [tmpf], [dB["kpe"]], slow=True)
            cp(kpeT[:, kcol0:kcol0 + 512], tmpf[0:64, 0, :], [tmpf], [kpeT])
        else:
            rope_fm(tmpf[0:64, 0, :], kpeT[:, kcol0:kcol0 + 512], ropek[:, :, t0:t0 + 512], [tmpf], kpeT)

    hTc2 = hTc
    hTc = alloc("hTcC", [128, 16, 512], BF16)
    for t0 in range(0, NSEQ, 512):
        kv_chunk(hTs_v, "hTs", t0, t0, False)
    kv_chunk(hTo_v, "hTo", 0, NKEY, True)
    cst = alloc("cst", [128, 2, 320]); cstb = alloc("cstb", [128, 2, 320], BF16)
    ld(cst[:, :, 0:256], c_ckv.rearrange("(a p) n -> p a n", p=128), [], [cst])
    ld(cst[:, :, 256:320], c_kpe.rearrange("(a p) n -> p a n", p=128), [], [cst])
    cp(cstb[:], cst[:], [cst], [cstb])
    for a in range(2):
        for m in range(2):
            tr(psb(3, BF16)[:, 0:128], cstb[:, a, 128 * m:128 * m + 128], idb[:], [cstb, idb], [psum[3]])
            cp(ckvT[:, m, NSEQ + a * 128:NSEQ + a * 128 + 128], psb(3, BF16)[:, 0:128], [psum[3]], [ckvT])
        tr(psb(3, BF16)[0:64, 0:128], cstb[:, a, 256:320], idb[:], [cstb, idb], [psum[3]])
        cp(kpeT[:, NSEQ + a * 128:NSEQ + a * 128 + 128], psb(3, BF16)[0:64, 0:128], [psum[3]], [kpeT])
    for tcn in range(3):
        ld(hTc[:], hTo_v[:, :, tcn * 512:tcn * 512 + 512], [dB["hTo"]], [hTc])
        for m in range(4):
            for c in range(16):
                mm(psum[m].t[:, :], wq[:, c, 128 * m:128 * m + 128], hTc[:, c, :], c == 0, c == 15, [wq, hTc], [psum[m]])
        fm_norm([psum[m] for m in range(4)], 128, 512.0, [gq[:, m:m + 1] for m in range(4)],
                [qcT[:, m, tcn * 512:tcn * 512 + 512] for m in range(4)], [qcT], [gq])
    phase_reset(markC2)
    wuqh = alloc("wuqh", [128, 4, 192], BF16); wukvh = alloc("wukvh", [128, 2, 256], BF16)
    knT = alloc("knT", [128, NKEY + 512], BF16); Vt = alloc("Vt", [128, 38, 128], BF16)
    qnT = alloc("qnT", [128, 512], BF16); qpT = alloc("qpT", [64, 512], BF16); qpf = alloc("qpf", [64, 512])
    PT = [alloc("PT%d" % i, [128, 512], BF16) for i in range(2)]
    SCALE = 192.0 ** -0.5
    for h in range(16):
        vq = wstgf.t[:, 0:768].rearrange("p (c n) -> p c n", c=4)
        vk = wstgf.t[:, 1024:1536].rearrange("p (c n) -> p c n", c=2)
        ld(vq, w_uq[:, 192 * h:192 * h + 192].rearrange("(c p) n -> p c n", p=128), [], [wstg])
        cp(wuqh[:], vq, [wstg], [wuqh])
        ld(vk, w_ukv[:, 256 * h:256 * h + 256].rearrange("(c p) n -> p c n", p=128), [], [wstg])
        cp(wukvh[:], vk, [wstg], [wukvh])
        nkt = (NKEY + 512) // 128
        for k0 in range(0, NKEY + 512, 512):
            w = min(512, NKEY + 512 - k0)
            for kc in range(2):
                mm(psum[0].t[:, 0:w], wukvh[:, kc, 0:128], ckvT[:, kc, k0:k0 + w], kc == 0, kc == 1, [wukvh, ckvT], [psum[0]])
            act(sq[:, 0, 0:w], psum[0].t[:, 0:w], AF.Square, [psum[0]], [sq])
            mm(psum[7].t[:, 0:w], onesb[:, :], sq[:, 0, 0:w], True, True, [onesb, sq], [psum[7]])
            ts(rstd[:, 0:w], psum[7].t[:, 0:w], 1.0 / 128, EPS, ALU.mult, ALU.add, [psum[7]], [rstd])
            act(rstd[:, 0:w], rstd[:, 0:w], AF.Sqrt, [rstd], [rstd])
            S.op("dve", lambda e, w=w: e.reciprocal(out=rstd[:, 0:w], in_=rstd[:, 0:w]), [rstd], [rstd])
            stt(knT[:, k0:k0 + w], psum[0].t[:, 0:w], gh[:, 1:2], rstd[:, 0:w], ALU.mult, ALU.mult, [psum[0], rstd, gh], [knT])
        for kt in range(nkt):
            pb = psum[1 + kt % 2]
            for kc in range(2):
                mm(pb.t[:, 0:128], ckvT[:, kc, kt * 128:kt * 128 + 128], wukvh[:, kc, 128:256], kc == 0, kc == 1, [ckvT, wukvh], [pb])
            cp(Vt[:, kt, :], pb.t[:, 0:128], [pb], [Vt], eng="act")
        groups = [(0, 256, [34, 35], None), (256, 256, [36, 37], None),
                  (512, 512, list(range(34)), 0), (1024, 512, list(range(34)), 512)]
        for (q0, nq, kts, rp) in groups:
            for kc in range(4):
                mm(psum[0].t[:, 0:nq], wuqh[:, kc, 0:128], qcT[:, kc, q0:q0 + nq], kc == 0, kc == 3, [wuqh, qcT], [psum[0]])
            for kc in range(4):
                mm(psum[3].t[0:64, 0:nq], wuqh[:, kc, 128:192], qcT[:, kc, q0:q0 + nq], kc == 0, kc == 3, [wuqh, qcT], [psum[3]])
            act(sq[:, 0, 0:nq], psum[0].t[:, 0:nq], AF.Square, [psum[0]], [sq])
            mm(psum[7].t[:, 0:nq], onesb[:, :], sq[:, 0, 0:nq], True, True, [onesb, sq], [psum[7]])
            ts(rstd[:, 0:nq], psum[7].t[:, 0:nq], 1.0 / 128, EPS, ALU.mult, ALU.add, [psum[7]], [rstd])
            act(rstd[:, 0:nq], rstd[:, 0:nq], AF.Sqrt, [rstd], [rstd])
            S.op("dve", lambda e, nq=nq: e.reciprocal(out=rstd[:, 0:nq], in_=rstd[:, 0:nq]), [rstd], [rstd])
            stt(qnT[:, 0:nq], psum[0].t[:, 0:nq], gh[:, 0:1], rstd[:, 0:nq], ALU.mult, ALU.mult, [psum[0], rstd, gh], [qnT])
            act(sq[0:64, 1, 0:nq], psum[3].t[0:64, 0:nq], AF.Square, [psum[3]], [sq])
            mm(psum[7].t[0:64, 0:nq], onesb[0:64, 0:64], sq[0:64, 1, 0:nq], True, True, [onesb, sq], [psum[7]])
            ts(rstd[0:64, 0:nq], psum[7].t[0:64, 0:nq], 1.0 / 64, EPS, ALU.mult, ALU.add, [psum[7]], [rstd])
            act(rstd[0:64, 0:nq], rstd[0:64, 0:nq], AF.Sqrt, [rstd], [rstd])
            S.op("dve", lambda e, nq=nq: e.reciprocal(out=rstd[0:64, 0:nq], in_=rstd[0:64, 0:nq]), [rstd], [rstd])
            stt(qpf[:, 0:nq], psum[3].t[0:64, 0:nq], gh[0:64, 2:3], rstd[0:64, 0:nq], ALU.mult, ALU.mult, [psum[3], rstd, gh], [qpf])
            if rp is None:
                cp(qpT[:, 0:nq], qpf[:, 0:nq], [qpf], [qpT])
            else:
                rope_fm(qpf[:, :], qpT[:, :], ropeq[:, :, rp:rp + 512], [qpf], qpT)
            for i, kt in enumerate(kts):
                pbs_ = psum[1 + i % 2]
                mm(pbs_.t[:, 0:nq], knT[:, kt * 128:kt * 128 + 128], qnT[:, 0:nq], True, False, [knT, qnT], [pbs_])
                mm(pbs_.t[:, 0:nq], kpeT[:, kt * 128:kt * 128 + 128], qpT[:, 0:nq], False, True, [kpeT, qpT], [pbs_])
                pt = PT[i % 2]
                act(pt[:, 0:nq], pbs_.t[:, 0:nq], AF.Exp, [pbs_], [pt], scale=SCALE, bias=-8.0)
                mm(psum[4].t[:, 0:nq], Vt[:, kt, :], pt[:, 0:nq], i == 0, i == len(kts) - 1, [Vt, pt], [psum[4]])
                mm(psum[5].t[:, 0:nq], onesb[:, :], pt[:, 0:nq], i == 0, i == len(kts) - 1, [onesb, pt], [psum[5]])
            S.op("dve", lambda e, nq=nq: e.reciprocal(out=rstd[:, 0:nq], in_=psum[5].t[:, 0:nq]), [psum[5]], [rstd])
            tt(ymT[:, h, q0:q0 + nq], psum[4].t[:, 0:nq], rstd[:, 0:nq], ALU.mult, [psum[4], rstd], [ymT])
    if dbg:
        ld(yhT_d, yhT[:], [yhT], [dB["dbg"]]); ld(ymT_d, ymT[:], [ymT], [dB["dbg"]])
    phase_reset(markC)

    markD = ar_pos[0]
    wsD = alloc("wsD", [128, 2048]); wsDb = S.bufs[-1]
    wsD16 = wsD.t.rearrange("p (c n) -> p c n", c=16)
    wgh = alloc("wgh", [128, 16, 128], BF16); wgm = alloc("wgm", [128, 16, 128], BF16)
    why = alloc("why", [128, 8, 128], BF16); wml = alloc("wml", [128, 16, 128], BF16)
    hTd = alloc("hTd", [128, 16, 512], BF16); mgT = alloc("mgT", [128, 16, 512], BF16)
    sg1 = alloc("sg1", [128, 512]); sg2 = alloc("sg2", [128, 512])
    wob = alloc("wob", [128, 16, 256], BF16)
    g1b = alloc("g1b", [128, 2, D])
    xres = alloc("xres", [128, 256]); x1t = alloc("x1t", [128, 256])
    for i, r in enumerate((0, 3)):
        ld(g1b[:, i, :], modD[r, 2 * D:3 * D].partition_broadcast(128), [dB["modD"]], [g1b])
    for tcn in range(3):
        gi = 0 if tcn == 0 else 1
        ld(hTd[:], hTo_v[:, :, tcn * 512:tcn * 512 + 512], [dB["hTo"]], [hTd])
        for m in range(16):
            for (dst, col0, nk, src) in ((wgh, 3904 + 128 * m, 16, w_in), (wgm, 5952 + 128 * m, 16, w_in),
                                         (why, 128 * m, 8, w_hy_out), (wml, 128 * m, 16, w_mla_out)):
                ld(wsD16[:, 0:nk, :], src[:, col0:col0 + 128].rearrange("(c p) n -> p c n", p=128), [], [wsDb])
                cp(dst[:], wsD16[:, 0:nk, :], [wsDb], [dst], eng="pool")
            for c in range(16):
                mm(psum[0].t[:, :], wgh[:, c, :], hTd[:, c, :], c == 0, c == 15, [wgh, hTd], [psum[0]])
            for c in range(16):
                mm(psum[1].t[:, :], wgm[:, c, :], hTd[:, c, :], c == 0, c == 15, [wgm, hTd], [psum[1]])
            for c in range(8):
                mm(psum[2].t[:, :], why[:, c, :], yhT[:, c, tcn * 512:tcn * 512 + 512], c == 0, c == 7, [why, yhT], [psum[2]])
            for c in range(16):
                mm(psum[3].t[:, :], wml[:, c, :], ymT[:, c, tcn * 512:tcn * 512 + 512], c == 0, c == 15, [wml, ymT], [psum[3]])
            act(sg1[:], psum[0].t[:, :], AF.Sigmoid, [psum[0]], [sg1])
            act(sg2[:], psum[1].t[:, :], AF.Sigmoid, [psum[1]], [sg2])
            tt(sg1[:], sg1[:], psum[2].t[:, :], ALU.mult, [sg1, psum[2]], [sg1])
            tt(sg2[:], sg2[:], psum[3].t[:, :], ALU.mult, [sg2, psum[3]], [sg2])
            tt(mgT[:, m, :], sg1[:], sg2[:], ALU.add, [sg1, sg2], [mgT])
        for c8 in range(8):
            ld(wsD16[:, :, :], w_o[:, 256 * c8:256 * c8 + 128].rearrange("(c p) n -> p c n", p=128), [], [wsDb])
            cp(wob[:, :, 0:128], wsD16[:, :, :], [wsDb], [wob], eng="pool")
            ld(wsD16[:, :, :], w_o[:, 256 * c8 + 128:256 * c8 + 256].rearrange("(c p) n -> p c n", p=128), [], [wsDb])
            cp(wob[:, :, 128:256], wsD16[:, :, :], [wsDb], [wob], eng="pool")
            for t4 in range(4):
                row0 = tcn * 512 + t4 * 128
                pb = psum[4 + t4 % 2]
                for c in range(16):
                    mm(pb.t[:, 0:256], mgT[:, c, t4 * 128:t4 * 128 + 128], wob[:, c, :], c == 0, c == 15, [mgT, wob], [pb])
                ld(xres[:], x_own[row0:row0 + 128, 256 * c8:256 * c8 + 256], [], [xres])
                tt(x1t[:], pb.t[:, 0:256], g1b[:, gi, 256 * c8:256 * c8 + 256], ALU.mult, [pb, g1b], [x1t])
                tt(x1t[:], x1t[:], xres[:], ALU.add, [x1t, xres], [x1t])
                ld(x1d[row0:row0 + 128, 256 * c8:256 * c8 + 256], x1t[:], [x1t], [dB["x1d"]])
    phase_reset(markY)

    h2T = alloc("h2T", [128, 16, NTOK], BF16)
    wts = alloc("wts", [128, 12, NE])
    markE = ar_pos[0]
    xt = alloc("xt2", [128, D]); xs = alloc("xs2", [128, D], BF16); junk = alloc("junk2", [128, D], BF16)
    ssq = alloc("ssq2", [128, 2]); hTt = alloc("hTt2", [128, 16, 128], BF16)
    wrs = alloc("wrs", [128, 16, NE]); wrb = alloc("wrb", [128, 16, NE], BF16); brb = alloc("brb", [128, NE])
    lg = alloc("lg", [128, NE]); mx8 = alloc("mx8", [128, 8]); msk = alloc("msk", [128, NE]); sm = alloc("sm", [128, 2])
    bdn = alloc("bdn", [NE, D]); wtT = alloc("wtT", [NE, 128]); ybi = alloc("ybi", [128, D])
    ld(wrs[:], w_router.rearrange("(c p) n -> p c n", p=128), [], [wrs]); cp(wrb[:], wrs[:], [wrs], [wrb])
    ld(brb[:], b_router.partition_broadcast(128), [], [brb])
    ld(bdn[:], b_dn, [], [bdn])
    for t in range(12):
        r = 0 if t < 4 else 3
        norm_T(x1d[t * 128:t * 128 + 128, :], [dB["x1d"]], r, gw2, 48,
               lambda t=t: cp(h2T[:, :, t * 128:t * 128 + 128], hTt[:], [hTt], [h2T], eng="pool"))
        for c in range(16):
            mm(psum[0].t[:, 0:NE], hTt[:, c, :], wrb[:, c, :], c == 0, c == 15, [hTt, wrb], [psum[0]])
        tt(lg[:], psum[0].t[:, 0:NE], brb[:], ALU.add, [psum[0], brb], [lg])
        S.op("dve", lambda e: e.max(out=mx8[:], in_=lg[:]), [lg], [mx8])
        ts(msk[:], lg[:], mx8[:, 3:4], None, ALU.is_ge, None, [lg, mx8], [msk])
        ts(sm[:, 1:2], mx8[:, 0:1], -1.0, None, ALU.mult, None, [mx8], [sm])
        act(lg[:], lg[:], AF.Exp, [lg, sm], [lg], bias=sm[:, 1:2])
        tt(lg[:], lg[:], msk[:], ALU.mult, [lg, msk], [lg])
        S.op("dve", lambda e: e.reduce_sum(out=sm[:, 0:1], in_=lg[:], axis=AX.X), [lg], [sm])
        S.op("dve", lambda e: e.reciprocal(out=sm[:, 0:1], in_=sm[:, 0:1]), [sm], [sm])
        ts(wts[:, t, :], lg[:], sm[:, 0:1], None, ALU.mult, None, [lg, sm], [wts])
        tr(psum[1].t[0:NE, 0:128], wts[:, t, :], idf[:], [wts, idf], [psum[1]])
        cp(wtT[:], psum[1].t[0:NE, 0:128], [psum[1]], [wtT])
        for c4 in range(4):
            mm(psum[2 + c4 % 2].t[:, :], wtT[:, :], bdn[:, 512 * c4:512 * c4 + 512], True, True, [wtT, bdn], [psum[2 + c4 % 2]])
            cp(ybi[:, 512 * c4:512 * c4 + 512], psum[2 + c4 % 2].t[:, :], [psum[2 + c4 % 2]], [ybi], eng="act")
        ld(yacc[t * 128:t * 128 + 128, :], ybi[:], [ybi], [dB["yacc"]])
    if dbg:
        ld(wts_d, wts[:], [wts], [dB["dbg"]]); ld(h2T_d, h2T[:], [h2T], [dB["dbg"]])
    phase_reset(markE)

    hid = alloc("hid", [128, 16, NTOK], BF16)
    wsE = [alloc("wsE%d" % i, [128, 16, 256]) for i in range(2)]
    wgb = [alloc("wgb%d" % i, [128, 16, 2, 128], BF16) for i in range(2)]
    wdb = [alloc("wdb%d" % i, [128, 16, 256], BF16) for i in range(2)]
    bgT = [alloc("bgT%d" % i, [128, 2]) for i in range(2)]
    gc = [alloc("gc%d" % i, [128, 512]) for i in range(2)]; sgm = [alloc("sgm%d" % i, [128, 512], BF16) for i in range(2)]
    uc = [alloc("uc%d" % i, [128, 512], BF16) for i in range(2)]
    osb = [alloc("osb%d" % i, [128, 6, 256]) for i in range(2)]
    yacc_v = yacc.rearrange("(t p) n -> p t n", p=128)
    wctr = 0; ectr = 0; octr = 0
    for e_ in range(0 if skip_moe else NE):
        for j in range(16):
            ws = wsE[wctr % 2]; wg = wgb[wctr % 2]; bg = bgT[wctr % 2]; wctr += 1
            ld(ws[:], w_gu[e_, :, 256 * j:256 * j + 256].rearrange("(c p) n -> p c n", p=128), [], [ws])
            cp(wg[:], ws[:].rearrange("p c (f two) -> p c two f", two=2), [ws], [wg], eng="pool")
            ld(bg[:], b_gu[e_, 256 * j:256 * j + 256].rearrange("(f two) -> f two", two=2), [], [bg], slow=True)
            for tcn in range(3):
                cols = slice(tcn * 512, tcn * 512 + 512)
                pg, pu = (psum[0], psum[1]) if ectr % 2 == 0 else (psum[6], psum[7])
                g_, s_, u_ = gc[ectr % 2], sgm[ectr % 2], uc[ectr % 2]
                ectr += 1
                for c in range(16):
                    mm(pg.t[:, :], wg[:, c, 0, :], h2T[:, c, cols], c == 0, c == 15, [wg, h2T], [pg])
                for c in range(16):
                    mm(pu.t[:, :], wg[:, c, 1, :], h2T[:, c, cols], c == 0, c == 15, [wg, h2T], [pu])
                ts(g_[:], pg.t[:, :], bg[:, 0:1], 7.0, ALU.add, ALU.min, [pg, bg], [g_])
                act(s_[:], g_[:], AF.Sigmoid, [g_], [s_], scale=1.702)
                ts(u_[:], pu.t[:, :], bg[:, 1:2], 7.0, ALU.add, ALU.min, [pu, bg], [u_])
                ts(u_[:], u_[:], -7.0, 1.0, ALU.max, ALU.add, [u_], [u_], eng="pool")
                tt(g_[:], g_[:], s_[:], ALU.mult, [g_, s_], [g_])
                tt(hid[:, j, cols], g_[:], u_[:], ALU.mult, [g_, u_], [hid], eng="pool")
        for c8 in range(8):
            ws = wsE[wctr % 2]; wd = wdb[wctr % 2]; wctr += 1
            ld(ws[:], w_dn[e_, :, 256 * c8:256 * c8 + 256].rearrange("(j p) n -> p j n", p=128), [], [ws])
            cp(wd[:], ws[:], [ws], [wd], eng="pool")
            for t in range(12):
                if t % 6 == 0:
                    ob = osb[octr % 2]; octr += 1
                pb = psum[2 + t % 4]
                for j in range(16):
                    mm(pb.t[:, 0:256], hid[:, j, t * 128:t * 128 + 128], wd[:, j, :], j == 0, j == 15, [hid, wd], [pb])
                if t % 2 == 0:
                    ts(ob[:, t % 6, :], pb.t[:, 0:256], wts[:, t, e_:e_ + 1], None, ALU.mult, None, [pb, wts], [ob])
                else:
                    act(ob[:, t % 6, :], pb.t[:, 0:256], AF.Copy, [pb, wts], [ob], scale=wts[:, t, e_:e_ + 1])
                if t % 6 == 5:
                    t0_ = t - 5
                    S.dma("pool", lambda e, ob=ob, c8=c8, t0_=t0_: e.dma_start(out=yacc_v[:, t0_:t0_ + 6, 256 * c8:256 * c8 + 256], in_=ob[:], accum_op=ALU.add),
                          [ob], [dB["yacc"]])
    phase_reset(markE)
    g2b = alloc("g2b", [128, 2, D]); ya = alloc("ya", [128, D]); xf = alloc("xf", [128, D])
    for i, r in enumerate((0, 3)):
        ld(g2b[:, i, :], modD[r, 5 * D:6 * D].partition_broadcast(128), [dB["modD"]], [g2b])
    for t in range(12):
        gi = 0 if t < 4 else 1
        ld(ya[:], yacc[t * 128:t * 128 + 128, :], [dB["yacc"]], [ya])
        ld(xf[:], x1d[t * 128:t * 128 + 128, :], [dB["x1d"]], [xf])
        tt(ya[:], ya[:], g2b[:, gi, :], ALU.mult, [ya, g2b], [ya])
        tt(ya[:], ya[:], xf[:], ALU.add, [ya, xf], [ya])
        ld(y_out[t * 128:t * 128 + 128, :], ya[:], [ya], [dB["out"]])
    S.op("sp", lambda e: e.nop(), [dB["out"], dB["ckv"], dB["kpe"], dB["dbg"]], [])
    S.finish(st)
    return nc, S, st


def _feat_table(lags, n):
    lags = np.asarray(lags, np.int64)
    valid = (np.abs(lags) <= n - 1)
    pos = np.where(lags >= 0, lags, -lags - 1)
    pos = np.clip(pos, 0, n - 1)
    t = np.linspace(0.0, 1.0, n, dtype=np.float32)
    w = (np.float32(2.0 * math.pi / n) * np.arange(n, dtype=np.float32)).astype(np.float32)
    bands = np.linspace(1e-4, 15.0, 16, dtype=np.float32)
    arg = (bands[None, :] * w[:, None]).astype(np.float32)
    feats = np.concatenate([t[:, None], np.cos(arg), -np.sin(arg)], axis=-1).astype(np.float32)
    f = np.ascontiguousarray(feats[pos].T)
    aux = np.stack([t[pos], (lags >= 0).astype(np.float32), valid.astype(np.float32),
                    (lags == 0).astype(np.float32)]).astype(np.float32)
    return f, np.ascontiguousarray(aux)


def _rope_tables(n_tokens):
    rows = n_tokens // 64
    row = np.repeat(np.arange(rows, dtype=np.float32), 64)
    col = np.tile(np.arange(64, dtype=np.float32), rows)
    n_freq = 16
    inv_freq = np.power(np.float32(10000.0), -np.arange(n_freq, dtype=np.float32) / n_freq).astype(np.float32)
    ang = np.concatenate([row[:, None] * inv_freq, col[:, None] * inv_freq], axis=-1)
    ang = np.concatenate([ang, ang], axis=-1).astype(np.float32)
    return np.cos(ang).astype(np.float32), np.sin(ang).astype(np.float32)


def kernel(x_prompt, x_sample, cache_ckv, cache_kpe, c, c_ctx, w_mod, b_mod, norm1_w, norm2_w,
           w_in, hy_conv_w, hy_conv_b, filt_w1, filt_b1, filt_w2, filt_b2, filt_w3, filt_b3,
           filt_freq, hy_skip, q_a_norm_w, w_uq, kv_a_norm_w, w_ukv, qn_norm_w, kn_norm_w,
           qr_norm_w, kr_norm_w, w_hy_out, w_mla_out, w_o, w_router, b_router, w_gate_up,
           b_gate_up, w_down, b_down):
    A = lambda a: np.ascontiguousarray(np.asarray(a, dtype=np.float32))
    x_prompt = A(x_prompt); x_sample = A(x_sample); cache_ckv = A(cache_ckv); cache_kpe = A(cache_kpe)
    c = A(c); c_ctx = A(c_ctx)
    dbg = bool(_NC_CACHE.get("dbg", False))
    if "nc" not in _NC_CACHE:
        _NC_CACHE["nc"] = build(dbg=dbg, skip_moe=dbg, **_NC_CACHE.get("bkw", {}))[0]
    nc = _NC_CACHE["nc"]
    ident = np.eye(128, dtype=np.float32)
    jdent = np.ascontiguousarray(ident[::-1])
    prot = np.zeros((64, 64), np.float32)
    for m in range(32):
        prot[m + 32, m] = -1.0
    for m in range(32, 64):
        prot[m - 32, m] = 1.0
    max_decay = math.log(1e-2) / 0.3
    min_decay = math.log(1e-2) / 1.5
    absdel = np.abs(np.linspace(min_decay, max_decay, 1024, dtype=np.float32)).astype(np.float32)
    cosr, sinr = _rope_tables(4096)
    ropek = np.ascontiguousarray(np.stack([cosr.T, sinr.T]))
    f_s1, a_s1 = _feat_table(4095 - np.arange(L1S), 4096)
    f_p1, a_p1 = _feat_table(255 - np.arange(LP), 256)
    f_p2, a_p2 = _feat_table(np.arange(LP) - 255, 256)
    shared = {
        "w_mod": A(w_mod[0]), "b_mod": A(b_mod[0]), "norm1_w": A(norm1_w[0]), "norm2_w": A(norm2_w[0]),
        "w_in": A(w_in[0]), "convw": A(hy_conv_w[0]), "convb": A(hy_conv_b[0]),
        "fw1": A(filt_w1[0]), "fb1": A(filt_b1[0]), "fw2": A(filt_w2[0]), "fb2": A(filt_b2[0]),
        "fw3": A(filt_w3[0]), "fb3": A(filt_b3[0]), "ffreq": A(filt_freq[0]), "skipw": A(hy_skip[0]),
        "q_a_w": A(q_a_norm_w[0]), "w_uq": A(w_uq[0]), "kv_a_w": A(kv_a_norm_w[0]), "w_ukv": A(w_ukv[0]),
        "qn_w": A(qn_norm_w[0]), "kn_w": A(kn_norm_w[0]), "qr_w": A(qr_norm_w[0]), "kr_w": A(kr_norm_w[0]),
        "w_hy_out": A(w_hy_out[0]), "w_mla_out": A(w_mla_out[0]), "w_o": A(w_o[0]),
        "w_router": A(w_router[0]), "b_router": A(b_router[0]),
        "w_gu": A(w_gate_up[0]), "b_gu": A(b_gate_up[0]), "w_dn": A(w_down[0]), "b_dn": A(b_down[0]),
        "ident": ident, "jdent": jdent, "prot": prot, "absdel": absdel, "ropek": ropek,
        "feat_s1": f_s1, "aux_s1": a_s1, "feat_p1": f_p1, "aux_p1": a_p1, "feat_p2": f_p2, "aux_p2": a_p2,
    }
    in_maps = []
    for k in range(8):
        b, j = k // 4, k % 4
        f_s2, a_s2 = _feat_table(np.arange(L2S) + 1024 * j - 4095, 4096)
        own1h = np.zeros(4, np.float32); own1h[j] = 1.0
        m = dict(shared)
        m.update({
            "x_own": np.ascontiguousarray(np.concatenate([x_prompt[2 * k], x_prompt[2 * k + 1],
                                                          x_sample[b, 1024 * j:1024 * j + 1024]], axis=0)),
            "x_seq": np.ascontiguousarray(x_sample[b]),
            "cvec": np.ascontiguousarray(np.stack([c_ctx, c[0], c[1], c[b]])),
            "c_ckv": np.ascontiguousarray(cache_ckv[b, 0]), "c_kpe": np.ascontiguousarray(cache_kpe[b, 0]),
            "feat_s2": f_s2, "aux_s2": a_s2, "own1h": own1h,
            "ropeq": np.ascontiguousarray(ropek[:, :, 1024 * j:1024 * j + 1024]),
        })
        in_maps.append(m)
    if dbg:
        in_maps = [{k2: v for k2, v in m.items() if k2 in _NC_CACHE["in_names"]} for m in in_maps]
    res = run_bass_kernel_spmd(nc, in_maps, core_ids=list(range(8)))
    if dbg:
        _NC_CACHE["res"] = res
        return None
    y_p = np.zeros((16, 256, D), np.float32); y_s = np.zeros((2, 4096, D), np.float32)
    n_ckv = np.zeros((16, 1, 256, 256), np.float32); n_kpe = np.zeros((16, 1, 256, 64), np.float32)
    for k in range(8):
        b, j = k // 4, k % 4
        r = res.results[k]
        yo = np.asarray(r["y_out"], np.float32)
        y_p[2 * k] = yo[0:256]; y_p[2 * k + 1] = yo[256:512]
        y_s[b, 1024 * j:1024 * j + 1024] = yo[512:1536]
        ck = np.asarray(r["ckv_out"], np.float32); kp = np.asarray(r["kpe_out"], np.float32)
        n_ckv[2 * k, 0] = ck[0:256]; n_ckv[2 * k + 1, 0] = ck[256:512]
        n_kpe[2 * k, 0] = kp[0:256]; n_kpe[2 * k + 1, 0] = kp[256:512]
    return (y_p, y_s, n_ckv, n_kpe)
```

```python
import contextlib
import math
import numpy as np
import concourse.bass as bass
import concourse.mybir as mybir
from concourse.bass_utils import run_bass_kernel_spmd

F32 = mybir.dt.float32
BF16 = mybir.dt.bfloat16
ALU = mybir.AluOpType
AF = mybir.ActivationFunctionType
AX = mybir.AxisListType
ENGS = ("pe", "act", "dve", "pool", "sp")

D = 2048
NE = 32
EPS = 1e-6
_NC_CACHE = {}


class Buf:
    __slots__ = ("name", "t", "w", "r", "dsem", "dcnt")

    def __init__(self, name, t=None):
        self.name = name
        self.t = t
        self.w = None
        self.r = []
        self.dsem = None
        self.dcnt = 0

    def __getitem__(self, idx):
        return self.t[idx]


class Op:
    __slots__ = ("eng", "fn", "reads", "writes", "dma", "idx", "waits", "mark", "dtok", "key")

    def __init__(self, eng, fn, reads, writes, dma=False, key=None):
        self.eng, self.fn, self.reads, self.writes, self.dma, self.key = eng, fn, reads, writes, dma, key
        self.waits = []
        self.mark = False
        self.dtok = None


class Sched:
    def __init__(self, nc):
        self.nc = nc
        self.ops = []
        self.bufs = []

    def buf(self, name, t=None):
        b = Buf(name, t)
        self.bufs.append(b)
        return b

    def op(self, eng, fn, reads=(), writes=()):
        self.ops.append(Op(eng, fn, tuple(reads), tuple(writes)))

    def dma(self, eng, fn, reads=(), writes=(), key=None):
        self.ops.append(Op(eng, fn, tuple(reads), tuple(writes), dma=True, key=key))

    def barrier(self):
        bb = Buf("bar%d" % len(self.ops))
        allb = list(self.bufs)
        self.ops.append(Op("sp", lambda e: e.nop(), (), tuple(allb) + (bb,)))
        for e in ("pe", "act", "dve", "pool"):
            self.ops.append(Op(e, lambda en: en.nop(), (bb,), ()))

    def finish(self, stack):
        nc = self.nc
        per_eng = {e: [] for e in ENGS}
        for o in self.ops:
            o.idx = len(per_eng[o.eng])
            per_eng[o.eng].append(o)
        esem = {e: stack.enter_context(nc.semaphore("s_" + e)) for e in ENGS}
        waited_e = {e: {f: -1 for f in ENGS} for e in ENGS}
        waited_d = {e: {} for e in ENGS}
        sem_pool = []
        for o in self.ops:
            deps = []
            for b in o.reads:
                if b.w is not None:
                    deps.append(b.w)
            for b in o.writes:
                if b.w is not None:
                    deps.append(b.w)
                deps.extend(b.r)
            if o.dma:
                k = o.key if o.key is not None else (o.writes[0] if o.writes else o.reads[0])
                if k.dsem is None:
                    k.dsem = stack.enter_context(nc.semaphore("d%d" % len(sem_pool)))
                    sem_pool.append(k.dsem)
                k.dcnt += 16
                tok = ("d", k, k.dcnt)
                o.dtok = tok
            else:
                tok = ("e", o.eng, o.idx)
            for t in deps:
                if t[0] == "e":
                    _, pe_, pidx = t
                    if pe_ == o.eng and pe_ == "pe" and not o.dma:
                        continue
                    if waited_e[o.eng][pe_] >= pidx:
                        continue
                    waited_e[o.eng][pe_] = pidx
                    o.waits.append(t)
                    per_eng[pe_][pidx].mark = True
                else:
                    _, k, val = t
                    if waited_d[o.eng].get(id(k), 0) >= val:
                        continue
                    waited_d[o.eng][id(k)] = val
                    o.waits.append(t)
            for b in o.reads:
                b.r.append(tok)
            for b in o.writes:
                b.w = tok
                b.r = []
        sig = {e: [] for e in ENGS}
        for e in ENGS:
            c = 0
            for o in per_eng[e]:
                if o.mark and not o.dma:
                    c += 1
                sig[e].append(c)
        self.nsem = len(sem_pool)
        self.nops = {e: len(per_eng[e]) for e in ENGS}
        with nc.Block() as block:
            def make(e):
                def body(engine):
                    for o in per_eng[e]:
                        for t in o.waits:
                            if t[0] == "e":
                                engine.wait_ge(esem[t[1]], sig[t[1]][t[2]])
                            else:
                                engine.wait_ge(t[1].dsem, t[2])
                        ins = o.fn(engine)
                        if o.dma:
                            ins.then_inc(o.dtok[1].dsem, 16)
                        elif o.mark:
                            ins.then_inc(esem[e], 1)
                return body
            block.tensor(make("pe"))
            block.scalar(make("act"))
            block.vector(make("dve"))
            block.gpsimd(make("pool"))
            block.sync(make("sp"))


NTOK = 1536
NSEQ = 4096
NKEY = 4352
L1S, L2S, LP = 8192, 5120, 512
HY = 1024


def build(dbg=False, skip_moe=False, ncc=8, hy_only=False):
    nc = bass.Bass("TRN2", target_bir_lowering=False)
    st = contextlib.ExitStack()
    S = Sched(nc)

    in_names = []
    _NC_CACHE["in_names"] = in_names

    def din(name, shape, dt=F32):
        in_names.append(name)
        return nc.dram_tensor(name, list(shape), dt, kind="ExternalInput").ap()

    def dout(name, shape, dt=F32):
        return nc.dram_tensor(name, list(shape), dt, kind="ExternalOutput").ap()

    def dscr(name, shape, dt):
        return nc.dram_tensor(name, list(shape), dt, kind=("ExternalOutput" if dbg else "Internal")).ap()

    x_own = din("x_own", [NTOK, D]); x_seq = din("x_seq", [NSEQ, D])
    cvec = din("cvec", [4, D]); w_mod = din("w_mod", [D, 6 * D]); b_mod = din("b_mod", [6 * D])
    norm1_w = din("norm1_w", [D]); norm2_w = din("norm2_w", [D])
    w_in = din("w_in", [D, 8000])
    convw = din("convw", [3, 3072]); convb = din("convb", [3072])
    fw1 = din("fw1", [33, 64]); fb1 = din("fb1", [64]); fw2 = din("fw2", [64, 64]); fb2 = din("fb2", [64])
    fw3 = din("fw3", [64, 4096]); fb3 = din("fb3", [4096]); ffreq = din("ffreq", [2, 64]); skipw = din("skipw", [2, HY])
    q_a_w = din("q_a_w", [512]); w_uq = din("w_uq", [512, 3072]); kv_a_w = din("kv_a_w", [256]); w_ukv = din("w_ukv", [256, 4096])
    qn_w = din("qn_w", [128]); kn_w = din("kn_w", [128]); qr_w = din("qr_w", [64]); kr_w = din("kr_w", [64])
    w_hy_out = din("w_hy_out", [HY, D]); w_mla_out = din("w_mla_out", [D, D]); w_o = din("w_o", [D, D])
    w_router = din("w_router", [D, NE]); b_router = din("b_router", [NE])
    b_gu = din("b_gu", [NE, 2 * D]); b_dn = din("b_dn", [NE, D])
    if not skip_moe:
        w_gu = din("w_gu", [NE, D, 2 * D]); w_dn = din("w_dn", [NE, D, D])
    c_ckv = din("c_ckv", [256, 256]); c_kpe = din("c_kpe", [256, 64])
    ident = din("ident", [128, 128]); jdent = din("jdent", [128, 128]); prot = din("prot", [64, 64])
    ftab = {}
    for nm, L in (("s1", L1S), ("s2", L2S), ("p1", LP), ("p2", LP)):
        ftab[nm] = (din("feat_" + nm, [33, L]), din("aux_" + nm, [4, L]), L)
    absdel = din("absdel", [HY])
    ropek = din("ropek", [2, 64, NSEQ])
    ropeq = din("ropeq", [2, 64, 1024])
    y_out = dout("y_out", [NTOK, D]); ckv_out = dout("ckv_out", [512, 256]); kpe_out = dout("kpe_out", [512, 64])
    modD = dscr("modD", [4, 6 * D], F32)
    hTs = dscr("hTs", [D, NSEQ], BF16); hTo = dscr("hTo", [D, NTOK], BF16)
    Rt = {nm: dscr("R_" + nm, [HY, ftab[nm][2]], BF16) for nm in ftab}
    yacc = dscr("yacc", [8, 128, 12, 256], F32)
    x1d = dscr("x1d", [NTOK, D], F32)
    if dbg:
        yhT_d = dout("yhT_d", [128, 8, NTOK], BF16); ymT_d = dout("ymT_d", [128, 16, NTOK], BF16)
        wts_d = dout("wts_d", [128, 12, NE]); h2T_d = dout("h2T_d", [128, 16, NTOK], BF16)

    ARW = 49000
    arena = st.enter_context(nc.sbuf_tensor("arena", [128, ARW], F32))
    ar_pos = [0]

    def alloc(name, shape, dt=F32):
        free = int(np.prod(shape[1:]))
        words = free if dt == F32 else (free + 1) // 2
        words = (words + 7) // 8 * 8
        o = ar_pos[0]
        ar_pos[0] += words
        assert ar_pos[0] <= ARW, (name, ar_pos[0])
        ap = arena[0:shape[0], o:o + words]
        if dt != F32:
            ap = ap.bitcast(dt)[:, 0:free]
        else:
            ap = ap[:, 0:free]
        if len(shape) > 2:
            names = " ".join("d%d" % i for i in range(len(shape) - 1))
            kw = {"d%d" % i: shape[i + 1] for i in range(len(shape) - 1)}
            ap = ap.rearrange("p (%s) -> p %s" % (names, names), **kw)
        return S.buf(name, ap)

    def phase_reset(mark):
        S.barrier()
        ar_pos[0] = mark

    psum = [S.buf("ps%d" % i, st.enter_context(nc.psum_tensor("ps%d" % i, [128, 512], F32))) for i in range(8)]

    def psb(i, dt=F32):
        return psum[i].t if dt == F32 else psum[i].t.bitcast(dt)

    dB = {n: S.buf("D_" + n) for n in ("modD", "hTs", "hTo", "R", "yacc", "x1d", "out", "ckv", "kpe", "dbg")}

    def mm(out, lhsT, rhs, start, stop, reads, writes):
        S.op("pe", lambda e: e.matmul(out, lhsT=lhsT, rhs=rhs, start=start, stop=stop), reads, writes)

    def tr(out, in_, idn, reads, writes):
        S.op("pe", lambda e: e.transpose(out, in_, idn), reads, writes)

    def act(out, in_, func, reads, writes, bias=None, scale=None, accum=None, eng="act"):
        kw = {}
        if bias is not None:
            kw["bias"] = bias
        if scale is not None:
            kw["scale"] = scale
        if accum is not None:
            kw["accum_out"] = accum
        S.op(eng, lambda e: e.activation(out=out, in_=in_, func=func, **kw), reads, writes)

    def ts(out, in0, s1, s2, op0, op1, reads, writes, eng="dve"):
        if op1 is None:
            S.op(eng, lambda e: e.tensor_scalar(out=out, in0=in0, scalar1=s1, scalar2=None, op0=op0), reads, writes)
        else:
            S.op(eng, lambda e: e.tensor_scalar(out=out, in0=in0, scalar1=s1, scalar2=s2, op0=op0, op1=op1), reads, writes)

    def tt(out, in0, in1, op, reads, writes, eng="dve"):
        S.op(eng, lambda e: e.tensor_tensor(out=out, in0=in0, in1=in1, op=op), reads, writes)

    def stt(out, in0, sc, in1, op0, op1, reads, writes, eng="dve"):
        S.op(eng, lambda e: e.scalar_tensor_tensor(out=out, in0=in0, scalar=sc, in1=in1, op0=op0, op1=op1), reads, writes)

    def cp(out, in_, reads, writes, eng="dve"):
        if eng == "act":
            S.op(eng, lambda e: e.activation(out=out, in_=in_, func=AF.Copy), reads, writes)
        else:
            S.op(eng, lambda e: e.tensor_copy(out=out, in_=in_), reads, writes)

    def ld(out, in_, reads, writes, eng="sp", slow=False, key=None):
        if slow:
            S.dma(eng, lambda e: e.dma_start(out=out, in_=in_, allow_slow_non_contiguous=True), reads, writes, key=key)
        else:
            S.dma(eng, lambda e: e.dma_start(out=out, in_=in_), reads, writes, key=key)

    def rsqrt_(out, in_, mult, buf):
        ts(out, in_, mult, EPS, ALU.mult, ALU.add, [buf], [buf])
        act(out, out, AF.Sqrt, [buf], [buf])
        S.op("dve", lambda e: e.reciprocal(out=out, in_=out), [buf], [buf])

    idf = alloc("idf", [128, 128]); idb = alloc("idb", [128, 128], BF16); jdb = alloc("jdb", [128, 128], BF16)
    onesb = alloc("onesb", [128, 128], BF16); protb = alloc("protb", [64, 64], BF16)
    tmpc = alloc("tmpc", [128, 128])
    ld(idf[:], ident, [], [idf])
    cp(idb[:], idf[:], [idf], [idb])
    ld(tmpc[:], jdent, [], [tmpc]); cp(jdb[:], tmpc[:], [tmpc], [jdb])
    ld(tmpc[0:64, 0:64], prot, [jdb], [tmpc]); cp(protb[:], tmpc[0:64, 0:64], [tmpc], [protb])
    S.op("dve", lambda e: e.memset(onesb[:], 1.0), [], [onesb])
    modT = alloc("modT", [128, 96, 4]); n1T = alloc("n1T", [128, 16]); n2T = alloc("n2T", [128, 16])
    gw1 = alloc("gw1", [128, 16, 4]); gw2 = alloc("gw2", [128, 16, 4])
    ld(n1T[:], norm1_w.rearrange("(c p) -> p c", p=128), [], [n1T], slow=True)
    ld(n2T[:], norm2_w.rearrange("(c p) -> p c", p=128), [], [n2T], slow=True)
    mark0 = ar_pos[0]
    cT = alloc("cT", [128, 16, 4]); bmT = alloc("bmT", [128, 96])
    for r in range(4):
        ld(cT[:, :, r], cvec[r].rearrange("(c p) -> p c", p=128), [], [cT], slow=True)
    ld(bmT[:], b_mod.rearrange("(q p) -> p q", p=128), [], [bmT], slow=True)
    act(cT[:], cT[:], AF.Silu, [cT], [cT])
    wm = alloc("wm", [128, 16, 512])
    for g in range(24):
        ld(wm[:], w_mod[:, 512 * g:512 * g + 512].rearrange("(c p) n -> p c n", p=128), [], [wm])
        for m in range(4):
            q = 4 * g + m
            pb = psum[q % 2]
            for kc in range(16):
                mm(pb.t[:, 0:4], wm[:, kc, 128 * m:128 * m + 128], cT[:, kc, :], kc == 0, kc == 15, [wm, cT], [pb])
            ts(modT[:, q, :], pb.t[:, 0:4], bmT[:, q:q + 1], None, ALU.add, None, [pb, bmT], [modT])
    for r in range(4):
        stt(gw1[:, :, r], modT[:, 16:32, r], 1.0, n1T[:], ALU.add, ALU.mult, [modT, n1T], [gw1])
        stt(gw2[:, :, r], modT[:, 64:80, r], 1.0, n2T[:], ALU.add, ALU.mult, [modT, n2T], [gw2])
    for r in range(4):
        ld(modD[r].rearrange("(q p) -> p q", p=128), modT[:, :, r], [modT], [dB["modD"]], slow=True)
    phase_reset(mark0)

    fw1s = alloc("fw1s", [33, 64]); fw2s = alloc("fw2s", [64, 64]); fw3s = alloc("fw3s", [64, 4096])
    fsc = alloc("fsc", [64, 6])
    ld(fw1s[:], fw1, [], [fw1s]); ld(fw2s[:], fw2, [], [fw2s]); ld(fw3s[:], fw3, [], [fw3s])
    ld(fsc[:, 0:1], fb1.rearrange("(p o) -> p o", o=1), [], [fsc], slow=True)
    ld(fsc[:, 1:2], fb2.rearrange("(p o) -> p o", o=1), [], [fsc], slow=True)
    for r in range(2):
        ld(fsc[:, 2 + r:3 + r], ffreq[r].rearrange("(p o) -> p o", o=1), [], [fsc], slow=True)
    inv2pi = 1.0 / (2.0 * math.pi)
    ts(fsc[:, 2:4], fsc[:, 2:4], inv2pi, None, ALU.mult, None, [fsc], [fsc])
    tt(fsc[:, 4:6], fsc[:, 0:2], fsc[:, 2:4], ALU.mult, [fsc], [fsc])
    fb3T = alloc("fb3T", [128, 32]); skT = alloc("skT", [128, 16]); adT = alloc("adT", [128, 8])
    ld(fb3T[:], fb3.rearrange("(q p) -> p q", p=128), [], [fb3T], slow=True)
    for r in range(2):
        ld(skT[:, 8 * r:8 * r + 8], skipw[r].rearrange("(q p) -> p q", p=128), [], [skT], slow=True)
    ld(adT[:], absdel.rearrange("(q p) -> p q", p=128), [], [adT], slow=True)
    ts(adT[:], adT[:], -1.0, None, ALU.mult, None, [adT], [adT])
    CH = 512
    featc = alloc("featc", [33, CH]); auxb = alloc("auxb", [128, 4, CH])
    h1 = alloc("h1", [64, CH]); h2 = alloc("h2", [64, CH]); tq = alloc("tq", [64, CH]); tk = alloc("tk", [64, CH])
    dec = alloc("dec", [128, CH]); ff = alloc("ff", [128, CH]); fbk = alloc("fbk", [128, CH]); rrow = alloc("rrow", [128, CH], BF16)
    MAGIC = 12582912.0
    SC2PI = 2.0 * math.pi * (1.0 - 2e-6)

    def sin_layer(outb, pb, col):
        ts(tq[:], pb.t[0:64, 0:CH], fsc[:, 2 + col:3 + col], fsc[:, 4 + col:5 + col], ALU.mult, ALU.add, [pb, fsc], [tq])
        ts(tk[:], tq[:], MAGIC, MAGIC, ALU.add, ALU.subtract, [tq], [tk])
        tt(tq[:], tq[:], tk[:], ALU.subtract, [tq, tk], [tq])
        act(outb[:], tq[:], AF.Sin, [tq], [outb], scale=SC2PI)

    for nm, o in (("s1", 0), ("s2", 1), ("p1", 0), ("p2", 1)):
        fdr, axr, L = ftab[nm]
        for c0 in range(0, L, CH):
            ld(featc[:], fdr[:, c0:c0 + CH], [], [featc])
            ld(auxb[:], axr[:, c0:c0 + CH].partition_broadcast(128), [], [auxb])
            mm(psum[0].t[0:64, 0:CH], fw1s[:, :], featc[:, :], True, True, [fw1s, featc], [psum[0]])
            sin_layer(h1, psum[0], 0)
            mm(psum[1].t[0:64, 0:CH], fw2s[:, :], h1[:, :], True, True, [fw2s, h1], [psum[1]])
            sin_layer(h2, psum[1], 1)
            for cc in range(8):
                qf = (o * 2 + 0) * 8 + cc
                qb = (o * 2 + 1) * 8 + cc
                mm(psum[2].t[:, 0:CH], fw3s[:, qf * 128:qf * 128 + 128], h2[:, :], True, True, [fw3s, h2], [psum[2]])
                mm(psum[3].t[:, 0:CH], fw3s[:, qb * 128:qb * 128 + 128], h2[:, :], True, True, [fw3s, h2], [psum[3]])
                ts(ff[:], psum[2].t[:, 0:CH], fb3T[:, qf:qf + 1], None, ALU.add, None, [psum[2], fb3T], [ff])
                ts(fbk[:], psum[3].t[:, 0:CH], fb3T[:, qb:qb + 1], None, ALU.add, None, [psum[3], fb3T], [fbk])
                tt(ff[:], ff[:], fbk[:], ALU.subtract, [ff, fbk], [ff])
                tt(ff[:], ff[:], auxb[:, 1, :], ALU.mult, [ff, auxb], [ff])
                tt(ff[:], ff[:], fbk[:], ALU.add, [ff, fbk], [ff])
                act(dec[:], auxb[:, 0, :], AF.Exp, [auxb, adT], [dec], scale=adT[:, cc:cc + 1])
                tt(ff[:], ff[:], dec[:], ALU.mult, [ff, dec], [ff])
                tt(ff[:], ff[:], auxb[:, 2, :], ALU.mult, [ff, auxb], [ff])
                stt(rrow[:], auxb[:, 3, :], skT[:, o * 8 + cc:o * 8 + cc + 1], ff[:], ALU.mult, ALU.add, [auxb, skT, ff], [rrow])
                ld(Rt[nm][cc * 128:cc * 128 + 128, c0:c0 + CH], rrow[:], [rrow], [dB["R"]])
    phase_reset(mark0)

    markY = ar_pos[0]
    yhT = alloc("yhT", [128, 8, NTOK], BF16)
    markA = ar_pos[0]
    xt = alloc("xt", [128, D]); xs = alloc("xs", [128, D], BF16); junk = alloc("junk", [128, D], BF16)
    ssq = alloc("ssq", [128, 2]); hTt = alloc("hTt", [128, 16, 128], BF16)

    def norm_T(src_ap, src_reads, r, gw, shbase, emit_dst):
        ld(xt[:], src_ap, src_reads, [xt])
        act(junk[:], xt[:], AF.Square, [xt], [junk, ssq], accum=ssq[:, 0:1])
        rsqrt_(ssq[:, 0:1], ssq[:, 0:1], 1.0 / D, ssq)
        act(xs[:], xt[:], AF.Copy, [xt, ssq], [xs], scale=ssq[:, 0:1])
        for c in range(16):
            pb = psum[4 + c // 8]
            tr(psb(4 + c // 8, BF16)[:, (c % 8) * 128:(c % 8) * 128 + 128], xs[:, c * 128:c * 128 + 128], idb[:], [xs, idb], [pb])
        for c in range(16):
            pb = psum[4 + c // 8]
            ts(hTt[:, c, :], psb(4 + c // 8, BF16)[:, (c % 8) * 128:(c % 8) * 128 + 128], gw[:, c, r:r + 1],
               modT[:, shbase + c, r:r + 1], ALU.mult, ALU.add, [pb, gw, modT], [hTt])
        emit_dst()

    hTo_v = hTo.rearrange("(c p) n -> p c n", p=128)
    hTs_v = hTs.rearrange("(c p) n -> p c n", p=128)
    for t in range(12):
        norm_T(x_own[t * 128:t * 128 + 128, :], [], 0 if t < 4 else 3, gw1, 0,
               lambda t=t: ld(hTo_v[:, :, t * 128:t * 128 + 128], hTt[:], [hTt], [dB["hTo"]]))
    for t in range(32):
        norm_T(x_seq[t * 128:t * 128 + 128, :], [], 3, gw1, 0,
               lambda t=t: ld(hTs_v[:, :, t * 128:t * 128 + 128], hTt[:], [hTt], [dB["hTs"]]))
    phase_reset(markA)

    own1h = din("own1h", [4])
    mskj = alloc("mskj", [128, 4])
    ld(mskj[:], own1h.partition_broadcast(128), [], [mskj])
    wst = alloc("wst", [128, 16, 128]); whb = alloc("whb", [128, 16, 3, 128], BF16)
    cw = alloc("cw", [128, 3, 4])
    hTc = alloc("hTc", [128, 16, 256], BF16)
    Pb = alloc("Pb", [128, 3, NSEQ], BF16)
    ut = alloc("ut", [128, 3, 128], BF16); utf = alloc("utf", [128, 3, 128])
    Z = alloc("Z", [128, 32, 3, 128], BF16)
    z1 = alloc("z1", [128, 32, 128], BF16); z2 = alloc("z2", [128, 8, 128], BF16); x2o = alloc("x2o", [128, 8, 128])
    NG = 4
    Gb = [alloc("G%d" % i, [128, L1S - 128], BF16) for i in range(NG)]
    gctr = [0]

    def hy_segment(cc, hT_v, hT_key, ntok, nseq, nblk, nm1, nm2, nout, out_col0):
        ntile = ntok // 128
        slen = nblk * 128
        for t0 in range(0, ntok, 256):
            ld(hTc[:], hT_v[:, :, t0:t0 + 256], [dB[hT_key]], [hTc])
            for o in range(3):
                pb = psum[o % 2]
                for c in range(16):
                    mm(pb.t[:, 0:256], whb[:, c, o, :], hTc[:, c, :], c == 0, c == 15, [whb, hTc], [pb])
                cp(Pb[:, o, t0:t0 + 256], pb.t[:, 0:256], [pb], [Pb], eng="act")
        for t in range(ntile):
            a0 = t * 128
            sfirst = (a0 % slen) == 0
            slast = ((a0 + 128) % slen) == 0
            for o in range(3):
                ts(utf[:, o, :], Pb[:, o, a0:a0 + 128], cw[:, o, 1:2], cw[:, o, 3:4], ALU.mult, ALU.add, [Pb, cw], [utf])
                lo = 1 if sfirst else 0
                stt(utf[:, o, lo:128], Pb[:, o, a0 + lo - 1:a0 + 127], cw[:, o, 0:1], utf[:, o, lo:128], ALU.mult, ALU.add, [Pb, cw, utf], [utf])
                hi = 127 if slast else 128
                stt(utf[:, o, 0:hi], Pb[:, o, a0 + 1:a0 + hi + 1], cw[:, o, 2:3], utf[:, o, 0:hi], ALU.mult, ALU.add, [Pb, cw, utf], [utf])
            cp(ut[:], utf[:], [utf], [ut], eng="act")
            pbk = psum[2 + t % 2]
            for o in range(3):
                tr(psb(2 + t % 2, BF16)[:, o * 128:o * 128 + 128], ut[:, o, :], idb[:], [ut, idb], [pbk])
            cp(Z[:, t, :, :], psb(2 + t % 2, BF16)[:, 0:384].rearrange("p (o c) -> p o c", o=3), [pbk], [Z])
            mm(pbk.t[:, 0:128], jdb[:, :], Z[:, t, 1, :], True, True, [jdb, Z], [pbk])
            cp(Z[:, t, 1, :], pbk.t[:, 0:128], [pbk], [Z], eng="act")
        L1 = ftab[nm1][2]
        ncol1 = ntile
        cpb1 = 512 // ncol1
        for c in range(128):
            if nseq == 1:
                g = Gb[gctr[0] % NG]; gctr[0] += 1
                src = bass.AP(tensor=Rt[nm1].tensor, offset=(cc * 128 + c) * L1, ap=[[1, 128], [1, L1 - 128]])
                ld(g[:, 0:L1 - 128], src, [dB["R"]], [g])
                gv = g.t
            else:
                if c % 8 == 0:
                    g = Gb[gctr[0] % NG]; gctr[0] += 1
                    src = bass.AP(tensor=Rt[nm1].tensor, offset=(cc * 128 + c) * L1, ap=[[1, 128], [L1, 8], [1, L1 - 128]])
                    ld(g[:, 0:8 * (L1 - 128)].rearrange("p (a n) -> p a n", a=8), src, [dB["R"]], [g])
                gv = g.t[:, (c % 8) * (L1 - 128):(c % 8 + 1) * (L1 - 128)]
            pb = psum[4 + (c // cpb1) % 2]
            cb = (c % cpb1) * ncol1
            das = [0] + [d for d in range(-(nblk - 1), nblk) if d != 0]
            for k, da in enumerate(das):
                f0 = 128 * (nblk - 1 - da)
                lo, hi = max(0, -da), min(nblk, nblk - da)
                if nseq == 1:
                    rhs = Z[:, lo:hi, 0, c]; out = pb.t[:, cb + lo + da:cb + hi + da]
                elif da == 0:
                    rhs = Z[:, 0:ntile, 0, c]; out = pb.t[:, cb:cb + ntile]
                else:
                    rhs = Z[:, lo:ntile:nblk, 0, c]; out = pb.t[:, cb + lo + da:cb + ntile:nblk]
                mm(out, gv[:, f0:f0 + 128], rhs, k == 0, k == len(das) - 1, [g, Z], [pb])
            if (c + 1) % cpb1 == 0:
                c0 = c + 1 - cpb1
                tt(z1[:, 0:ntile, c0:c0 + cpb1], pb.t[:, 0:cpb1 * ncol1].rearrange("p (c t) -> p t c", t=ncol1),
                   Z[:, 0:ntile, 1, c0:c0 + cpb1], ALU.mult, [pb, Z], [z1])
        L2 = ftab[nm2][2]
        cpb2 = 512 // nout
        if nseq == 1:
            for i in range(8):
                ts(x2o[:, i, :], Z[:, i, 2, :], mskj[:, 0:1], None, ALU.mult, None, [Z, mskj], [x2o])
                for jj in range(1, 4):
                    stt(x2o[:, i, :], Z[:, 8 * jj + i, 2, :], mskj[:, jj:jj + 1], x2o[:, i, :], ALU.mult, ALU.add, [Z, mskj, x2o], [x2o])
        for c in range(128):
            if nseq == 1:
                g = Gb[gctr[0] % NG]; gctr[0] += 1
                src = bass.AP(tensor=Rt[nm2].tensor, offset=(cc * 128 + c) * L2, ap=[[1, 128], [1, L2 - 128]])
                ld(g[:, 0:L2 - 128], src, [dB["R"]], [g])
                gv = g.t
            else:
                if c % 8 == 0:
                    g = Gb[gctr[0] % NG]; gctr[0] += 1
                    src = bass.AP(tensor=Rt[nm2].tensor, offset=(cc * 128 + c) * L2, ap=[[1, 128], [L2, 8], [1, L2 - 128]])
                    ld(g[:, 0:8 * (L2 - 128)].rearrange("p (a n) -> p a n", a=8), src, [dB["R"]], [g])
                gv = g.t[:, (c % 8) * (L2 - 128):(c % 8 + 1) * (L2 - 128)]
            pb = psum[6 + (c // cpb2) % 2]
            cb = (c % cpb2) * nout
            if nseq == 1:
                das = [0] + [d for d in range(-(nblk - 1), nout) if d != 0]
            else:
                das = [0] + [d for d in range(-(nblk - 1), nblk) if d != 0]
            for k, da in enumerate(das):
                f0 = 128 * (da + nblk - 1)
                if nseq == 1:
                    ilo, ihi = max(0, da), min(nout, nblk + da)
                    rhs = z1[:, ilo - da:ihi - da, c]; out = pb.t[:, cb + ilo:cb + ihi]
                elif da == 0:
                    rhs = z1[:, 0:ntile, c]; out = pb.t[:, cb:cb + ntile]
                else:
                    lo = max(0, -da)
                    rhs = z1[:, lo:ntile:nblk, c]; out = pb.t[:, cb + lo + da:cb + ntile:nblk]
                mm(out, gv[:, f0:f0 + 128], rhs, k == 0, k == len(das) - 1, [g, z1], [pb])
            if (c + 1) % cpb2 == 0:
                c0 = c + 1 - cpb2
                x2src = x2o[:, 0:nout, c0:c0 + cpb2] if nseq == 1 else Z[:, 0:nout, 2, c0:c0 + cpb2]
                tt(z2[:, 0:nout, c0:c0 + cpb2], pb.t[:, 0:cpb2 * nout].rearrange("p (c t) -> p t c", t=nout),
                   x2src, ALU.mult, [pb, Z, x2o], [z2])
        for i in range(nout):
            pbk = psum[2 + i % 2]
            tr(psb(2 + i % 2, BF16)[:, 0:128], z2[:, i, :], idb[:], [z2, idb], [pbk])
            cp(yhT[:, cc, out_col0 + i * 128:out_col0 + i * 128 + 128], psb(2 + i % 2, BF16)[:, 0:128], [pbk], [yhT], eng="act")

    for cc in range(ncc):
        for o in range(3):
            col = o * 1024 + cc * 128
            ld(wst[:], w_in[:, col:col + 128].rearrange("(c p) n -> p c n", p=128), [], [wst])
            cp(whb[:, :, o, :], wst[:], [wst], [whb])
            for tap in range(3):
                ld(cw[:, o, tap:tap + 1], convw[tap, col:col + 128].rearrange("(p o) -> p o", o=1), [], [cw], slow=True)
            ld(cw[:, o, 3:4], convb[col:col + 128].rearrange("(p o) -> p o", o=1), [], [cw], slow=True)
        hy_segment(cc, hTo_v, "hTo", 512, 2, 2, "p1", "p2", 4, 0)
        hy_segment(cc, hTs_v, "hTs", NSEQ, 1, 32, "s1", "s2", 8, 512)
    phase_reset(markA)

    if hy_only:
        ld(yhT_d, yhT[:], [yhT], [dB["dbg"]])
        S.op("sp", lambda e: e.nop(), [dB["dbg"]], [])
        S.finish(st)
        return nc, S, st
    ymT = alloc("ymT", [128, 16, NTOK], BF16)
    markC = ar_pos[0]
    wstgf = alloc("wstgf", [128, 2048])
    wstg = S.bufs[-1]
    wst16 = wstgf.t.rearrange("p (c n) -> p c n", c=16)
    gq = alloc("gq", [128, 8])
    gh = alloc("gh", [128, 4])
    ld(gq[:, 0:4], q_a_w.rearrange("(c p) -> p c", p=128), [], [gq], slow=True)
    ld(gq[:, 4:6], kv_a_w.rearrange("(c p) -> p c", p=128), [], [gq], slow=True)
    ld(gh[:, 0:1], qn_w.rearrange("(p o) -> p o", o=1), [], [gh], slow=True)
    ld(gh[:, 1:2], kn_w.rearrange("(p o) -> p o", o=1), [], [gh], slow=True)
    ld(gh[0:64, 2:3], qr_w.rearrange("(p o) -> p o", o=1), [], [gh], slow=True)
    ld(gh[0:64, 3:4], kr_w.rearrange("(p o) -> p o", o=1), [], [gh], slow=True)
    ckvT = alloc("ckvT", [128, 2, NKEY + 512], BF16)
    kpeT = alloc("kpeT", [64, NKEY + 512], BF16)
    qcT = alloc("qcT", [128, 4, NTOK], BF16)
    sq = alloc("sq", [128, 4, 512], BF16); rstd = alloc("rstd", [128, 512]); tmpf = alloc("tmpf", [128, 2, 512])
    rope_t = alloc("rope_t", [64, 2, 512]); ybf = alloc("ybf", [64, 512], BF16)
    markC2 = ar_pos[0]
    wq = alloc("wq", [128, 16, 512], BF16); wkv = alloc("wkv", [128, 16, 320], BF16)
    for hh in range(4):
        ld(wst16, w_in[:, 3072 + 128 * hh:3200 + 128 * hh].rearrange("(c p) n -> p c n", p=128), [], [wstg])
        cp(wq[:, :, 128 * hh:128 * hh + 128], wst16, [wstg], [wq])
    for hh in range(2):
        ld(wst16, w_in[:, 3584 + 128 * hh:3712 + 128 * hh].rearrange("(c p) n -> p c n", p=128), [], [wstg])
        cp(wkv[:, :, 128 * hh:128 * hh + 128], wst16, [wstg], [wkv])
    ld(wst16[:, :, 0:64], w_in[:, 3840:3904].rearrange("(c p) n -> p c n", p=128), [], [wstg]); cp(wkv[:, :, 256:320], wst16[:, :, 0:64], [wstg], [wkv])

    def fm_norm(pbs, rows, dim, gains, outs, out_bufs, extra_reads=()):
        n = len(pbs)
        for i, pb in enumerate(pbs):
            act(sq[0:rows, i, :], pb.t[0:rows, :], AF.Square, [pb], [sq])
        for i in range(n):
            mm(psum[7].t[0:rows, :], onesb[0:rows, 0:rows], sq[0:rows, i, :], i == 0, i == n - 1, [onesb, sq], [psum[7]])
        ts(rstd[0:rows, :], psum[7].t[0:rows, :], 1.0 / dim, EPS, ALU.mult, ALU.add, [psum[7]], [rstd])
        act(rstd[0:rows, :], rstd[0:rows, :], AF.Sqrt, [rstd], [rstd])
        S.op("dve", lambda e: e.reciprocal(out=rstd[0:rows, :], in_=rstd[0:rows, :]), [rstd], [rstd])
        for i, pb in enumerate(pbs):
            stt(outs[i], pb.t[0:rows, :], gains[i], rstd[0:rows, :], ALU.mult, ALU.mult, [pb, rstd] + list(extra_reads), out_bufs)

    def rope_fm(src_f32, dst_bf, tab_ap, reads, dst_buf):
        ld(rope_t[:], tab_ap.rearrange("a p n -> p a n"), [], [rope_t])
        cp(ybf[:], src_f32, reads, [ybf])
        mm(psum[7].t[0:64, :], protb[:, :], ybf[:, :], True, True, [protb, ybf], [psum[7]])
        tt(src_f32, src_f32, rope_t[:, 0, :], ALU.mult, reads + [rope_t], reads)
        tt(rstd[0:64, :], psum[7].t[0:64, :], rope_t[:, 1, :], ALU.mult, [psum[7], rope_t], [rstd])
        tt(dst_bf, src_f32, rstd[0:64, :], ALU.add, reads + [rstd], [dst_buf])

    def kv_chunk(hT_v, key, t0, kcol0, is_prompt):
        ld(hTc[:], hT_v[:, :, t0:t0 + 512], [dB[key]], [hTc])
        for m in range(2):
            for c in range(16):
                mm(psum[m].t[:, :], wkv[:, c, 128 * m:128 * m + 128], hTc[:, c, :], c == 0, c == 15, [wkv, hTc], [psum[m]])
        for c in range(16):
            mm(psum[2].t[0:64, :], wkv[:, c, 256:320], hTc[:, c, :], c == 0, c == 15, [wkv, hTc], [psum[2]])
        fm_norm([psum[0], psum[1]], 128, 256.0, [gq[:, 4:5], gq[:, 5:6]], [tmpf[:, 0, :], tmpf[:, 1, :]], [tmpf], [gq])
        cp(ckvT[:, :, kcol0:kcol0 + 512], tmpf[:], [tmpf], [ckvT])
        if is_prompt:
            for m in range(2):
                ld(ckv_out[t0:t0 + 512, 128 * m:128 * m + 128].rearrange("t p -> p t"), tmpf[:, m, :], [tmpf], [dB["ckv"]], slow=True)
        fm_norm([psum[2]], 64, 64.0, [gh[0:64, 3:4]], [tmpf[0:64, 0, :]], [tmpf], [gh])
        if is_prompt:
            ld(kpe_out[t0:t0 + 512, :].rearrange("t p -> p t"), tmpf[0:64, 0, :], [tmpf], [dB["kpe"]], slow=True)
            cp(kpeT[:, kcol0:kcol0 + 512], tmpf[0:64, 0, :], [tmpf], [kpeT])
        else:
            rope_fm(tmpf[0:64, 0, :], kpeT[:, kcol0:kcol0 + 512], ropek[:, :, t0:t0 + 512], [tmpf], kpeT)

    hTc2 = hTc
    hTc = alloc("hTcC", [128, 16, 512], BF16)
    for t0 in range(0, NSEQ, 512):
        kv_chunk(hTs_v, "hTs", t0, t0, False)
    kv_chunk(hTo_v, "hTo", 0, NKEY, True)
    cst = alloc("cst", [128, 2, 320]); cstb = alloc("cstb", [128, 2, 320], BF16)
    ld(cst[:, :, 0:256], c_ckv.rearrange("(a p) n -> p a n", p=128), [], [cst])
    ld(cst[:, :, 256:320], c_kpe.rearrange("(a p) n -> p a n", p=128), [], [cst])
    cp(cstb[:], cst[:], [cst], [cstb])
    for a in range(2):
        for m in range(2):
            tr(psb(3, BF16)[:, 0:128], cstb[:, a, 128 * m:128 * m + 128], idb[:], [cstb, idb], [psum[3]])
            cp(ckvT[:, m, NSEQ + a * 128:NSEQ + a * 128 + 128], psb(3, BF16)[:, 0:128], [psum[3]], [ckvT])
        tr(psb(3, BF16)[0:64, 0:128], cstb[:, a, 256:320], idb[:], [cstb, idb], [psum[3]])
        cp(kpeT[:, NSEQ + a * 128:NSEQ + a * 128 + 128], psb(3, BF16)[0:64, 0:128], [psum[3]], [kpeT])
    for tcn in range(3):
        ld(hTc[:], hTo_v[:, :, tcn * 512:tcn * 512 + 512], [dB["hTo"]], [hTc])
        for m in range(4):
            for c in range(16):
                mm(psum[m].t[:, :], wq[:, c, 128 * m:128 * m + 128], hTc[:, c, :], c == 0, c == 15, [wq, hTc], [psum[m]])
        fm_norm([psum[m] for m in range(4)], 128, 512.0, [gq[:, m:m + 1] for m in range(4)],
                [qcT[:, m, tcn * 512:tcn * 512 + 512] for m in range(4)], [qcT], [gq])
    phase_reset(markC2)
    wuqh = alloc("wuqh", [128, 4, 192], BF16); wukvh = alloc("wukvh", [128, 2, 256], BF16)
    knT = alloc("knT", [128, NKEY + 512], BF16); Vt = alloc("Vt", [128, 38, 128], BF16)
    qnT = alloc("qnT", [128, 512], BF16); qpT = alloc("qpT", [64, 512], BF16); qpf = alloc("qpf", [64, 512])
    PT = [alloc("PT%d" % i, [128, 512], BF16) for i in range(2)]
    SCALE = 192.0 ** -0.5
    for h in range(16):
        vq = wstgf.t[:, 0:768].rearrange("p (c n) -> p c n", c=4)
        vk = wstgf.t[:, 1024:1536].rearrange("p (c n) -> p c n", c=2)
        ld(vq, w_uq[:, 192 * h:192 * h + 192].rearrange("(c p) n -> p c n", p=128), [], [wstg])
        cp(wuqh[:], vq, [wstg], [wuqh])
        ld(vk, w_ukv[:, 256 * h:256 * h + 256].rearrange("(c p) n -> p c n", p=128), [], [wstg])
        cp(wukvh[:], vk, [wstg], [wukvh])
        nkt = (NKEY + 512) // 128
        for k0 in range(0, NKEY + 512, 512):
            w = min(512, NKEY + 512 - k0)
            for kc in range(2):
                mm(psum[0].t[:, 0:w], wukvh[:, kc, 0:128], ckvT[:, kc, k0:k0 + w], kc == 0, kc == 1, [wukvh, ckvT], [psum[0]])
            act(sq[:, 0, 0:w], psum[0].t[:, 0:w], AF.Square, [psum[0]], [sq])
            mm(psum[7].t[:, 0:w], onesb[:, :], sq[:, 0, 0:w], True, True, [onesb, sq], [psum[7]])
            ts(rstd[:, 0:w], psum[7].t[:, 0:w], 1.0 / 128, EPS, ALU.mult, ALU.add, [psum[7]], [rstd])
            act(rstd[:, 0:w], rstd[:, 0:w], AF.Sqrt, [rstd], [rstd])
            S.op("dve", lambda e, w=w: e.reciprocal(out=rstd[:, 0:w], in_=rstd[:, 0:w]), [rstd], [rstd])
            stt(knT[:, k0:k0 + w], psum[0].t[:, 0:w], gh[:, 1:2], rstd[:, 0:w], ALU.mult, ALU.mult, [psum[0], rstd, gh], [knT])
        for kt in range(nkt):
            pb = psum[1 + kt % 2]
            for kc in range(2):
                mm(pb.t[:, 0:128], ckvT[:, kc, kt * 128:kt * 128 + 128], wukvh[:, kc, 128:256], kc == 0, kc == 1, [ckvT, wukvh], [pb])
            cp(Vt[:, kt, :], pb.t[:, 0:128], [pb], [Vt], eng="act")
        groups = [(0, 256, [34, 35], None), (256, 256, [36, 37], None),
                  (512, 512, list(range(34)), 0), (1024, 512, list(range(34)), 512)]
        for (q0, nq, kts, rp) in groups:
            for kc in range(4):
                mm(psum[0].t[:, 0:nq], wuqh[:, kc, 0:128], qcT[:, kc, q0:q0 + nq], kc == 0, kc == 3, [wuqh, qcT], [psum[0]])
            for kc in range(4):
                mm(psum[3].t[0:64, 0:nq], wuqh[:, kc, 128:192], qcT[:, kc, q0:q0 + nq], kc == 0, kc == 3, [wuqh, qcT], [psum[3]])
            act(sq[:, 0, 0:nq], psum[0].t[:, 0:nq], AF.Square, [psum[0]], [sq])
            mm(psum[7].t[:, 0:nq], onesb[:, :], sq[:, 0, 0:nq], True, True, [onesb, sq], [psum[7]])
            ts(rstd[:, 0:nq], psum[7].t[:, 0:nq], 1.0 / 128, EPS, ALU.mult, ALU.add, [psum[7]], [rstd])
            act(rstd[:, 0:nq], rstd[:, 0:nq], AF.Sqrt, [rstd], [rstd])
            S.op("dve", lambda e, nq=nq: e.reciprocal(out=rstd[:, 0:nq], in_=rstd[:, 0:nq]), [rstd], [rstd])
            stt(qnT[:, 0:nq], psum[0].t[:, 0:nq], gh[:, 0:1], rstd[:, 0:nq], ALU.mult, ALU.mult, [psum[0], rstd, gh], [qnT])
            act(sq[0:64, 1, 0:nq], psum[3].t[0:64, 0:nq], AF.Square, [psum[3]], [sq])
            mm(psum[7].t[0:64, 0:nq], onesb[0:64, 0:64], sq[0:64, 1, 0:nq], True, True, [onesb, sq], [psum[7]])
            ts(rstd[0:64, 0:nq], psum[7].t[0:64, 0:nq], 1.0 / 64, EPS, ALU.mult, ALU.add, [psum[7]], [rstd])
            act(rstd[0:64, 0:nq], rstd[0:64, 0:nq], AF.Sqrt, [rstd], [rstd])
            S.op("dve", lambda e, nq=nq: e.reciprocal(out=rstd[0:64, 0:nq], in_=rstd[0:64, 0:nq]), [rstd], [rstd])
            stt(qpf[:, 0:nq], psum[3].t[0:64, 0:nq], gh[0:64, 2:3], rstd[0:64, 0:nq], ALU.mult, ALU.mult, [psum[3], rstd, gh], [qpf])
            if rp is None:
                cp(qpT[:, 0:nq], qpf[:, 0:nq], [qpf], [qpT])
            else:
                rope_fm(qpf[:, :], qpT[:, :], ropeq[:, :, rp:rp + 512], [qpf], qpT)
            for i, kt in enumerate(kts):
                pbs_ = psum[1 + i % 2]
                mm(pbs_.t[:, 0:nq], knT[:, kt * 128:kt * 128 + 128], qnT[:, 0:nq], True, False, [knT, qnT], [pbs_])
                mm(pbs_.t[:, 0:nq], kpeT[:, kt * 128:kt * 128 + 128], qpT[:, 0:nq], False, True, [kpeT, qpT], [pbs_])
                pt = PT[i % 2]
                act(pt[:, 0:nq], pbs_.t[:, 0:nq], AF.Exp, [pbs_], [pt], scale=SCALE, bias=-8.0)
                mm(psum[4].t[:, 0:nq], Vt[:, kt, :], pt[:, 0:nq], i == 0, i == len(kts) - 1, [Vt, pt], [psum[4]])
                mm(psum[5].t[:, 0:nq], onesb[:, :], pt[:, 0:nq], i == 0, i == len(kts) - 1, [onesb, pt], [psum[5]])
            S.op("dve", lambda e, nq=nq: e.reciprocal(out=rstd[:, 0:nq], in_=psum[5].t[:, 0:nq]), [psum[5]], [rstd])
            tt(ymT[:, h, q0:q0 + nq], psum[4].t[:, 0:nq], rstd[:, 0:nq], ALU.mult, [psum[4], rstd], [ymT])
    if dbg:
        ld(yhT_d, yhT[:], [yhT], [dB["dbg"]]); ld(ymT_d, ymT[:], [ymT], [dB["dbg"]])
    phase_reset(markC)

    markD = ar_pos[0]
    wsDl = [alloc("wsD%d" % i, [128, 2048]) for i in range(2)]
    wsD16l = [w.t.rearrange("p (c n) -> p c n", c=16) for w in wsDl]
    wghl = [alloc("wgh%d" % i, [128, 16, 128], BF16) for i in range(2)]; wgml = [alloc("wgm%d" % i, [128, 16, 128], BF16) for i in range(2)]
    whyl = [alloc("why%d" % i, [128, 8, 128], BF16) for i in range(2)]; wmll = [alloc("wml%d" % i, [128, 16, 128], BF16) for i in range(2)]
    dctr = 0
    hTd = alloc("hTd", [128, 16, 512], BF16); mgT = alloc("mgT", [128, 16, 512], BF16)
    sg1 = alloc("sg1", [128, 512]); sg2 = alloc("sg2", [128, 512])
    wob = alloc("wob", [128, 16, 256], BF16)
    g1b = alloc("g1b", [128, 2, D])
    xres = alloc("xres", [128, 256]); x1t = alloc("x1t", [128, 256])
    for i, r in enumerate((0, 3)):
        ld(g1b[:, i, :], modD[r, 2 * D:3 * D].partition_broadcast(128), [dB["modD"]], [g1b])
    for tcn in range(3):
        gi = 0 if tcn == 0 else 1
        ld(hTd[:], hTo_v[:, :, tcn * 512:tcn * 512 + 512], [dB["hTo"]], [hTd])
        for m in range(16):
            wgh, wgm, why, wml = wghl[m % 2], wgml[m % 2], whyl[m % 2], wmll[m % 2]
            for (dst, col0, nk, src) in ((wgh, 3904 + 128 * m, 16, w_in), (wgm, 5952 + 128 * m, 16, w_in),
                                         (why, 128 * m, 8, w_hy_out), (wml, 128 * m, 16, w_mla_out)):
                wsDb = wsDl[dctr % 2]; wsD16 = wsD16l[dctr % 2]
                ld(wsD16[:, 0:nk, :], src[:, col0:col0 + 128].rearrange("(c p) n -> p c n", p=128), [], [wsDb])
                cp(dst[:], wsD16[:, 0:nk, :], [wsDb], [dst], eng=("pool" if dctr % 2 == 0 else "dve"))
                dctr += 1
            for c in range(16):
                mm(psum[0].t[:, :], wgh[:, c, :], hTd[:, c, :], c == 0, c == 15, [wgh, hTd], [psum[0]])
            for c in range(16):
                mm(psum[1].t[:, :], wgm[:, c, :], hTd[:, c, :], c == 0, c == 15, [wgm, hTd], [psum[1]])
            for c in range(8):
                mm(psum[2].t[:, :], why[:, c, :], yhT[:, c, tcn * 512:tcn * 512 + 512], c == 0, c == 7, [why, yhT], [psum[2]])
            for c in range(16):
                mm(psum[3].t[:, :], wml[:, c, :], ymT[:, c, tcn * 512:tcn * 512 + 512], c == 0, c == 15, [wml, ymT], [psum[3]])
            act(sg1[:], psum[0].t[:, :], AF.Sigmoid, [psum[0]], [sg1])
            act(sg2[:], psum[1].t[:, :], AF.Sigmoid, [psum[1]], [sg2])
            tt(sg1[:], sg1[:], psum[2].t[:, :], ALU.mult, [sg1, psum[2]], [sg1])
            tt(sg2[:], sg2[:], psum[3].t[:, :], ALU.mult, [sg2, psum[3]], [sg2])
            tt(mgT[:, m, :], sg1[:], sg2[:], ALU.add, [sg1, sg2], [mgT])
        for c8 in range(8):
            for hh in range(2):
                wsDb = wsDl[dctr % 2]; wsD16 = wsD16l[dctr % 2]
                ld(wsD16[:, :, :], w_o[:, 256 * c8 + 128 * hh:256 * c8 + 128 * hh + 128].rearrange("(c p) n -> p c n", p=128), [], [wsDb])
                cp(wob[:, :, 128 * hh:128 * hh + 128], wsD16[:, :, :], [wsDb], [wob], eng=("pool" if dctr % 2 == 0 else "dve"))
                dctr += 1
            for t4 in range(4):
                row0 = tcn * 512 + t4 * 128
                pb = psum[4 + t4 % 2]
                for c in range(16):
                    mm(pb.t[:, 0:256], mgT[:, c, t4 * 128:t4 * 128 + 128], wob[:, c, :], c == 0, c == 15, [mgT, wob], [pb])
                ld(xres[:], x_own[row0:row0 + 128, 256 * c8:256 * c8 + 256], [], [xres])
                tt(x1t[:], pb.t[:, 0:256], g1b[:, gi, 256 * c8:256 * c8 + 256], ALU.mult, [pb, g1b], [x1t])
                tt(x1t[:], x1t[:], xres[:], ALU.add, [x1t, xres], [x1t])
                ld(x1d[row0:row0 + 128, 256 * c8:256 * c8 + 256], x1t[:], [x1t], [dB["x1d"]])
    phase_reset(markY)

    h2T = alloc("h2T", [128, 16, NTOK], BF16)
    wts = alloc("wts", [128, 12, NE])
    markE = ar_pos[0]
    xt = alloc("xt2", [128, D]); xs = alloc("xs2", [128, D], BF16); junk = alloc("junk2", [128, D], BF16)
    ssq = alloc("ssq2", [128, 2]); hTt = alloc("hTt2", [128, 16, 128], BF16)
    wrs = alloc("wrs", [128, 16, NE]); wrb = alloc("wrb", [128, 16, NE], BF16); brb = alloc("brb", [128, NE])
    lg = alloc("lg", [128, NE]); mx8 = alloc("mx8", [128, 8]); msk = alloc("msk", [128, NE]); sm = alloc("sm", [128, 2])
    bdn = alloc("bdn", [NE, D]); wtT = alloc("wtT", [NE, 128]); ybi = alloc("ybi", [128, D])
    ld(wrs[:], w_router.rearrange("(c p) n -> p c n", p=128), [], [wrs]); cp(wrb[:], wrs[:], [wrs], [wrb])
    ld(brb[:], b_router.partition_broadcast(128), [], [brb])
    ld(bdn[:], b_dn, [], [bdn])
    for t in range(12):
        r = 0 if t < 4 else 3
        norm_T(x1d[t * 128:t * 128 + 128, :], [dB["x1d"]], r, gw2, 48,
               lambda t=t: cp(h2T[:, :, t * 128:t * 128 + 128], hTt[:], [hTt], [h2T], eng="pool"))
        for c in range(16):
            mm(psum[0].t[:, 0:NE], hTt[:, c, :], wrb[:, c, :], c == 0, c == 15, [hTt, wrb], [psum[0]])
        tt(lg[:], psum[0].t[:, 0:NE], brb[:], ALU.add, [psum[0], brb], [lg])
        S.op("dve", lambda e: e.max(out=mx8[:], in_=lg[:]), [lg], [mx8])
        ts(msk[:], lg[:], mx8[:, 3:4], None, ALU.is_ge, None, [lg, mx8], [msk])
        ts(sm[:, 1:2], mx8[:, 0:1], -1.0, None, ALU.mult, None, [mx8], [sm])
        act(lg[:], lg[:], AF.Exp, [lg, sm], [lg], bias=sm[:, 1:2])
        tt(lg[:], lg[:], msk[:], ALU.mult, [lg, msk], [lg])
        S.op("dve", lambda e: e.reduce_sum(out=sm[:, 0:1], in_=lg[:], axis=AX.X), [lg], [sm])
        S.op("dve", lambda e: e.reciprocal(out=sm[:, 0:1], in_=sm[:, 0:1]), [sm], [sm])
        ts(wts[:, t, :], lg[:], sm[:, 0:1], None, ALU.mult, None, [lg, sm], [wts])
        tr(psum[1].t[0:NE, 0:128], wts[:, t, :], idf[:], [wts, idf], [psum[1]])
        cp(wtT[:], psum[1].t[0:NE, 0:128], [psum[1]], [wtT])
        for c4 in range(4):
            mm(psum[2 + c4 % 2].t[:, :], wtT[:, :], bdn[:, 512 * c4:512 * c4 + 512], True, True, [wtT, bdn], [psum[2 + c4 % 2]])
            cp(ybi[:, 512 * c4:512 * c4 + 512], psum[2 + c4 % 2].t[:, :], [psum[2 + c4 % 2]], [ybi], eng="act")
        ld(yacc[:, :, t, :].rearrange("c p n -> p c n"), ybi[:].rearrange("p (c n) -> p c n", c=8), [ybi], [dB["yacc"]])
    if dbg:
        ld(wts_d, wts[:], [wts], [dB["dbg"]]); ld(h2T_d, h2T[:], [h2T], [dB["dbg"]])
    phase_reset(markE)

    hid = alloc("hid", [128, 16, NTOK], BF16)
    wsE = [alloc("wsE%d" % i, [128, 16, 256]) for i in range(2)]
    wgb = [alloc("wgb%d" % i, [128, 16, 2, 128], BF16) for i in range(2)]
    wdb = [alloc("wdb%d" % i, [128, 16, 256], BF16) for i in range(2)]
    bgT = [alloc("bgT%d" % i, [128, 2]) for i in range(2)]
    gc = [alloc("gc%d" % i, [128, 512]) for i in range(2)]; sgm = [alloc("sgm%d" % i, [128, 512], BF16) for i in range(2)]
    uc = [alloc("uc%d" % i, [128, 512], BF16) for i in range(2)]
    osb = [alloc("osb%d" % i, [128, 6, 256]) for i in range(2)]
    wctr = 0; ectr = 0; octr = 0
    for e_ in range(0 if skip_moe else NE):
        for j in range(16):
            ws = wsE[wctr % 2]; wg = wgb[wctr % 2]; bg = bgT[wctr % 2]; wctr += 1
            ld(ws[:], w_gu[e_, :, 256 * j:256 * j + 256].rearrange("(c p) n -> p c n", p=128), [], [ws])
            cp(wg[:], ws[:].rearrange("p c (f two) -> p c two f", two=2), [ws], [wg], eng="act")
            ld(bg[:], b_gu[e_, 256 * j:256 * j + 256].rearrange("(f two) -> f two", two=2), [], [bg], slow=True)
            for tcn in range(3):
                cols = slice(tcn * 512, tcn * 512 + 512)
                pg, pu = (psum[0], psum[1]) if ectr % 2 == 0 else (psum[6], psum[7])
                g_, s_, u_ = gc[ectr % 2], sgm[ectr % 2], uc[ectr % 2]
                ectr += 1
                for c in range(16):
                    mm(pg.t[:, :], wg[:, c, 0, :], h2T[:, c, cols], c == 0, c == 15, [wg, h2T], [pg])
                for c in range(16):
                    mm(pu.t[:, :], wg[:, c, 1, :], h2T[:, c, cols], c == 0, c == 15, [wg, h2T], [pu])
                ts(g_[:], pg.t[:, :], bg[:, 0:1], 7.0, ALU.add, ALU.min, [pg, bg], [g_])
                act(s_[:], g_[:], AF.Sigmoid, [g_], [s_], scale=1.702)
                ts(u_[:], pu.t[:, :], bg[:, 1:2], 7.0, ALU.add, ALU.min, [pu, bg], [u_])
                ts(u_[:], u_[:], -7.0, 1.0, ALU.max, ALU.add, [u_], [u_])
                tt(g_[:], g_[:], s_[:], ALU.mult, [g_, s_], [g_])
                tt(hid[:, j, cols], g_[:], u_[:], ALU.mult, [g_, u_], [hid], eng=("pool" if tcn == 1 else "dve"))
        for c8 in range(8):
            ws = wsE[wctr % 2]; wd = wdb[wctr % 2]; wctr += 1
            ld(ws[:], w_dn[e_, :, 256 * c8:256 * c8 + 256].rearrange("(j p) n -> p j n", p=128), [], [ws])
            cp(wd[:], ws[:], [ws], [wd], eng="act")
            for t in range(12):
                if t % 6 == 0:
                    ob = osb[octr % 2]; octr += 1
                pb = psum[2 + t % 4]
                for j in range(16):
                    mm(pb.t[:, 0:256], hid[:, j, t * 128:t * 128 + 128], wd[:, j, :], j == 0, j == 15, [hid, wd], [pb])
                if t % 2 == 0:
                    ts(ob[:, t % 6, :], pb.t[:, 0:256], wts[:, t, e_:e_ + 1], None, ALU.mult, None, [pb, wts], [ob])
                else:
                    act(ob[:, t % 6, :], pb.t[:, 0:256], AF.Copy, [pb, wts], [ob], scale=wts[:, t, e_:e_ + 1])
                if t % 6 == 5:
                    t0_ = t - 5
                    S.dma("pool", lambda e, ob=ob, c8=c8, t0_=t0_: e.dma_start(out=yacc[c8, :, t0_:t0_ + 6, :], in_=ob[:], accum_op=ALU.add),
                          [ob], [dB["yacc"]])
    phase_reset(markE)
    g2b = alloc("g2b", [128, 2, D]); ya = alloc("ya", [128, D]); xf = alloc("xf", [128, D])
    for i, r in enumerate((0, 3)):
        ld(g2b[:, i, :], modD[r, 5 * D:6 * D].partition_broadcast(128), [dB["modD"]], [g2b])
    for t in range(12):
        gi = 0 if t < 4 else 1
        ld(ya[:].rearrange("p (c n) -> p c n", c=8), yacc[:, :, t, :].rearrange("c p n -> p c n"), [dB["yacc"]], [ya])
        ld(xf[:], x1d[t * 128:t * 128 + 128, :], [dB["x1d"]], [xf])
        tt(ya[:], ya[:], g2b[:, gi, :], ALU.mult, [ya, g2b], [ya])
        tt(ya[:], ya[:], xf[:], ALU.add, [ya, xf], [ya])
        ld(y_out[t * 128:t * 128 + 128, :], ya[:], [ya], [dB["out"]])
    S.op("sp", lambda e: e.nop(), [dB["out"], dB["ckv"], dB["kpe"], dB["dbg"]], [])
    S.finish(st)
    return nc, S, st


def _feat_table(lags, n):
    lags = np.asarray(lags, np.int64)
    valid = (np.abs(lags) <= n - 1)
    pos = np.where(lags >= 0, lags, -lags - 1)
    pos = np.clip(pos, 0, n - 1)
    t = np.linspace(0.0, 1.0, n, dtype=np.float32)
    w = (np.float32(2.0 * math.pi / n) * np.arange(n, dtype=np.float32)).astype(np.float32)
    bands = np.linspace(1e-4, 15.0, 16, dtype=np.float32)
    arg = (bands[None, :] * w[:, None]).astype(np.float32)
    feats = np.concatenate([t[:, None], np.cos(arg), -np.sin(arg)], axis=-1).astype(np.float32)
    f = np.ascontiguousarray(feats[pos].T)
    aux = np.stack([t[pos], (lags >= 0).astype(np.float32), valid.astype(np.float32),
                    (lags == 0).astype(np.float32)]).astype(np.float32)
    return f, np.ascontiguousarray(aux)


def _rope_tables(n_tokens):
    rows = n_tokens // 64
    row = np.repeat(np.arange(rows, dtype=np.float32), 64)
    col = np.tile(np.arange(64, dtype=np.float32), rows)
    n_freq = 16
    inv_freq = np.power(np.float32(10000.0), -np.arange(n_freq, dtype=np.float32) / n_freq).astype(np.float32)
    ang = np.concatenate([row[:, None] * inv_freq, col[:, None] * inv_freq], axis=-1)
    ang = np.concatenate([ang, ang], axis=-1).astype(np.float32)
    return np.cos(ang).astype(np.float32), np.sin(ang).astype(np.float32)


def kernel(x_prompt, x_sample, cache_ckv, cache_kpe, c, c_ctx, w_mod, b_mod, norm1_w, norm2_w,
           w_in, hy_conv_w, hy_conv_b, filt_w1, filt_b1, filt_w2, filt_b2, filt_w3, filt_b3,
           filt_freq, hy_skip, q_a_norm_w, w_uq, kv_a_norm_w, w_ukv, qn_norm_w, kn_norm_w,
           qr_norm_w, kr_norm_w, w_hy_out, w_mla_out, w_o, w_router, b_router, w_gate_up,
           b_gate_up, w_down, b_down):
    A = lambda a: np.ascontiguousarray(np.asarray(a, dtype=np.float32))
    x_prompt = A(x_prompt); x_sample = A(x_sample); cache_ckv = A(cache_ckv); cache_kpe = A(cache_kpe)
    c = A(c); c_ctx = A(c_ctx)
    dbg = bool(_NC_CACHE.get("dbg", False))
    if "nc" not in _NC_CACHE:
        _NC_CACHE["nc"] = build(dbg=dbg, skip_moe=dbg, **_NC_CACHE.get("bkw", {}))[0]
    nc = _NC_CACHE["nc"]
    ident = np.eye(128, dtype=np.float32)
    jdent = np.ascontiguousarray(ident[::-1])
    prot = np.zeros((64, 64), np.float32)
    for m in range(32):
        prot[m + 32, m] = -1.0
    for m in range(32, 64):
        prot[m - 32, m] = 1.0
    max_decay = math.log(1e-2) / 0.3
    min_decay = math.log(1e-2) / 1.5
    absdel = np.abs(np.linspace(min_decay, max_decay, 1024, dtype=np.float32)).astype(np.float32)
    cosr, sinr = _rope_tables(4096)
    ropek = np.ascontiguousarray(np.stack([cosr.T, sinr.T]))
    f_s1, a_s1 = _feat_table(4095 - np.arange(L1S), 4096)
    f_p1, a_p1 = _feat_table(255 - np.arange(LP), 256)
    f_p2, a_p2 = _feat_table(np.arange(LP) - 255, 256)
    shared = {
        "w_mod": A(w_mod[0]), "b_mod": A(b_mod[0]), "norm1_w": A(norm1_w[0]), "norm2_w": A(norm2_w[0]),
        "w_in": A(w_in[0]), "convw": A(hy_conv_w[0]), "convb": A(hy_conv_b[0]),
        "fw1": A(filt_w1[0]), "fb1": A(filt_b1[0]), "fw2": A(filt_w2[0]), "fb2": A(filt_b2[0]),
        "fw3": A(filt_w3[0]), "fb3": A(filt_b3[0]), "ffreq": A(filt_freq[0]), "skipw": A(hy_skip[0]),
        "q_a_w": A(q_a_norm_w[0]), "w_uq": A(w_uq[0]), "kv_a_w": A(kv_a_norm_w[0]), "w_ukv": A(w_ukv[0]),
        "qn_w": A(qn_norm_w[0]), "kn_w": A(kn_norm_w[0]), "qr_w": A(qr_norm_w[0]), "kr_w": A(kr_norm_w[0]),
        "w_hy_out": A(w_hy_out[0]), "w_mla_out": A(w_mla_out[0]), "w_o": A(w_o[0]),
        "w_router": A(w_router[0]), "b_router": A(b_router[0]),
        "w_gu": A(w_gate_up[0]), "b_gu": A(b_gate_up[0]), "w_dn": A(w_down[0]), "b_dn": A(b_down[0]),
        "ident": ident, "jdent": jdent, "prot": prot, "absdel": absdel, "ropek": ropek,
        "feat_s1": f_s1, "aux_s1": a_s1, "feat_p1": f_p1, "aux_p1": a_p1, "feat_p2": f_p2, "aux_p2": a_p2,
    }
    in_maps = []
    for k in range(8):
        b, j = k // 4, k % 4
        f_s2, a_s2 = _feat_table(np.arange(L2S) + 1024 * j - 4095, 4096)
        own1h = np.zeros(4, np.float32); own1h[j] = 1.0
        m = dict(shared)
        m.update({
            "x_own": np.ascontiguousarray(np.concatenate([x_prompt[2 * k], x_prompt[2 * k + 1],
                                                          x_sample[b, 1024 * j:1024 * j + 1024]], axis=0)),
            "x_seq": np.ascontiguousarray(x_sample[b]),
            "cvec": np.ascontiguousarray(np.stack([c_ctx, c[0], c[1], c[b]])),
            "c_ckv": np.ascontiguousarray(cache_ckv[b, 0]), "c_kpe": np.ascontiguousarray(cache_kpe[b, 0]),
            "feat_s2": f_s2, "aux_s2": a_s2, "own1h": own1h,
            "ropeq": np.ascontiguousarray(ropek[:, :, 1024 * j:1024 * j + 1024]),
        })
        in_maps.append(m)
    if dbg:
        in_maps = [{k2: v for k2, v in m.items() if k2 in _NC_CACHE["in_names"]} for m in in_maps]
    if dbg:
        res = run_bass_kernel_spmd(nc, in_maps, core_ids=list(range(8)), trace=True)
        print("EXEC_TIME_NS", res.exec_time_ns, flush=True)
        _NC_CACHE["res"] = res
        return None
    res = run_bass_kernel_spmd(nc, in_maps, core_ids=list(range(8)))
    y_p = np.zeros((16, 256, D), np.float32); y_s = np.zeros((2, 4096, D), np.float32)
    n_ckv = np.zeros((16, 1, 256, 256), np.float32); n_kpe = np.zeros((16, 1, 256, 64), np.float32)
    for k in range(8):
        b, j = k // 4, k % 4
        r = res.results[k]
        yo = np.asarray(r["y_out"], np.float32)
        y_p[2 * k] = yo[0:256]; y_p[2 * k + 1] = yo[256:512]
        y_s[b, 1024 * j:1024 * j + 1024] = yo[512:1536]
        ck = np.asarray(r["ckv_out"], np.float32); kp = np.asarray(r["kpe_out"], np.float32)
        n_ckv[2 * k, 0] = ck[0:256]; n_ckv[2 * k + 1, 0] = ck[256:512]
        n_kpe[2 * k, 0] = kp[0:256]; n_kpe[2 * k + 1, 0] = kp[256:512]
    return (y_p, y_s, n_ckv, n_kpe)
```
